# Optimizing a Trainium2 kernel written in Bass

```python
import jax, jax.numpy as jnp
from jax import lax
import numpy as np

D_MODEL = 1024
BATCH = 16
SEQ = 2048
DEPTH = 4

CHUNK = 64
Q_BLOCK = 128
DSA_HEADS = 8
DSA_HEAD_DIM = 64
DSA_WIDTH = DSA_HEADS * DSA_HEAD_DIM
Q_LORA = 256
KV_LORA = 128
IDX_HEADS = 4
IDX_DIM = 64
TOPK_MAX = 256
RWKV_HEADS = 8
RWKV_HEAD_DIM = 64
RWKV_WIDTH = RWKV_HEADS * RWKV_HEAD_DIM
DECAY_LORA = 64
AAA_LORA = 64
MV_LORA = 32
GATE_LORA = 160
MIX_WIDTH = DSA_WIDTH + RWKV_WIDTH
D_FF = ((8 * D_MODEL + 3 * 256 - 1) // (3 * 256)) * 256
NORM_EPS = 1e-6
LNX_EPS = 64e-5
DSA_COLS = [Q_LORA, KV_LORA, IDX_DIM, IDX_HEADS]
RWKV_COLS = [RWKV_WIDTH, RWKV_WIDTH, RWKV_WIDTH, DECAY_LORA, AAA_LORA, GATE_LORA]
N_DSA_COLS = sum(DSA_COLS)
N_RWKV_COLS = sum(RWKV_COLS)

kernel_name = 'hybrid_dsa_rwkv7_sandwich_adaln'


def rms_norm(x, g):
    xf = x.astype(jnp.float32)
    y = xf * lax.rsqrt(jnp.mean(xf * xf, axis=-1, keepdims=True) + NORM_EPS)
    return y.astype(x.dtype) * g


def layer_norm(x, g, b):
    xf = x.astype(jnp.float32)
    mu = jnp.mean(xf, axis=-1, keepdims=True)
    var = jnp.mean(jnp.square(xf - mu), axis=-1, keepdims=True)
    return ((xf - mu) * lax.rsqrt(var + NORM_EPS)).astype(x.dtype) * g + b


def split_cols(a, sizes):
    return jnp.split(a, np.cumsum(sizes)[:-1].tolist(), axis=-1)


def token_shift(u, mu):
    prev = jnp.pad(u, ((0, 0), (1, 0), (0, 0)))[:, :-1]
    return u + (prev - u) * mu


def dsa_mixer(c_q, c_kv, k_idx_raw, w_idx, q_norm_g, kv_norm_g, w_q_up, w_qi_up,
              w_k_up, w_v_up, kidx_ln_g, kidx_ln_b):
    B, S, _ = c_q.shape
    top_k = min(TOPK_MAX, S // 4)
    nblk = S // Q_BLOCK
    cq = rms_norm(c_q, q_norm_g)
    ckv = rms_norm(c_kv, kv_norm_g)
    q = jnp.einsum('bsl,lhd->bshd', cq, w_q_up)
    q_abs = jnp.einsum('bshd,rhd->bshr', q, w_k_up) * (DSA_HEAD_DIM ** -0.5)
    q_idx = jnp.einsum('bsl,lhd->bshd', cq, w_qi_up)
    k_idx = layer_norm(k_idx_raw, kidx_ln_g, kidx_ln_b)
    w_head = w_idx * ((IDX_HEADS ** -0.5) * (IDX_DIM ** -0.5))
    key_chunk = jnp.arange(S) // CHUNK

    def to_blocks(a):
        return a.reshape((B, nblk, Q_BLOCK) + a.shape[2:]).swapaxes(0, 1)

    def block_fn(args):
        qa, qi, wi, start = args
        q_chunk = (start + jnp.arange(Q_BLOCK)) // CHUNK
        admissible = key_chunk[None, :] <= q_chunk[:, None]
        logits = jnp.einsum('bthd,bsd->bths', qi, k_idx)
        score = jnp.einsum('bths,bth->bts', jax.nn.relu(logits), wi).astype(jnp.float32)
        score = jnp.where(admissible[None], score, -jnp.inf)
        top_val, top_idx = lax.top_k(score, top_k)
        valid = jnp.isfinite(top_val)
        kv_sel = jax.vmap(lambda lat, idx: lat[idx])(ckv, top_idx)
        att = jnp.einsum('bthr,btkr->bthk', qa, kv_sel).astype(jnp.float32)
        att = jnp.where(valid[:, :, None, :], att, -jnp.inf)
        p = jax.nn.softmax(att, axis=-1).astype(kv_sel.dtype)
        return jnp.einsum('bthk,btkr->bthr', p, kv_sel)

    starts = jnp.arange(nblk, dtype=jnp.int32) * Q_BLOCK
    o_lat = lax.map(block_fn, (to_blocks(q_abs), to_blocks(q_idx), to_blocks(w_head), starts))
    o_lat = o_lat.swapaxes(0, 1).reshape(B, S, DSA_HEADS, KV_LORA)
    o = jnp.einsum('bshr,rhd->bshd', o_lat, w_v_up)
    return o.reshape(B, S, DSA_WIDTH)


def rwkv7_mixer(r, k, v, w_lo, a_lo, g_lo, w0, w2, a0, a2, g2, k_k, k_a, r_k, lnx_g, lnx_b):
    B, S, _ = r.shape
    H, N = RWKV_HEADS, RWKV_HEAD_DIM
    f32 = jnp.float32

    def heads(t):
        return t.reshape(B, S, H, N)

    w_log = -jax.nn.softplus(-(w0 + jnp.tanh(w_lo) @ w2)) - 0.5
    decay = jnp.exp(-jnp.exp(w_log.astype(f32)))
    a = jax.nn.sigmoid(a0 + a_lo @ a2)
    g = jax.nn.sigmoid(g_lo) @ g2
    kk = heads(k * k_k).astype(f32)
    kk = kk / jnp.maximum(jnp.linalg.norm(kk, axis=-1, keepdims=True), 1e-12)
    k = k * (1 + (a - 1) * k_a)
    r_h, k_h, v_h, a_h = heads(r), heads(k), heads(v), heads(a)
    xs = [jnp.moveaxis(t.astype(f32), 1, 0) for t in (r_h, heads(decay), k_h, v_h, kk, a_h)]

    def step(state, inp):
        r_t, w_t, k_t, v_t, kk_t, a_t = inp
        sa = jnp.einsum('bhvk,bhk->bhv', state, kk_t)
        state = (state * w_t[:, :, None, :]
                 - sa[..., None] * (kk_t * a_t)[:, :, None, :]
                 + v_t[..., None] * k_t[:, :, None, :])
        return state, jnp.einsum('bhvk,bhk->bhv', state, r_t)

    state0 = jnp.zeros((B, H, N, N), f32)
    _, y = lax.scan(step, state0, (xs[0], xs[1], xs[2], xs[3], xs[4], xs[5]))
    y = jnp.moveaxis(y, 0, 1)
    mu = jnp.mean(y, axis=-1, keepdims=True)
    var = jnp.mean(jnp.square(y - mu), axis=-1, keepdims=True)
    y = ((y - mu) * lax.rsqrt(var + LNX_EPS)).reshape(B, S, RWKV_WIDTH).astype(r.dtype)
    y = y * lnx_g + lnx_b
    bonus = (jnp.sum(r_h * k_h * r_k, axis=-1, keepdims=True) * v_h).reshape(B, S, RWKV_WIDTH)
    return (y + bonus) * g


def setup_inputs(seed: int = 0) -> dict:
    key = jax.random.key(seed)
    keys = jax.random.split(key, 40)
    f32 = jnp.float32

    def nrm(i, shape, scale):
        return jax.random.normal(keys[i], shape, f32) * scale

    def gain(i, shape):
        return 1.0 + nrm(i, shape, 0.05)

    def unif(i, shape, lo, hi):
        return jax.random.uniform(keys[i], shape, f32, lo, hi)

    L, Lv, D, RW = DEPTH, DEPTH - 1, D_MODEL, RWKV_WIDTH
    P = N_DSA_COLS + N_RWKV_COLS
    return {
        'x': nrm(0, (BATCH, SEQ, D), 1.0),
        'c': nrm(1, (BATCH, D), 1.0),
        'ada_w': nrm(2, (L, D, 6 * D), 0.5 * D ** -0.5),
        'ada_b': nrm(3, (L, 6 * D), 0.02),
        'pre_g_mix': gain(4, (L, D)),
        'post_g_mix': gain(5, (L, D)),
        'pre_g_ffn': gain(6, (L, D)),
        'post_g_ffn': gain(7, (L, D)),
        'w_in': nrm(8, (L, D, P), D ** -0.5),
        'w_in_vres': nrm(9, (Lv, D, MV_LORA), D ** -0.5),
        'mu_shift': unif(10, (L, N_RWKV_COLS), 0.0, 1.0),
        'mu_vres': unif(11, (Lv, MV_LORA), 0.0, 1.0),
        'w_out': nrm(12, (L, MIX_WIDTH, D), MIX_WIDTH ** -0.5),
        'q_norm_g': gain(13, (L, Q_LORA)),
        'kv_norm_g': gain(14, (L, KV_LORA)),
        'w_q_up': nrm(15, (L, Q_LORA, DSA_HEADS, DSA_HEAD_DIM), Q_LORA ** -0.5),
        'w_qi_up': nrm(16, (L, Q_LORA, IDX_HEADS, IDX_DIM), Q_LORA ** -0.5),
        'w_k_up': nrm(17, (L, KV_LORA, DSA_HEADS, DSA_HEAD_DIM), KV_LORA ** -0.5),
        'w_v_up': nrm(18, (L, KV_LORA, DSA_HEADS, DSA_HEAD_DIM), KV_LORA ** -0.5),
        'kidx_ln_g': gain(19, (L, IDX_DIM)),
        'kidx_ln_b': nrm(20, (L, IDX_DIM), 0.02),
        'w0': unif(21, (L, RW), -3.0, 1.0),
        'w2': nrm(22, (L, DECAY_LORA, RW), 0.1),
        'a0': nrm(23, (L, RW), 0.1),
        'a2': nrm(24, (L, AAA_LORA, RW), 0.5 * AAA_LORA ** -0.5),
        'g2': nrm(25, (L, GATE_LORA, RW), GATE_LORA ** -0.5),
        'v0': nrm(26, (Lv, RW), 0.5),
        'v2': nrm(27, (Lv, MV_LORA, RW), 0.5 * MV_LORA ** -0.5),
        'k_k': 0.85 + nrm(28, (L, RW), 0.05),
        'k_a': gain(29, (L, RW)),
        'r_k': nrm(30, (L, RWKV_HEADS, RWKV_HEAD_DIM), 0.1),
        'lnx_g': gain(31, (L, RW)),
        'lnx_b': nrm(32, (L, RW), 0.02),
        'w_fc': nrm(33, (L, D, 2 * D_FF), D ** -0.5),
        'w_down': nrm(34, (L, D_FF, D), D_FF ** -0.5),
    }


def reference(x, c, ada_w, ada_b, pre_g_mix, post_g_mix, pre_g_ffn, post_g_ffn,
              w_in, w_in_vres, mu_shift, mu_vres, w_out,
              q_norm_g, kv_norm_g, w_q_up, w_qi_up, w_k_up, w_v_up, kidx_ln_g, kidx_ln_b,
              w0, w2, a0, a2, g2, v0, v2, k_k, k_a, r_k, lnx_g, lnx_b,
              w_fc, w_down):
    cond = jax.nn.silu(c)
    v_first = None
    for l in range(DEPTH):
        mod = cond @ ada_w[l] + ada_b[l]
        sh_a, sc_a, gt_a, sh_f, sc_f, gt_f = [m[:, None, :] for m in jnp.split(mod, 6, axis=-1)]

        h = rms_norm(x, pre_g_mix[l]) * (1 + sc_a) + sh_a
        if l == 0:
            w_proj, mu = w_in[l], mu_shift[l]
        else:
            w_proj = jnp.concatenate([w_in[l], w_in_vres[l - 1]], axis=-1)
            mu = jnp.concatenate([mu_shift[l], mu_vres[l - 1]], axis=-1)
        cols = h @ w_proj
        dsa_cols = cols[..., :N_DSA_COLS]
        rwkv_cols = token_shift(cols[..., N_DSA_COLS:], mu)
        c_q, c_kv, k_idx_raw, w_idx = split_cols(dsa_cols, DSA_COLS)
        if l == 0:
            r, k, v, w_lo, a_lo, g_lo = split_cols(rwkv_cols, RWKV_COLS)
            v_first = v
        else:
            r, k, v, w_lo, a_lo, g_lo, mv_lo = split_cols(rwkv_cols, RWKV_COLS + [MV_LORA])
            v = v + (v_first - v) * jax.nn.sigmoid(v0[l - 1] + mv_lo @ v2[l - 1])
        y_dsa = dsa_mixer(c_q, c_kv, k_idx_raw, w_idx, q_norm_g[l], kv_norm_g[l], w_q_up[l],
                          w_qi_up[l], w_k_up[l], w_v_up[l], kidx_ln_g[l], kidx_ln_b[l])
        y_rwkv = rwkv7_mixer(r, k, v, w_lo, a_lo, g_lo, w0[l], w2[l], a0[l], a2[l], g2[l],
                             k_k[l], k_a[l], r_k[l], lnx_g[l], lnx_b[l])
        y = jnp.concatenate([y_dsa, y_rwkv], axis=-1) @ w_out[l]
        x = x + gt_a * rms_norm(y, post_g_mix[l])

        h = rms_norm(x, pre_g_ffn[l]) * (1 + sc_f) + sh_f
        gate, up = jnp.split(h @ w_fc[l], 2, axis=-1)
        y = (jax.nn.silu(gate) * up) @ w_down[l]
        x = x + gt_f * rms_norm(y, post_g_ffn[l])
    return x
```

```python
import contextlib
import numpy as np
import concourse.bass as bass
import concourse.mybir as mybir
from concourse.bass_utils import run_bass_kernel_spmd
from concourse.alu_op_type import AluOpType as ALU

F32 = mybir.dt.float32
BF16 = mybir.dt.bfloat16
AF = mybir.ActivationFunctionType
AX = mybir.AxisListType

N_DMA_SLOTS = 32
D = 1024
L = 4
S = 2048
NB = 2
T = NB * S
PC = 2432
DFF = 2816
NEG = -1.0e30


class Prog:
    ENGS = ('pe', 'dve', 'act', 'pool', 'sp')

    def __init__(self, nc, stack):
        self.nc = nc
        self.stack = stack
        self.q = {e: [] for e in self.ENGS}
        self.cnt = {e: 0 for e in self.ENGS}
        self.waited = {e: {} for e in self.ENGS}
        self.lastw = {}
        self.readers = {}
        self.sems = {}
        for e in ('pe', 'dve', 'act', 'pool'):
            self.sems[e] = stack.enter_context(nc.semaphore('s_' + e))
        self.slot_cnt = []
        for i in range(N_DMA_SLOTS):
            self.sems['d%d' % i] = stack.enter_context(nc.semaphore('s_d%d' % i))
            self.slot_cnt.append(0)
        self.rr = 0
        self.n_ops = 0
        self.tid = 0
        self.scopes = []

    def push(self):
        st = contextlib.ExitStack()
        st.__enter__()
        self.scopes.append(st)

    def pop(self):
        self.barrier()
        self.scopes.pop().__exit__(None, None, None)

    def _st(self):
        return self.scopes[-1] if self.scopes else self.stack

    def sb(self, name, shape, dt):
        self.tid += 1
        return self._st().enter_context(self.nc.sbuf_tensor('%s_%d' % (name, self.tid), list(shape), dt))

    def ps(self, name, shape, dt):
        return self.stack.enter_context(self.nc.psum_tensor(name, list(shape), dt))

    @staticmethod
    def _res(items):
        out = []
        for it in items:
            if it is None:
                continue
            if isinstance(it, str):
                out.append(it)
            elif isinstance(it, (int, float)):
                continue
            else:
                out.append(it.tensor.name)
        return out

    def _deps(self, e, reads, writes):
        deps = []
        for r in reads:
            ev = self.lastw.get(r)
            if ev is not None:
                deps.append(ev)
        for w in writes:
            ev = self.lastw.get(w)
            if ev is not None:
                deps.append(ev)
            deps.extend(self.readers.get(w, ()))
        need = {}
        for (sk, v) in deps:
            if sk == e and e == 'pe':
                continue
            if self.waited[e].get(sk, 0) >= v:
                continue
            if need.get(sk, 0) < v:
                need[sk] = v
        return need

    def _emit_waits(self, e, need):
        for sk, v in need.items():
            self.waited[e][sk] = v
            self.q[e].append(('w', self.sems[sk], v))

    def _record(self, ev, reads, writes):
        for r in reads:
            self.readers.setdefault(r, []).append(ev)
        for w in writes:
            self.lastw[w] = ev
            self.readers[w] = []

    def op(self, e, fn, reads=(), writes=()):
        reads = self._res(reads)
        writes = self._res(writes)
        need = self._deps(e, reads, writes)
        self._emit_waits(e, need)
        self.cnt[e] += 1
        ev = (e, self.cnt[e])
        self.q[e].append(('i', fn, self.sems[e], 1))
        self._record(ev, reads, writes)
        self.n_ops += 1

    def dma(self, e, out, in_, reads=None, writes=None):
        reads = self._res(reads if reads is not None else [in_])
        writes = self._res(writes if writes is not None else [out])
        need = self._deps(e, reads, writes)
        slot = self.rr
        self.rr = (self.rr + 1) % N_DMA_SLOTS
        sk = 'd%d' % slot
        prev = self.slot_cnt[slot]
        if prev > 0 and self.waited[e].get(sk, 0) < prev and need.get(sk, 0) < prev:
            need[sk] = prev
        self._emit_waits(e, need)
        self.slot_cnt[slot] = prev + 16
        ev = (sk, prev + 16)
        self.q[e].append(('i', lambda eng: eng.dma_start(out=out, in_=in_), self.sems[sk], 16))
        self._record(ev, reads, writes)
        self.n_ops += 1

    def barrier(self):
        evs = {}
        for e in ('pe', 'dve', 'act', 'pool'):
            if self.cnt[e] > 0:
                evs[e] = self.cnt[e]
        for i, c in enumerate(self.slot_cnt):
            if c > 0:
                evs['d%d' % i] = c
        for e in self.ENGS:
            need = {}
            for sk, v in evs.items():
                if sk == e and e == 'pe':
                    continue
                if self.waited[e].get(sk, 0) < v:
                    need[sk] = v
            self._emit_waits(e, need)
        self.lastw = {}
        self.readers = {}

    def emit(self):
        nc = self.nc
        q = self.q
        with nc.Block() as block:
            def run(eng, items):
                for it in items:
                    if it[0] == 'w':
                        eng.wait_ge(it[1], it[2])
                    else:
                        it[1](eng).then_inc(it[2], it[3])

            @block.tensor
            def _(eng):
                run(eng, q['pe'])

            @block.vector
            def _(eng):
                run(eng, q['dve'])

            @block.scalar
            def _(eng):
                run(eng, q['act'])

            @block.gpsimd
            def _(eng):
                run(eng, q['pool'])

            @block.sync
            def _(eng):
                run(eng, q['sp'])

    def mm(self, out, lhsT, rhs, start=True, stop=True):
        self.op('pe', lambda e: e.matmul(out, lhsT=lhsT, rhs=rhs, start=start, stop=stop),
                reads=[lhsT, rhs], writes=[out])

    def tr(self, out, in_, ident):
        self.op('pe', lambda e: e.transpose(out=out, in_=in_, identity=ident),
                reads=[in_, ident], writes=[out])

    def act(self, out, in_, func, bias=None, scale=None, e='act'):
        kw = {}
        if bias is not None:
            kw['bias'] = bias
        if scale is not None:
            kw['scale'] = scale
        self.op('act', lambda eng: eng.activation(out=out, in_=in_, func=func, **kw),
                reads=[in_, bias, scale], writes=[out])

    def tt(self, out, in0, in1, op, e='dve'):
        self.op(e, lambda eng: eng.tensor_tensor(out=out, in0=in0, in1=in1, op=op),
                reads=[in0, in1], writes=[out])

    def ts(self, out, in0, s1, op0, s2=None, op1=None, e='dve', accum=None):
        kw = {}
        if op1 is not None:
            kw['op1'] = op1
        if accum is not None:
            kw['accum_out'] = accum
        self.op(e, lambda eng: eng.tensor_scalar(out=out, in0=in0, scalar1=s1, scalar2=s2, op0=op0, **kw),
                reads=[in0, s1, s2], writes=[out, accum])

    def stt(self, out, in0, scalar, in1, op0, op1):
        self.op('dve', lambda eng: eng.scalar_tensor_tensor(out=out, in0=in0, scalar=scalar, in1=in1, op0=op0, op1=op1),
                reads=[in0, scalar, in1], writes=[out])

    def copy(self, out, in_, e='dve'):
        self.op(e, lambda eng: eng.tensor_copy(out=out, in_=in_), reads=[in_], writes=[out])

    def memset(self, ap, val, e='pool'):
        self.op(e, lambda eng: eng.memset(ap, val), writes=[ap])

    def recip(self, out, in_):
        self.op('dve', lambda eng: eng.reciprocal(out=out, in_=in_), reads=[in_], writes=[out])

    def reduce(self, out, in_, op, axis=AX.X):
        self.op('dve', lambda eng: eng.tensor_reduce(out=out, in_=in_, axis=axis, op=op), reads=[in_], writes=[out])


def bc(ap, shape):
    return ap.to_broadcast(list(shape))


class K:
    pass


def build(n_layers=L, dbg=()):
    nc = bass.Bass("TRN2", target_bir_lowering=False)
    k = K()
    k.nc = nc

    def din(name, shape, dt=F32):
        return nc.dram_tensor(name, list(shape), dt, kind="ExternalInput").ap()

    def dscr(name, shape, dt):
        return nc.dram_tensor(name, list(shape), dt, kind="Internal").ap()

    xT = din('xT', [D, T])
    cT = din('cT', [128, 8, NB])
    ada_w = din('ada_w', [L, D, 6 * D])
    ada_bT = din('ada_bT', [128, L, 48])
    gains = din('gains', [128, 4, L, 8])
    w_in = din('w_in', [L, D, PC])
    muT = din('muT', [128, L, 19])
    w_out = din('w_out', [L, D, D])
    qng = din('qng', [128, L, 2])
    kvg = din('kvg', [128, L])
    w_q = din('w_q', [L, 256, 512])
    w_qi = din('w_qi', [L, 256, 256])
    wkT = din('wkT', [L, 128, 4, 128])
    wvP = din('wvP', [L, 128, 8, 128])
    kiln = din('kiln', [128, L, 2])
    rwp = din('rwp', [128, 7, L, 4])
    v0T = din('v0T', [128, L, 4])
    w2a2 = din('w2a2', [L, 128, 512])
    g2a = din('g2a', [L, 128, 512])
    g2bv2 = din('g2bv2', [L, 64, 512])
    w_fc = din('w_fc', [L, D, 2 * DFF])
    w_down = din('w_down', [L, DFF, D])
    consts = din('consts', [128, 1024])
    outT = nc.dram_tensor('outT', [D, T], F32, kind="ExternalOutput").ap()

    XT = dscr('XT', [D, T], F32)
    COLS = dscr('COLS', [PC, T], F32)
    YMIX = dscr('YMIX', [D, T], BF16)
    VF = dscr('VF', [512, T], F32)
    GS = dscr('GS', [512, T], F32)
    BON = dscr('BON', [512, T], F32)
    H2 = dscr('H2', [D, T], BF16)
    UT = dscr('UT', [DFF, T], BF16)
    dbg_out = {}
    for nm, shp in dbg:
        dbg_out[nm] = nc.dram_tensor('dbg_' + nm, list(shp), F32, kind="ExternalOutput").ap()

    with contextlib.ExitStack() as st:
        P = Prog(nc, st)
        k.P = P
        psA = P.ps('psA', [128, 1024], F32)
        psB = P.ps('psB', [128, 1024], F32)
        psC = P.ps('psC', [128, 512], F32)
        psD = P.ps('psD', [128, 512], F32)
        psE = P.ps('psE', [128, 512], F32)
        psF = P.ps('psF', [128, 512], F32)
        cst = P.sb('cst', [128, 1024], F32)
        P.dma('sp', cst[:], consts[:, :])
        idb = P.sb('idb', [128, 128], BF16)
        idf = P.sb('idf', [128, 128], F32)
        onesb = P.sb('onesb', [128, 128], BF16)
        blkb = P.sb('blkb', [128, 128], BF16)
        P.copy(idb[:], cst[:, 0:128])
        P.copy(idf[:], cst[:, 0:128])
        P.memset(onesb[:], 1.0)
        P.copy(blkb[:], cst[:, 128:256])
        mk1 = P.sb('mk1', [64, 128], F32)
        mk3 = P.sb('mk3', [64, 64], F32)
        id64b = P.sb('id64b', [64, 64], BF16)
        rmask = P.sb('rmask', [128, 512], F32)
        P.copy(mk1[:], cst[0:64, 256:384])
        P.copy(mk3[:], cst[0:64, 384:448])
        P.copy(id64b[:], cst[0:64, 0:64])
        P.memset(rmask[:], 1.0)
        P.memset(rmask[:].rearrange("p (c t) -> p c t", t=64)[:, :, 0:1], 0.0)
        pow2 = cst[:, 448:480]
        gn = P.sb('gn', [128, 4, L, 8], F32)
        P.dma('sp', gn[:], gains[:, :, :, :])
        abT = P.sb('abT', [128, L, 48], F32)
        P.dma('sp', abT[:], ada_bT[:, :, :])
        mu = P.sb('mu', [128, L, 19], F32)
        P.dma('sp', mu[:], muT[:, :, :])
        qg = P.sb('qg', [128, L, 2], F32)
        P.dma('sp', qg[:], qng[:, :, :])
        kg = P.sb('kg', [128, L], F32)
        P.dma('sp', kg[:], kvg[:, :])
        kl = P.sb('kl', [128, L, 2], F32)
        P.dma('sp', kl[:], kiln[:, :, :])
        rp = P.sb('rp', [128, 7, L, 4], F32)
        P.dma('sp', rp[:], rwp[:, :, :, :])
        v0s = P.sb('v0s', [128, L, 4], F32)
        P.dma('sp', v0s[:], v0T[:, :, :])
        mod = P.sb('mod', [128, L, 48, NB], F32)
        A1 = P.sb('A1', [128, L, 8, NB], F32)
        A2 = P.sb('A2', [128, L, 8, NB], F32)
        G1 = P.sb('G1', [128, L, 8, NB], F32)
        G2 = P.sb('G2', [128, L, 8, NB], F32)

        PSB = [psC, psD, psE, psF]

        P.push()
        ct = P.sb('ct', [128, 8, NB], F32)
        cond = P.sb('cond', [128, 8, NB], F32)
        P.dma('sp', ct[:], cT[:, :, :])
        P.act(cond[:], ct[:], AF.Silu)
        wa = [P.sb('wa%d' % i, [128, 8, 768], F32) for i in range(2)]
        it = 0
        for l in range(n_layers):
            for grp in range(8):
                w = wa[it % 2]
                ps = PSB[it % 2]
                it += 1
                for kk in range(8):
                    P.dma('sp', w[:, kk, :], ada_w[l, kk * 128:(kk + 1) * 128, grp * 768:(grp + 1) * 768])
                for mi in range(6):
                    for kk in range(8):
                        P.mm(ps[:, mi * 2:(mi + 1) * 2], lhsT=w[:, kk, mi * 128:(mi + 1) * 128], rhs=cond[:, kk, :],
                             start=(kk == 0), stop=(kk == 7))
                P.tt(mod[:, l, grp * 6:(grp + 1) * 6, :], ps[:, 0:12].rearrange("p (m b) -> p m b", b=NB),
                     bc(abT[:, l, grp * 6:(grp + 1) * 6].unsqueeze(2), [128, 6, NB]), ALU.add)
            for (dst, gi, sc0) in ((A1, 0, 8), (A2, 2, 32)):
                P.stt(dst[:, l, :, :], mod[:, l, sc0:sc0 + 8, :], 1.0,
                      bc(gn[:, gi, l, :].unsqueeze(2), [128, 8, NB]), ALU.add, ALU.mult)
            for (dst, gi, g0) in ((G1, 1, 16), (G2, 3, 40)):
                P.tt(dst[:, l, :, :], mod[:, l, g0:g0 + 8, :], bc(gn[:, gi, l, :].unsqueeze(2), [128, 8, NB]), ALU.mult)
        P.pop()

        P.push()
        xc = [P.sb('xc%d' % i, [128, 2048], F32) for i in range(2)]
        it = 0
        for kk in range(8):
            for hf in range(2):
                t_ = xc[it % 2]
                it += 1
                P.dma('sp', t_[:], xT[kk * 128:(kk + 1) * 128, hf * 2048:(hf + 1) * 2048])
                P.dma('sp', XT[kk * 128:(kk + 1) * 128, hf * 2048:(hf + 1) * 2048], t_[:])
        P.pop()

        def rstd_of(x, K_, N, inv_dim, eps, sq, ps, rs, ones=None, np_=128):
            P.act(sq[0:np_, 0:K_, 0:N], x, AF.Square)
            for kk in range(K_):
                P.mm(ps[:, 0:N], lhsT=(ones if ones is not None else onesb[0:np_, :]), rhs=sq[0:np_, kk, 0:N],
                     start=(kk == 0), stop=(kk == K_ - 1))
            P.act(rs[:, 0:N], ps[:, 0:N], AF.Sqrt, bias=eps_ap(eps), scale=inv_dim)
            P.recip(rs[:, 0:N], rs[:, 0:N])

        epst = P.sb('epst', [128, 4], F32)
        P.memset(epst[:, 0:1], 1e-6)
        P.memset(epst[:, 1:2], 64e-5)
        P.memset(epst[:, 2:3], 0.0)

        def eps_ap(eps):
            if eps == 1e-6:
                return epst[:, 0:1]
            if eps == 64e-5:
                return epst[:, 1:2]
            return epst[:, 2:3]

        stg = [P.sb('stg%d' % i, [128, 1024], F32) for i in range(2)]
        stg_i = [0]

        def ldw(dst, src):
            if len(dst.shape) == 3:
                dst = dst.rearrange("p a b -> p (a b)")
            if len(src.shape) == 3:
                src = src.rearrange("p a b -> p (a b)")
            rows, cols = dst.shape[0], dst.shape[1]
            for c0 in range(0, cols, 1024):
                cw = min(1024, cols - c0)
                t_ = stg[stg_i[0] % 2]
                stg_i[0] += 1
                P.dma('sp', t_[0:rows, 0:cw], src[:, c0:c0 + cw])
                P.copy(dst[:, c0:c0 + cw], t_[0:rows, 0:cw], e='pool')

        k.__dict__.update(locals())
        for l in range(n_layers):
            stage_proj(k, l)
            if 'cols' in dbg_out and l == 0:
                dump(k, COLS, dbg_out['cols'], PC)
            for s in range(NB):
                stage_rwkv(k, l, s)
                stage_dsa(k, l, s)
            if 'ymix' in dbg_out and l == 0:
                dump(k, YMIX, dbg_out['ymix'], D, bf=True)
            stage_out_ffn(k, l)
            if 'xl0' in dbg_out and l == 0:
                dump(k, XT, dbg_out['xl0'], D)
        P.push()
        xc = [P.sb('xo%d' % i, [128, 2048], F32) for i in range(2)]
        it = 0
        for kk in range(8):
            for hf in range(2):
                t_ = xc[it % 2]
                it += 1
                P.dma('sp', t_[:], XT[kk * 128:(kk + 1) * 128, hf * 2048:(hf + 1) * 2048])
                P.dma('sp', outT[kk * 128:(kk + 1) * 128, hf * 2048:(hf + 1) * 2048], t_[:])
        P.pop()
        P.emit()
    return nc


def dump(k, src, dst, rows, bf=False):
    P = k.P
    P.push()
    nchunk = (rows + 127) // 128
    bufs = [P.sb('dmp%d' % i, [128, 2048], BF16 if bf else F32) for i in range(2)]
    bufs2 = [P.sb('dmq%d' % i, [128, 2048], F32) for i in range(2)] if bf else None
    it = 0
    for m in range(nchunk):
        r = min(128, rows - m * 128)
        for hf in range(T // 2048):
            b = bufs[it % 2]
            P.dma('sp', b[0:r, :], src[m * 128:m * 128 + r, hf * 2048:(hf + 1) * 2048])
            if bf:
                b2 = bufs2[it % 2]
                P.copy(b2[0:r, :], b[0:r, :])
                b = b2
            P.dma('sp', dst[m * 128:m * 128 + r, hf * 2048:(hf + 1) * 2048], b[0:r, :])
            it += 1
    P.pop()


def stage_proj(k, l):
    P = k.P
    P.push()
    win = P.sb('win', [128, 8, PC], BF16)
    for kk in range(8):
        k.ldw(win[:, kk, :], k.w_in[l, kk * 128:(kk + 1) * 128, :])
    xt = P.sb('xt', [128, 8, 512], F32)
    sq = P.sb('sq', [128, 8, 512], BF16)
    rs = P.sb('rs', [128, 512], F32)
    hb = P.sb('hb', [128, 8, 512], BF16)
    co = [P.sb('co%d' % i, [128, 512], F32) for i in range(4)]
    it = 0
    for n in range(T // 512):
        b = n // 4
        tok = slice(n * 512, (n + 1) * 512)
        P.dma('sp', xt[:], k.XT[:, tok].rearrange("(k p) n -> p k n", p=128))
        k.rstd_of(xt[:], 8, 512, 1.0 / D, 1e-6, sq, k.psC, rs)
        P.tt(xt[:], xt[:], bc(rs[:].unsqueeze(1), [128, 8, 512]), ALU.mult)
        for kk in range(8):
            P.act(hb[:, kk, :], xt[:, kk, :], AF.Identity, bias=k.mod[:, l, kk, b:b + 1], scale=k.A1[:, l, kk, b:b + 1])
        for m in range(19):
            ps = k.PSB[1 + (it % 3)]
            c = co[it % 4]
            for kk in range(8):
                P.mm(ps[:, :], lhsT=win[:, kk, m * 128:(m + 1) * 128], rhs=hb[:, kk, :], start=(kk == 0), stop=(kk == 7))
            if it % 2 == 0:
                P.act(c[:], ps[:, :], AF.Copy)
            else:
                P.copy(c[:], ps[:, :])
            P.dma('sp', k.COLS[m * 128:(m + 1) * 128, tok], c[:], writes=['COLS:%d:%d' % (m, n)])
            it += 1
    P.pop()


def cols_res(m, s):
    return ['COLS:%d:%d' % (m, n) for n in range(s * 4, s * 4 + 4)]


EXPH = 0.6065306597126334


SKIP = set()


def stage_rwkv(k, l, s):
    if 'rwkv' in SKIP:
        return
    P = k.P
    rp, mu = k.rp, k.mu
    psA, psB, psC, psD, psE, psF = k.psA, k.psB, k.psC, k.psD, k.psE, k.psF
    t0 = s * S
    P.push()
    w2a2b = P.sb('w2a2b', [128, 512], BF16)
    g2ab = P.sb('g2ab', [128, 512], BF16)
    g2bv = P.sb('g2bv', [64, 512], BF16)
    k.ldw(w2a2b[:], k.w2a2[l, :, :])
    k.ldw(g2ab[:], k.g2a[l, :, :])
    k.ldw(g2bv[:], k.g2bv2[l, :, :])
    KR = P.sb('KR', [128, 4, 32, 2, 64], BF16)
    BT = P.sb('BT', [128, 4, 2048], BF16)
    KT = P.sb('KT', [128, 4, 2048], BF16)
    VB = P.sb('VB', [128, 4, 2048], BF16)
    WC = P.sb('WC', [128, 4, 32], F32)
    P.push()
    ush = P.sb('ush', [128, 2049], F32)
    shd = P.sb('shd', [128, 2048], F32)
    lt = P.sb('lt', [128, 2048], F32)
    lwb = P.sb('lwb', [128, 2048], BF16)
    sg1 = P.sb('sg1', [128, 2048], BF16)
    lg2 = P.sb('lg2', [64, 2048], BF16)
    P.memset(ush[:, 0:1], 0.0)

    def shift_load(m, dst, rows=128):
        P.dma('sp', ush[0:rows, 1:2049], k.COLS[m * 128:m * 128 + rows, t0:t0 + S], reads=cols_res(m, s))
        P.tt(shd[0:rows, :], ush[0:rows, 0:2048], ush[0:rows, 1:2049], ALU.subtract)
        P.stt(dst, shd[0:rows, :], mu[0:rows, l, m:m + 1], ush[0:rows, 1:2049], ALU.mult, ALU.add)

    shift_load(16, lt[:, :])
    P.act(lwb[0:64, :], lt[0:64, :], AF.Tanh)
    P.copy(lwb[64:128, :], lt[64:128, :])
    shift_load(17, lt[:, :])
    P.act(sg1[:, :], lt[:, :], AF.Sigmoid)
    shift_load(18, lt[0:64, :], rows=64)
    P.act(lg2[0:32, :], lt[0:32, :], AF.Sigmoid)
    P.copy(lg2[32:64, :], lt[32:64, :])
    rj = P.sb('rj', [128, 2048], F32)
    kj = P.sb('kj', [128, 2048], F32)
    vj = P.sb('vj', [128, 2048], F32)
    names = ['sig', 'aa', 'gg', 'vm', 'vf', 'kk', 'rn', 't1', 'kp', 'be', 'cs', 'Wt', 'Wi', 'csm', 'Wp', 'bon']
    W_ = {nm: P.sb(nm, [128, 512], F32) for nm in names}
    kq = P.sb('kq', [128, 512], BF16)
    rk = P.sb('rk', [128, 512], BF16)
    c3 = lambda ap: ap.rearrange("p (c t) -> p c t", t=64)
    for j in range(4):
        jc = slice(j * 128, (j + 1) * 128)
        shift_load(4 + j, rj[:, :])
        shift_load(8 + j, kj[:, :])
        shift_load(12 + j, vj[:, :])
        for n in range(4):
            tk = slice(n * 512, (n + 1) * 512)
            gt = slice(t0 + n * 512, t0 + (n + 1) * 512)
            sig, aa, gg, vm, vf, kk, rn, t1, kp, be, cs, Wt, Wi, csm, Wp, bon = [W_[nm] for nm in names]
            P.mm(psD[:, :], lhsT=w2a2b[0:64, jc], rhs=lwb[0:64, tk])
            P.act(sig[:], psD[:, :], AF.Sigmoid, bias=rp[:, 0, l, j:j + 1])
            P.mm(psE[:, :], lhsT=w2a2b[64:128, jc], rhs=lwb[64:128, tk])
            P.act(aa[:], psE[:, :], AF.Sigmoid, bias=rp[:, 1, l, j:j + 1])
            P.mm(psF[:, :], lhsT=g2ab[:, jc], rhs=sg1[:, tk], start=True, stop=False)
            P.mm(psF[:, :], lhsT=g2bv[0:32, jc], rhs=lg2[0:32, tk], start=False, stop=True)
            P.copy(gg[:], psF[:, :])
            P.dma('sp', k.GS[jc, gt], gg[:])
            if l > 0:
                P.mm(psC[:, :], lhsT=g2bv[32:64, jc], rhs=lg2[32:64, tk])
                P.act(vm[:], psC[:, :], AF.Sigmoid, bias=k.v0s[:, l, j:j + 1])
                P.dma('sp', vf[:], k.VF[jc, gt])
                P.tt(vf[:], vf[:], vj[:, tk], ALU.subtract)
                P.tt(vf[:], vf[:], vm[:], ALU.mult)
                P.tt(vj[:, tk], vj[:, tk], vf[:], ALU.add)
            else:
                P.dma('sp', k.VF[jc, gt], vj[:, tk])
            P.copy(VB[:, j, tk], vj[:, tk], e='pool')
            P.ts(kk[:], kj[:, tk], rp[:, 2, l, j:j + 1], ALU.mult)
            P.act(kq[:], kk[:], AF.Square)
            P.mm(psD[:, :], lhsT=k.blkb[:, :], rhs=kq[:])
            P.act(rn[:], psD[:, :], AF.Sqrt)
            P.ts(rn[:], rn[:], 1e-12, ALU.max)
            P.recip(rn[:], rn[:])
            P.tt(kk[:], kk[:], rn[:], ALU.mult)
            P.ts(t1[:], aa[:], -1.0, ALU.add, rp[:, 3, l, j:j + 1], ALU.mult)
            P.stt(kp[:], t1[:], 1.0, kj[:, tk], ALU.add, ALU.mult)
            P.tt(be[:], kk[:], aa[:], ALU.mult, e='pool')
            P.op('dve', lambda eng, cs=cs, sig=sig: eng.tensor_tensor_scan(out=cs[:], data0=k.rmask[:], data1=sig[:], initial=0.0, op0=ALU.mult, op1=ALU.add),
                 reads=[k.rmask[:], sig[:]], writes=[cs[:]])
            P.act(Wt[:], cs[:], AF.Exp, scale=-EXPH)
            P.act(Wi[:], cs[:], AF.Exp, scale=EXPH)
            P.tt(csm[:], cs[:], sig[:], ALU.subtract, e='pool')
            P.act(Wp[:], csm[:], AF.Exp, scale=-EXPH)
            P.copy(WC[:, j, n * 8:(n + 1) * 8], c3(Wt[:])[:, :, 63])
            P.tt(KR[:, j, n * 8:(n + 1) * 8, 0, :], c3(kk[:]), c3(Wp[:]), ALU.mult)
            P.tt(KR[:, j, n * 8:(n + 1) * 8, 1, :], c3(rj[:, tk]), c3(Wt[:]), ALU.mult)
            P.tt(BT[:, j, tk], be[:], Wi[:], ALU.mult, e='pool')
            P.tt(KT[:, j, tk], kp[:], Wi[:], ALU.mult)
            P.stt(rk[:], rj[:, tk], rp[:, 6, l, j:j + 1], kp[:], ALU.mult, ALU.mult)
            P.mm(psE[:, :], lhsT=k.blkb[:, :], rhs=rk[:])
            P.tt(bon[:], psE[:, :], vj[:, tk], ALU.mult)
            P.dma('sp', k.BON[jc, gt], bon[:])
    P.pop()
    P.push()
    Mf = P.sb('Mf', [128, 4, 128], F32)
    Mb = P.sb('Mb', [128, 4, 128], BF16)
    Mt = P.sb('Mt', [128, 4, 128], F32)
    blk32 = P.sb('blk32', [128, 128], F32)
    P.copy(blk32[:], k.cst[:, 128:256])
    P.memset(Mf[:], 0.0)
    P.memset(Mb[:], 0.0)
    tok3 = P.sb('tok3', [64, 3, 512], BF16)
    m1 = P.sb('m1', [64, 8, 128], BF16)
    m2 = P.sb('m2', [64, 8, 128], BF16)
    am = P.sb('am', [64, 8, 64], BF16)
    xs = [P.sb('xs%d' % i, [64, 8, 64], BF16) for i in range(2)]
    zs = [P.sb('zs%d' % i, [64, 8, 64], BF16) for i in range(2)]
    pps = [P.sb('pp%d' % i, [64, 8, 64], BF16) for i in range(2)]
    nr = P.sb('nr', [64, 8, 64], BF16)
    ub = P.sb('ub', [64, 8, 64], BF16)
    yt = P.sb('yt', [64, 8, 64], F32)
    ysq = P.sb('ysq', [64, 8, 64], F32)
    st_ = {nm: P.sb(nm, [64, 8], F32) for nm in ('s1', 's2', 'mean', 'msq', 'var', 'rstd')}
    yfm = P.sb('yfm', [128, 4, 512], F32)
    ye = P.sb('ye', [128, 512], F32)
    bo2 = P.sb('bo2', [128, 512], F32)
    gg2 = P.sb('gg2', [128, 512], F32)
    yo16 = P.sb('yo16', [128, 512], BF16)
    pAb = psA[:, :].bitcast(BF16)
    h8 = lambda ap, w: ap.rearrange("p (h c) -> p h c", c=w)
    for c in range(0 if 'rec' in SKIP else 32):
        cs_ = slice(c * 64, (c + 1) * 64)
        for i, src in enumerate((VB, BT, KT)):
            for j in range(4):
                P.tr(pAb[0:64, i * 512 + j * 128:i * 512 + (j + 1) * 128], src[:, j, cs_], k.idb[:, :])
        P.act(tok3[:].rearrange("p a b -> p (a b)"), pAb[0:64, 0:1536], AF.Copy)
        Vt = tok3[:, 0, :]
        Bt = tok3[:, 1, :]
        Kt = tok3[:, 2, :]
        hp = lambda h: slice((h % 2) * 64, (h % 2) * 64 + 64)
        pos = lambda h: (h % 2) * 4 + h // 2
        par = lambda t_, q: t_[:].rearrange("p (j two) c -> p j two c", two=2)[:, :, q, :]
        for h in range(8):
            P.mm(psB[0:64, pos(h) * 128:(pos(h) + 1) * 128], lhsT=BT[hp(h), h // 2, cs_],
                 rhs=KR[hp(h), h // 2, c, :, :].rearrange("p a b -> p (a b)"))
        for q in range(2):
            P.tt(par(m1, q), h8(psB[0:64, q * 512:(q + 1) * 512], 128), bc(k.mk1[:].unsqueeze(1), [64, 4, 128]), ALU.mult)
        for h in range(8):
            P.mm(psA[0:64, pos(h) * 128:(pos(h) + 1) * 128], lhsT=KT[hp(h), h // 2, cs_],
                 rhs=KR[hp(h), h // 2, c, :, :].rearrange("p a b -> p (a b)"))
        for q in range(2):
            P.tt(par(m2, q), h8(psA[0:64, q * 512:(q + 1) * 512], 128), bc(k.mk1[:].unsqueeze(1), [64, 4, 128]), ALU.mult)
        for h in range(8):
            pq = psC if h % 2 == 0 else psD
            P.mm(pq[0:64, (h // 2) * 64:(h // 2 + 1) * 64], lhsT=KR[hp(h), h // 2, c, 0, :], rhs=BT[hp(h), h // 2, cs_])
        for q in range(2):
            pq = psC if q == 0 else psD
            P.tt(par(am, q), h8(pq[0:64, 0:256], 64), bc(k.mk3[:].unsqueeze(1), [64, 4, 64]), ALU.mult)
        P.tt(pps[0][:], bc(k.id64b[:].unsqueeze(1), [64, 8, 64]), m1[:, :, 0:64], ALU.subtract)
        X = m1[:, :, 0:64]
        Z = am[:]
        Pc = pps[0]
        for lev in range(1, 6):
            Xn = xs[lev % 2]
            Zn = zs[lev % 2]
            Pn = pps[lev % 2]
            if lev < 5:
                for h in range(8):
                    P.mm(psD[0:64, h * 64:(h + 1) * 64], lhsT=Z[:, h, :], rhs=X[:, h, :])
            for h in range(8):
                P.mm(psE[0:64, h * 64:(h + 1) * 64], lhsT=X[:, h, :], rhs=Z[:, h, :])
            if lev < 5:
                P.act(Xn[:], h8(psD[0:64, :], 64), AF.Copy)
            P.copy(Zn[:], h8(psE[0:64, :], 64))
            for h in range(8):
                P.mm(psF[0:64, h * 64:(h + 1) * 64], lhsT=Zn[:, h, :], rhs=Pc[:, h, :])
            P.tt(Pn[:], h8(psF[0:64, :], 64), Pc[:], ALU.add)
            X, Z, Pc = Xn[:], Zn[:], Pn
        TT = Pc
        if 'ph2' in SKIP:
            continue
        for j in range(4):
            jc = slice(j * 128, (j + 1) * 128)
            P.mm(psD[0:64, jc], lhsT=KR[:, j, c, 0, :], rhs=Mb[:, j, :], start=True, stop=False)
            for h in (2 * j, 2 * j + 1):
                P.mm(psD[0:64, h * 64:(h + 1) * 64], lhsT=m2[:, h, 0:64], rhs=Vt[:, h * 64:(h + 1) * 64],
                     start=False, stop=(h == 2 * j + 1))
        P.act(nr[:], h8(psD[0:64, :], 64), AF.Copy, scale=-1.0)
        for h in range(8):
            P.mm(psE[0:64, h * 64:(h + 1) * 64], lhsT=TT[:, h, :], rhs=nr[:, h, :])
        P.copy(ub[:], h8(psE[0:64, :], 64))
        ubf = ub[:].rearrange("p h c -> p (h c)")
        for j in range(4):
            jc = slice(j * 128, (j + 1) * 128)
            P.mm(psF[0:64, jc], lhsT=KR[:, j, c, 1, :], rhs=Mb[:, j, :], start=True, stop=False)
            for h in (2 * j, 2 * j + 1):
                o = psF[0:64, h * 64:(h + 1) * 64]
                P.mm(o, lhsT=m1[:, h, 64:128], rhs=ub[:, h, :], start=False, stop=False)
                P.mm(o, lhsT=m2[:, h, 64:128], rhs=Vt[:, h * 64:(h + 1) * 64], start=False, stop=(h == 2 * j + 1))
        P.act(yt[:], h8(psF[0:64, :], 64), AF.Copy)
        for j in range(4):
            jc = slice(j * 128, (j + 1) * 128)
            P.mm(psC[:, jc], lhsT=Bt[:, jc], rhs=ubf[:, jc], start=True, stop=False)
            P.mm(psC[:, jc], lhsT=Kt[:, jc], rhs=Vt[:, jc], start=False, stop=True)
        P.tt(Mt[:], h8(psC[:, :], 128), bc(blk32[:].unsqueeze(1), [128, 4, 128]), ALU.mult)
        P.tt(Mf[:], Mt[:], Mf[:], ALU.add)
        P.tt(Mf[:], Mf[:], bc(WC[:, :, c:c + 1], [128, 4, 128]), ALU.mult)
        P.copy(Mb[:], Mf[:], e='pool')
        s1, s2, mean, msq, var, rstd = [st_[nm] for nm in ('s1', 's2', 'mean', 'msq', 'var', 'rstd')]
        P.reduce(s1[:], yt[:], ALU.add)
        P.act(ysq[:], yt[:], AF.Square)
        P.reduce(s2[:], ysq[:], ALU.add)
        P.ts(mean[:], s1[:], 1.0 / 64, ALU.mult)
        P.tt(msq[:], mean[:], mean[:], ALU.mult)
        P.stt(var[:], s2[:], 1.0 / 64, msq[:], ALU.mult, ALU.subtract)
        P.act(rstd[:], var[:], AF.Sqrt, bias=k.epst[0:64, 1:2])
        P.recip(rstd[:], rstd[:])
        P.tt(yt[:], yt[:], bc(mean[:].unsqueeze(2), [64, 8, 64]), ALU.subtract)
        P.tt(yt[:], yt[:], bc(rstd[:].unsqueeze(2), [64, 8, 64]), ALU.mult)
        ytf = yt[:].rearrange("p h c -> p (h c)")
        for j in range(4):
            P.tr(psB[:, j * 64:(j + 1) * 64], ytf[:, j * 128:(j + 1) * 128], k.idf[0:64, 0:64])
        P.copy(yfm[:, :, (c % 8) * 64:(c % 8) * 64 + 64], h8(psB[:, 0:256], 64))
        if c % 8 == 7:
            n = c // 8
            gt = slice(t0 + n * 512, t0 + (n + 1) * 512)
            for j in range(4):
                jc = slice(j * 128, (j + 1) * 128)
                P.ts(ye[:], yfm[:, j, :], rp[:, 4, l, j:j + 1], ALU.mult, rp[:, 5, l, j:j + 1], ALU.add)
                P.dma('sp', bo2[:], k.BON[jc, gt])
                P.dma('sp', gg2[:], k.GS[jc, gt])
                P.tt(ye[:], ye[:], bo2[:], ALU.add)
                P.tt(yo16[:], ye[:], gg2[:], ALU.mult)
                P.dma('sp', k.YMIX[512 + j * 128:512 + (j + 1) * 128, gt], yo16[:])
    P.pop()
    P.pop()


NIT = 16


def stage_dsa(k, l, s):
    if 'dsa' in SKIP:
        return
    P = k.P
    psA, psB, psC, psD, psE, psF = k.psA, k.psB, k.psC, k.psD, k.psE, k.psF
    t0 = s * S
    P.push()
    wq = P.sb('wq', [128, 2, 512], BF16)
    wqi = P.sb('wqi', [128, 2, 256], BF16)
    wk = P.sb('wk', [128, 4, 128], BF16)
    wv = P.sb('wv', [128, 8, 128], BF16)
    for kk in range(2):
        k.ldw(wq[:, kk, :], k.w_q[l, kk * 128:(kk + 1) * 128, :])
        k.ldw(wqi[:, kk, :], k.w_qi[l, kk * 128:(kk + 1) * 128, :])
    k.ldw(wk[:], k.wkT[l, :, :, :])
    k.ldw(wv[:], k.wvP[l, :, :, :])
    blkf = P.sb('blkf', [128, 128], F32)
    P.copy(blkf[:], k.cst[:, 128:256])
    cqb = P.sb('cqb', [128, 2, 2048], BF16)
    ckvT = P.sb('ckvT', [128, 2048], BF16)
    ckvTok = P.sb('ckvTok', [128, 16, 128], BF16)
    kix = P.sb('kix', [128, 2048], BF16)
    qab = P.sb('qab', [128, 8, 2048], BF16)
    qib = P.sb('qib', [128, 2, 2048], BF16)
    wht = P.sb('wht', [128, 16, 4], F32)
    pAb = psA[:, :].bitcast(BF16)
    P.push()
    cq32 = P.sb('cq32', [128, 2, 512], F32)
    sq = P.sb('sq', [128, 2, 512], BF16)
    rs = P.sb('rs', [128, 512], F32)
    kv32 = P.sb('kv32', [128, 512], F32)
    ki32 = P.sb('ki32', [128, 512], F32)
    sqf = P.sb('sqf', [128, 512], F32)
    mean = P.sb('mean', [128, 512], F32)
    msq = P.sb('msq', [128, 512], F32)
    var = P.sb('var', [128, 512], F32)
    wi = P.sb('wi', [4, 512], F32)
    qb = P.sb('qb', [128, 4, 512], BF16)
    for n in range(4):
        tk = slice(n * 512, (n + 1) * 512)
        gt = slice(t0 + n * 512, t0 + (n + 1) * 512)
        nn = s * 4 + n
        for kk in range(2):
            P.dma('sp', cq32[:, kk, :], k.COLS[kk * 128:(kk + 1) * 128, gt], reads=['COLS:%d:%d' % (kk, nn)])
        k.rstd_of(cq32[:], 2, 512, 1.0 / 256, 1e-6, sq, psC, rs)
        P.tt(cq32[:], cq32[:], bc(rs[:].unsqueeze(1), [128, 2, 512]), ALU.mult)
        for kk in range(2):
            P.act(cqb[:, kk, tk], cq32[:, kk, :], AF.Copy, scale=k.qg[:, l, kk:kk + 1])
        P.dma('sp', kv32[:], k.COLS[256:384, gt], reads=['COLS:2:%d' % nn])
        k.rstd_of(kv32[:].unsqueeze(1), 1, 512, 1.0 / 128, 1e-6, sq, psC, rs)
        P.tt(kv32[:], kv32[:], rs[:], ALU.mult)
        P.act(ckvT[:, tk], kv32[:], AF.Copy, scale=k.kg[:, l:l + 1])
        for i in range(4):
            P.tr(pAb[:, i * 128:(i + 1) * 128], ckvT[:, n * 512 + i * 128:n * 512 + (i + 1) * 128], k.idb[:, :])
        P.copy(ckvTok[:, n * 4:(n + 1) * 4, :], pAb[:, 0:512].rearrange("p (a b) -> p a b", b=128))
        P.dma('sp', ki32[0:64, :], k.COLS[384:448, gt], reads=['COLS:3:%d' % nn])
        P.dma('sp', ki32[64:128, :], k.COLS[384:448, gt], reads=['COLS:3:%d' % nn])
        P.mm(psD[:, :], lhsT=blkf[:, :], rhs=ki32[:, :])
        P.act(sqf[:], ki32[:], AF.Square)
        P.mm(psE[:, :], lhsT=blkf[:, :], rhs=sqf[:, :])
        P.ts(mean[:], psD[:, :], 1.0 / 64, ALU.mult)
        P.tt(msq[:], mean[:], mean[:], ALU.mult)
        P.stt(var[:], psE[:, :], 1.0 / 64, msq[:], ALU.mult, ALU.subtract)
        P.act(var[:], var[:], AF.Sqrt, bias=k.epst[:, 0:1])
        P.recip(var[:], var[:])
        P.tt(ki32[:], ki32[:], mean[:], ALU.subtract)
        P.tt(ki32[:], ki32[:], var[:], ALU.mult)
        P.act(kix[:, tk], ki32[:], AF.Identity, bias=k.kl[:, l, 1:2], scale=k.kl[:, l, 0:1])
        P.dma('sp', wi[:, :], k.COLS[448:452, gt], reads=['COLS:3:%d' % nn])
        for i in range(4):
            P.tr(psD[:, i * 4:(i + 1) * 4], wi[0:4, i * 128:(i + 1) * 128], k.idf[0:4, 0:4])
        P.ts(wht[:, n * 4:(n + 1) * 4, :], psD[:, 0:16].rearrange("p (a b) -> p a b", b=4), 0.0625, ALU.mult)
        for m in range(4):
            for kk in range(2):
                P.mm(psE[:, :], lhsT=wq[:, kk, m * 128:(m + 1) * 128], rhs=cqb[:, kk, tk], start=(kk == 0), stop=(kk == 1))
            P.copy(qb[:, m, :], psE[:, :])
        for h in range(8):
            hp = slice((h % 2) * 64, (h % 2) * 64 + 64)
            P.mm(psF[:, :], lhsT=wk[hp, h // 2, :], rhs=qb[hp, h // 2, :])
            P.act(qab[:, h, tk], psF[:, :], AF.Copy, scale=0.125)
        for m in range(2):
            for kk in range(2):
                P.mm(psE[:, :], lhsT=wqi[:, kk, m * 128:(m + 1) * 128], rhs=cqb[:, kk, tk], start=(kk == 0), stop=(kk == 1))
            P.copy(qib[:, m, tk], psE[:, :])
    P.pop()
    P.push()
    sc = P.sb('sc', [128, 2048], F32)
    tmp = P.sb('tmp', [128, 2048], F32)
    junk = P.sb('junk', [128, 2048], BF16)
    maskb = P.sb('maskb', [128, 2048], BF16)
    mT = P.sb('mT', [128, 2048], BF16)
    E = P.sb('E', [128, 1024], BF16)
    Pm = P.sb('Pm', [128, 1024], BF16)
    rd = P.sb('rd', [128, 1024], F32)
    olat = P.sb('olat', [128, 1024], BF16)
    yd = P.sb('yd', [128, 4, 128], BF16)
    sm = {nm: P.sb(nm, [128, 1], F32) for nm in ('hi', 'lo', 'w0', 'mid', 'cnt', 'gh')}
    Hs = P.sb('Hs', [128, 32], F32)
    for qt in range(16):
        q_ = slice(qt * 128, (qt + 1) * 128)
        N = (qt + 1) * 128
        for hi in range(4):
            hp = slice((hi % 2) * 64, (hi % 2) * 64 + 64)
            for half, pst in enumerate((psA, psB)):
                cols_h = min(1024, N - half * 1024)
                if cols_h <= 0:
                    continue
                for bnk in range((cols_h + 511) // 512):
                    cw = min(512, cols_h - bnk * 512)
                    k0 = half * 1024 + bnk * 512
                    P.mm(pst[:, bnk * 512:bnk * 512 + cw], lhsT=qib[hp, hi // 2, q_], rhs=kix[hp, k0:k0 + cw])
                dst = sc if hi == 0 else tmp
                seg = slice(half * 1024, half * 1024 + cols_h)
                P.ts(dst[:, seg], pst[:, 0:cols_h], 0.0, ALU.max, wht[:, qt, hi:hi + 1], ALU.mult)
                if hi > 0:
                    P.tt(sc[:, seg], sc[:, seg], tmp[:, seg], ALU.add, e='pool')
        P.memset(sc[0:64, qt * 128 + 64:(qt + 1) * 128], NEG)
        lo = sm['lo']
        if qt >= 2:
            P.reduce(sm['hi'][:], sc[:, 0:N], ALU.max)
            P.reduce(lo[:], sc[:, 0:qt * 128 + 64], ALU.min)
            P.tt(sm['w0'][:], sm['hi'][:], lo[:], ALU.subtract)
            P.ts(Hs[:, 0:NIT], k.cst[:, 448:448 + NIT], sm['w0'][:, 0:1], ALU.mult)
            for i in range(NIT):
                P.tt(sm['mid'][:], lo[:], Hs[:, i:i + 1], ALU.add)
                P.ts(junk[:, 0:N], sc[:, 0:N], sm['mid'][:, 0:1], ALU.is_gt, None, ALU.add, accum=sm['cnt'][:])
                P.ts(sm['gh'][:], sm['cnt'][:], 255.5, ALU.is_gt, Hs[:, i:i + 1], ALU.mult)
                P.tt(lo[:], lo[:], sm['gh'][:], ALU.add)
        else:
            P.memset(lo[:], -1.0e29, e='dve')
        P.ts(maskb[:, 0:N], sc[:, 0:N], lo[:, 0:1], ALU.is_gt)
        for kt in range(qt + 1):
            P.tr(pAb[:, kt * 128:(kt + 1) * 128], maskb[:, kt * 128:(kt + 1) * 128], k.idb[:, :])
        P.copy(mT[:, 0:N], pAb[:, 0:N])
        for kt in range(qt + 1):
            kk_ = slice(kt * 128, (kt + 1) * 128)
            for g in range(2):
                P.mm(psA[:, g * 512:(g + 1) * 512], lhsT=ckvT[:, kk_], rhs=qab[:, g * 4:(g + 1) * 4, q_])
            P.act(E[:, :], psA[:, :], AF.Exp)
            P.tt(Pm[:].rearrange("p (h q) -> p h q", q=128), E[:].rearrange("p (h q) -> p h q", q=128),
                 bc(mT[:, kk_].unsqueeze(1), [128, 8, 128]), ALU.mult, e=('pool' if kt % 2 else 'dve'))
            for g in range(2):
                P.mm(psB[:, g * 512:(g + 1) * 512], lhsT=ckvTok[:, kt, :], rhs=Pm[:, g * 512:(g + 1) * 512],
                     start=(kt == 0), stop=(kt == qt))
            P.mm(psC[:, :], lhsT=k.onesb[:, :], rhs=Pm[:, 0:512], start=(kt == 0), stop=(kt == qt))
            P.mm(psD[:, :], lhsT=k.onesb[:, :], rhs=Pm[:, 512:1024], start=(kt == 0), stop=(kt == qt))
        P.recip(rd[:, 0:512], psC[:, :])
        P.recip(rd[:, 512:1024], psD[:, :])
        P.tt(olat[:, :], psB[:, :], rd[:, :], ALU.mult)
        for j in range(4):
            o = psE[:, j * 128:(j + 1) * 128]
            P.mm(o, lhsT=wv[:, 2 * j, :], rhs=olat[:, 2 * j * 128:(2 * j + 1) * 128], start=True, stop=False)
            P.mm(o, lhsT=wv[:, 2 * j + 1, :], rhs=olat[:, (2 * j + 1) * 128:(2 * j + 2) * 128], start=False, stop=True)
        P.copy(yd[:], psE[:, :].rearrange("p (j q) -> p j q", q=128))
        P.dma('sp', k.YMIX[0:512, t0 + qt * 128:t0 + (qt + 1) * 128].rearrange("(j p) q -> p j q", p=128), yd[:])
    P.pop()
    P.pop()


def stage_out_ffn(k, l):
    if 'ffn' in SKIP:
        return
    P = k.P
    psC, psD, psE, psF = k.psC, k.psD, k.psE, k.psF
    P.push()
    wo = P.sb('wo', [128, 8, 1024], BF16)
    for kk in range(8):
        k.ldw(wo[:, kk, :], k.w_out[l, kk * 128:(kk + 1) * 128, :])
    ym = P.sb('ym', [128, 8, 512], BF16)
    yo = P.sb('yo', [128, 8, 512], F32)
    xt = P.sb('xt', [128, 8, 512], F32)
    sq = P.sb('sq', [128, 8, 512], BF16)
    rs = P.sb('rs', [128, 512], F32)
    hb = P.sb('hb', [128, 8, 512], BF16)
    it = 0
    for n in range(T // 512):
        b = n // 4
        tok = slice(n * 512, (n + 1) * 512)
        P.dma('sp', ym[:], k.YMIX[:, tok].rearrange("(k p) n -> p k n", p=128))
        P.dma('sp', xt[:], k.XT[:, tok].rearrange("(k p) n -> p k n", p=128))
        for m in range(8):
            ps = k.PSB[1 + (it % 3)]
            it += 1
            for kk in range(8):
                P.mm(ps[:, :], lhsT=wo[:, kk, m * 128:(m + 1) * 128], rhs=ym[:, kk, :], start=(kk == 0), stop=(kk == 7))
            if m % 2 == 0:
                P.act(yo[:, m, :], ps[:, :], AF.Copy)
            else:
                P.copy(yo[:, m, :], ps[:, :])
        k.rstd_of(yo[:], 8, 512, 1.0 / D, 1e-6, sq, psC, rs)
        P.tt(yo[:], yo[:], bc(rs[:].unsqueeze(1), [128, 8, 512]), ALU.mult)
        for m in range(8):
            P.stt(xt[:, m, :], yo[:, m, :], k.G1[:, l, m, b:b + 1], xt[:, m, :], ALU.mult, ALU.add)
        P.dma('sp', k.XT[:, tok].rearrange("(k p) n -> p k n", p=128), xt[:])
        k.rstd_of(xt[:], 8, 512, 1.0 / D, 1e-6, sq, psC, rs)
        P.tt(yo[:], xt[:], bc(rs[:].unsqueeze(1), [128, 8, 512]), ALU.mult)
        for kk in range(8):
            P.act(hb[:, kk, :], yo[:, kk, :], AF.Identity, bias=k.mod[:, l, 24 + kk, b:b + 1], scale=k.A2[:, l, kk, b:b + 1])
        P.dma('sp', k.H2[:, tok].rearrange("(k p) n -> p k n", p=128), hb[:])
    P.pop()
    P.push()
    wg = [P.sb('wg%d' % i, [128, 8, 512], BF16) for i in range(2)]
    wu = [P.sb('wu%d' % i, [128, 8, 512], BF16) for i in range(2)]
    h2 = [P.sb('h2%d' % i, [128, 8, 512], BF16) for i in range(2)]
    sgl = [P.sb('sgl%d' % i, [128, 512], F32) for i in range(2)]
    u16 = [P.sb('u16%d' % i, [128, 512], BF16) for i in range(2)]
    it = 0
    ih = 0
    for grp in range(6):
        nch = min(4, 22 - grp * 4)
        g_, u_ = wg[grp % 2], wu[grp % 2]
        for kk in range(8):
            k.ldw(g_[:, kk, 0:nch * 128], k.w_fc[l, kk * 128:(kk + 1) * 128, grp * 512:grp * 512 + nch * 128])
            k.ldw(u_[:, kk, 0:nch * 128], k.w_fc[l, kk * 128:(kk + 1) * 128, DFF + grp * 512:DFF + grp * 512 + nch * 128])
        for n in range(T // 512):
            tok = slice(n * 512, (n + 1) * 512)
            hh = h2[ih % 2]
            ih += 1
            P.dma('sp', hh[:], k.H2[:, tok].rearrange("(k p) n -> p k n", p=128))
            for c in range(nch):
                pg = psC if it % 2 == 0 else psE
                pu = psD if it % 2 == 0 else psF
                sg_, uu = sgl[it % 2], u16[it % 2]
                it += 1
                for kk in range(8):
                    P.mm(pg[:, :], lhsT=g_[:, kk, c * 128:(c + 1) * 128], rhs=hh[:, kk, :], start=(kk == 0), stop=(kk == 7))
                for kk in range(8):
                    P.mm(pu[:, :], lhsT=u_[:, kk, c * 128:(c + 1) * 128], rhs=hh[:, kk, :], start=(kk == 0), stop=(kk == 7))
                P.act(sg_[:], pg[:, :], AF.Silu)
                P.tt(uu[:], sg_[:], pu[:, :], ALU.mult)
                r0 = (grp * 4 + c) * 128
                P.dma('sp', k.UT[r0:r0 + 128, tok], uu[:], writes=['UT:%d:%d' % (grp * 4 + c, n)])
    P.pop()
    P.push()
    wd = P.sb('wd', [128, 22, 1024], BF16)
    for kk in range(22):
        k.ldw(wd[:, kk, :], k.w_down[l, kk * 128:(kk + 1) * 128, :])
    ut = P.sb('ut', [128, 22, 512], BF16)
    yo = P.sb('yo', [128, 8, 512], F32)
    xt = P.sb('xt', [128, 8, 512], F32)
    sq = P.sb('sq', [128, 8, 512], BF16)
    rs = P.sb('rs', [128, 512], F32)
    it = 0
    for n in range(T // 512):
        b = n // 4
        tok = slice(n * 512, (n + 1) * 512)
        P.dma('sp', ut[:], k.UT[:, tok].rearrange("(k p) n -> p k n", p=128), reads=['UT:%d:%d' % (c, n) for c in range(22)])
        P.dma('sp', xt[:], k.XT[:, tok].rearrange("(k p) n -> p k n", p=128))
        for m in range(8):
            ps = k.PSB[1 + (it % 3)]
            it += 1
            for kk in range(22):
                P.mm(ps[:, :], lhsT=wd[:, kk, m * 128:(m + 1) * 128], rhs=ut[:, kk, :], start=(kk == 0), stop=(kk == 21))
            if m % 2 == 0:
                P.act(yo[:, m, :], ps[:, :], AF.Copy)
            else:
                P.copy(yo[:, m, :], ps[:, :])
        k.rstd_of(yo[:], 8, 512, 1.0 / D, 1e-6, sq, psC, rs)
        P.tt(yo[:], yo[:], bc(rs[:].unsqueeze(1), [128, 8, 512]), ALU.mult)
        for m in range(8):
            P.stt(xt[:, m, :], yo[:, m, :], k.G2[:, l, m, b:b + 1], xt[:, m, :], ALU.mult, ALU.add)
        P.dma('sp', k.XT[:, tok].rearrange("(k p) n -> p k n", p=128), xt[:])
    P.pop()


def prep_shared(inp):
    f = np.float32
    sh = {}
    sh['ada_w'] = np.ascontiguousarray(inp['ada_w'], f)
    sh['ada_bT'] = np.ascontiguousarray(inp['ada_b'].reshape(L, 48, 128).transpose(2, 0, 1), f)
    g = np.stack([inp['pre_g_mix'], inp['post_g_mix'], inp['pre_g_ffn'], inp['post_g_ffn']], 0)
    sh['gains'] = np.ascontiguousarray(g.reshape(4, L, 8, 128).transpose(3, 0, 1, 2), f)
    W = np.zeros((L, D, PC), f)
    M = np.zeros((L, PC), f)
    wi = inp['w_in']
    ms = inp['mu_shift']
    W[:, :, 0:452] = wi[:, :, 0:452]
    W[:, :, 512:2048] = wi[:, :, 452:1988]
    M[:, 512:2048] = ms[:, 0:1536]
    W[:, :, 2048:2176] = wi[:, :, 1988:2116]
    M[:, 2048:2176] = ms[:, 1536:1664]
    W[:, :, 2176:2336] = wi[:, :, 2116:2276]
    M[:, 2176:2336] = ms[:, 1664:1824]
    W[1:, :, 2336:2368] = inp['w_in_vres']
    M[1:, 2336:2368] = inp['mu_vres']
    sh['w_in'] = W
    sh['muT'] = np.ascontiguousarray(M.reshape(L, 19, 128).transpose(2, 0, 1), f)
    sh['w_out'] = np.ascontiguousarray(inp['w_out'], f)
    sh['qng'] = np.ascontiguousarray(inp['q_norm_g'].reshape(L, 2, 128).transpose(2, 0, 1), f)
    sh['kvg'] = np.ascontiguousarray(inp['kv_norm_g'].T, f)
    sh['w_q'] = np.ascontiguousarray(inp['w_q_up'].reshape(L, 256, 512), f)
    sh['w_qi'] = np.ascontiguousarray(inp['w_qi_up'].reshape(L, 256, 256), f)
    wk = inp['w_k_up'].reshape(L, 128, 4, 2, 64)
    sh['wkT'] = np.ascontiguousarray(wk.transpose(0, 3, 4, 2, 1).reshape(L, 128, 4, 128), f)
    wv = np.zeros((L, 128, 8, 128), f)
    for h in range(8):
        wv[:, :, h, (h % 2) * 64:(h % 2) * 64 + 64] = inp['w_v_up'][:, :, h, :]
    sh['wvP'] = wv
    kl = np.stack([inp['kidx_ln_g'], inp['kidx_ln_b']], -1)
    kl = np.concatenate([kl, kl], 1)
    sh['kiln'] = np.ascontiguousarray(kl.transpose(1, 0, 2), f)
    rw = np.stack([inp['w0'], inp['a0'], inp['k_k'], inp['k_a'], inp['lnx_g'], inp['lnx_b'],
                   inp['r_k'].reshape(L, 512)], 0)
    sh['rwp'] = np.ascontiguousarray(rw.reshape(7, L, 4, 128).transpose(3, 0, 1, 2), f)
    v0 = np.zeros((L, 512), f)
    v0[1:] = inp['v0']
    sh['v0T'] = np.ascontiguousarray(v0.reshape(L, 4, 128).transpose(2, 0, 1), f)
    sh['w2a2'] = np.ascontiguousarray(np.concatenate([inp['w2'], inp['a2']], 1), f)
    sh['g2a'] = np.ascontiguousarray(inp['g2'][:, 0:128], f)
    gb = np.zeros((L, 64, 512), f)
    gb[:, 0:32] = inp['g2'][:, 128:160]
    gb[1:, 32:64] = inp['v2']
    sh['g2bv2'] = gb
    sh['w_fc'] = np.ascontiguousarray(inp['w_fc'], f)
    sh['w_down'] = np.ascontiguousarray(inp['w_down'], f)
    c = np.zeros((128, 1024), f)
    c[:, 0:128] = np.eye(128)
    c[0:64, 128:192] = 1.0
    c[64:128, 192:256] = 1.0
    si = np.arange(64)[:, None]
    ti = np.arange(64)[None, :]
    c[0:64, 256:320] = (si < ti)
    c[0:64, 320:384] = (si <= ti)
    c[0:64, 384:448] = (si > ti)
    c[:, 448:480] = 2.0 ** -(np.arange(32) + 1.0)
    sh['consts'] = c
    return sh


def prep_core(inp, ci):
    f = np.float32
    xb = np.asarray(inp['x'][ci * NB:(ci + 1) * NB], f).reshape(T, D)
    cb = np.asarray(inp['c'][ci * NB:(ci + 1) * NB], f)
    return {'xT': np.ascontiguousarray(xb.T),
            'cT': np.ascontiguousarray(cb.reshape(NB, 8, 128).transpose(2, 1, 0))}


_NC = None


def kernel(**inputs):
    global _NC
    inp = {k_: np.asarray(v) for k_, v in inputs.items()}
    if _NC is None:
        _NC = build()
    sh = prep_shared(inp)
    ncores = 8
    in_maps = []
    for ci in range(ncores):
        m = dict(sh)
        m.update(prep_core(inp, ci))
        in_maps.append(m)
    res = run_bass_kernel_spmd(_NC, in_maps, core_ids=list(range(ncores)))
    out = np.empty((16, S, D), np.float32)
    for ci in range(ncores):
        o = np.asarray(res.results[ci]['outT'])
        out[ci * NB:(ci + 1) * NB] = o.T.reshape(NB, S, D)
    return out
```

```python
import contextlib
import numpy as np
import concourse.bass as bass
import concourse.mybir as mybir
from concourse.bass_utils import run_bass_kernel_spmd
from concourse.alu_op_type import AluOpType as ALU

F32 = mybir.dt.float32
BF16 = mybir.dt.bfloat16
AF = mybir.ActivationFunctionType
AX = mybir.AxisListType

N_DMA_SLOTS = 32
D = 1024
L = 4
S = 2048
NB = 2
T = NB * S
PC = 2432
DFF = 2816
NEG = -1.0e30


class Prog:
    ENGS = ('pe', 'dve', 'act', 'pool', 'sp')

    def __init__(self, nc, stack):
        self.nc = nc
        self.stack = stack
        self.q = {e: [] for e in self.ENGS}
        self.cnt = {e: 0 for e in self.ENGS}
        self.waited = {e: {} for e in self.ENGS}
        self.lastw = {}
        self.readers = {}
        self.sems = {}
        for e in ('pe', 'dve', 'act', 'pool'):
            self.sems[e] = stack.enter_context(nc.semaphore('s_' + e))
        self.slot_cnt = []
        for i in range(N_DMA_SLOTS):
            self.sems['d%d' % i] = stack.enter_context(nc.semaphore('s_d%d' % i))
            self.slot_cnt.append(0)
        self.rr = 0
        self.n_ops = 0
        self.tid = 0
        self.scopes = []

    def push(self):
        st = contextlib.ExitStack()
        st.__enter__()
        self.scopes.append(st)

    def pop(self):
        self.barrier()
        self.scopes.pop().__exit__(None, None, None)

    def _st(self):
        return self.scopes[-1] if self.scopes else self.stack

    def sb(self, name, shape, dt):
        self.tid += 1
        return self._st().enter_context(self.nc.sbuf_tensor('%s_%d' % (name, self.tid), list(shape), dt))

    def ps(self, name, shape, dt):
        return self.stack.enter_context(self.nc.psum_tensor(name, list(shape), dt))

    @staticmethod
    def _res(items):
        out = []
        for it in items:
            if it is None:
                continue
            if isinstance(it, str):
                out.append(it)
            elif isinstance(it, (int, float)):
                continue
            else:
                out.append(it.tensor.name)
        return out

    def _deps(self, e, reads, writes):
        deps = []
        for r in reads:
            ev = self.lastw.get(r)
            if ev is not None:
                deps.append(ev)
        for w in writes:
            ev = self.lastw.get(w)
            if ev is not None:
                deps.append(ev)
            deps.extend(self.readers.get(w, ()))
        need = {}
        for (sk, v) in deps:
            if sk == e and e == 'pe':
                continue
            if self.waited[e].get(sk, 0) >= v:
                continue
            if need.get(sk, 0) < v:
                need[sk] = v
        return need

    def _emit_waits(self, e, need):
        for sk, v in need.items():
            self.waited[e][sk] = v
            self.q[e].append(('w', self.sems[sk], v))

    def _record(self, ev, reads, writes):
        for r in reads:
            self.readers.setdefault(r, []).append(ev)
        for w in writes:
            self.lastw[w] = ev
            self.readers[w] = []

    def op(self, e, fn, reads=(), writes=()):
        reads = self._res(reads)
        writes = self._res(writes)
        need = self._deps(e, reads, writes)
        self._emit_waits(e, need)
        self.cnt[e] += 1
        ev = (e, self.cnt[e])
        self.q[e].append(('i', fn, self.sems[e], 1))
        self._record(ev, reads, writes)
        self.n_ops += 1

    def dma(self, e, out, in_, reads=None, writes=None):
        reads = self._res(reads if reads is not None else [in_])
        writes = self._res(writes if writes is not None else [out])
        need = self._deps(e, reads, writes)
        slot = self.rr
        self.rr = (self.rr + 1) % N_DMA_SLOTS
        sk = 'd%d' % slot
        prev = self.slot_cnt[slot]
        if prev > 0 and self.waited[e].get(sk, 0) < prev and need.get(sk, 0) < prev:
            need[sk] = prev
        self._emit_waits(e, need)
        self.slot_cnt[slot] = prev + 16
        ev = (sk, prev + 16)
        self.q[e].append(('i', lambda eng: eng.dma_start(out=out, in_=in_), self.sems[sk], 16))
        self._record(ev, reads, writes)
        self.n_ops += 1

    def barrier(self):
        evs = {}
        for e in ('pe', 'dve', 'act', 'pool'):
            if self.cnt[e] > 0:
                evs[e] = self.cnt[e]
        for i, c in enumerate(self.slot_cnt):
            if c > 0:
                evs['d%d' % i] = c
        for e in self.ENGS:
            need = {}
            for sk, v in evs.items():
                if sk == e and e == 'pe':
                    continue
                if self.waited[e].get(sk, 0) < v:
                    need[sk] = v
            self._emit_waits(e, need)
        self.lastw = {}
        self.readers = {}

    def emit(self):
        nc = self.nc
        q = self.q
        with nc.Block() as block:
            def run(eng, items):
                for it in items:
                    if it[0] == 'w':
                        eng.wait_ge(it[1], it[2])
                    else:
                        it[1](eng).then_inc(it[2], it[3])

            @block.tensor
            def _(eng):
                run(eng, q['pe'])

            @block.vector
            def _(eng):
                run(eng, q['dve'])

            @block.scalar
            def _(eng):
                run(eng, q['act'])

            @block.gpsimd
            def _(eng):
                run(eng, q['pool'])

            @block.sync
            def _(eng):
                run(eng, q['sp'])

    def mm(self, out, lhsT, rhs, start=True, stop=True):
        self.op('pe', lambda e: e.matmul(out, lhsT=lhsT, rhs=rhs, start=start, stop=stop),
                reads=[lhsT, rhs], writes=[out])

    def tr(self, out, in_, ident):
        self.op('pe', lambda e: e.transpose(out=out, in_=in_, identity=ident),
                reads=[in_, ident], writes=[out])

    def act(self, out, in_, func, bias=None, scale=None, e='act'):
        kw = {}
        if bias is not None:
            kw['bias'] = bias
        if scale is not None:
            kw['scale'] = scale
        self.op('act', lambda eng: eng.activation(out=out, in_=in_, func=func, **kw),
                reads=[in_, bias, scale], writes=[out])

    def tt(self, out, in0, in1, op, e='dve'):
        self.op(e, lambda eng: eng.tensor_tensor(out=out, in0=in0, in1=in1, op=op),
                reads=[in0, in1], writes=[out])

    def ts(self, out, in0, s1, op0, s2=None, op1=None, e='dve', accum=None):
        kw = {}
        if op1 is not None:
            kw['op1'] = op1
        if accum is not None:
            kw['accum_out'] = accum
        self.op(e, lambda eng: eng.tensor_scalar(out=out, in0=in0, scalar1=s1, scalar2=s2, op0=op0, **kw),
                reads=[in0, s1, s2], writes=[out, accum])

    def stt(self, out, in0, scalar, in1, op0, op1):
        self.op('dve', lambda eng: eng.scalar_tensor_tensor(out=out, in0=in0, scalar=scalar, in1=in1, op0=op0, op1=op1),
                reads=[in0, scalar, in1], writes=[out])

    def copy(self, out, in_, e='dve'):
        self.op(e, lambda eng: eng.tensor_copy(out=out, in_=in_), reads=[in_], writes=[out])

    def memset(self, ap, val, e='pool'):
        self.op(e, lambda eng: eng.memset(ap, val), writes=[ap])

    def recip(self, out, in_):
        self.op('dve', lambda eng: eng.reciprocal(out=out, in_=in_), reads=[in_], writes=[out])

    def reduce(self, out, in_, op, axis=AX.X):
        self.op('dve', lambda eng: eng.tensor_reduce(out=out, in_=in_, axis=axis, op=op), reads=[in_], writes=[out])


def bc(ap, shape):
    return ap.to_broadcast(list(shape))


class K:
    pass


def build(n_layers=L, dbg=()):
    nc = bass.Bass("TRN2", target_bir_lowering=False)
    k = K()
    k.nc = nc

    def din(name, shape, dt=F32):
        return nc.dram_tensor(name, list(shape), dt, kind="ExternalInput").ap()

    def dscr(name, shape, dt):
        return nc.dram_tensor(name, list(shape), dt, kind="Internal").ap()

    xT = din('xT', [D, T])
    cT = din('cT', [128, 8, NB])
    ada_w = din('ada_w', [L, D, 6 * D])
    ada_bT = din('ada_bT', [128, L, 48])
    gains = din('gains', [128, 4, L, 8])
    w_in = din('w_in', [L, D, PC])
    muT = din('muT', [128, L, 19])
    w_out = din('w_out', [L, D, D])
    qng = din('qng', [128, L, 2])
    kvg = din('kvg', [128, L])
    w_q = din('w_q', [L, 256, 512])
    w_qi = din('w_qi', [L, 256, 256])
    wkT = din('wkT', [L, 128, 4, 128])
    wvP = din('wvP', [L, 128, 8, 128])
    kiln = din('kiln', [128, L, 2])
    rwp = din('rwp', [128, 7, L, 4])
    v0T = din('v0T', [128, L, 4])
    w2a2 = din('w2a2', [L, 128, 512])
    g2a = din('g2a', [L, 128, 512])
    g2bv2 = din('g2bv2', [L, 64, 512])
    w_fc = din('w_fc', [L, D, 2 * DFF])
    w_down = din('w_down', [L, DFF, D])
    consts = din('consts', [128, 1024])
    outT = nc.dram_tensor('outT', [D, T], F32, kind="ExternalOutput").ap()

    XT = dscr('XT', [D, T], F32)
    COLS = dscr('COLS', [PC, T], F32)
    YMIX = dscr('YMIX', [D, T], BF16)
    VF = dscr('VF', [512, T], F32)
    GS = dscr('GS', [512, T], F32)
    BON = dscr('BON', [512, T], F32)
    H2 = dscr('H2', [D, T], BF16)
    UT = dscr('UT', [DFF, T], BF16)
    dbg_out = {}
    for nm, shp in dbg:
        dbg_out[nm] = nc.dram_tensor('dbg_' + nm, list(shp), F32, kind="ExternalOutput").ap()

    with contextlib.ExitStack() as st:
        P = Prog(nc, st)
        k.P = P
        psA = P.ps('psA', [128, 1024], F32)
        psB = P.ps('psB', [128, 1024], F32)
        psC = P.ps('psC', [128, 512], F32)
        psD = P.ps('psD', [128, 512], F32)
        psE = P.ps('psE', [128, 512], F32)
        psF = P.ps('psF', [128, 512], F32)
        cst = P.sb('cst', [128, 1024], F32)
        P.dma('sp', cst[:], consts[:, :])
        idb = P.sb('idb', [128, 128], BF16)
        idf = P.sb('idf', [128, 128], F32)
        onesb = P.sb('onesb', [128, 128], BF16)
        blkb = P.sb('blkb', [128, 128], BF16)
        P.copy(idb[:], cst[:, 0:128])
        P.copy(idf[:], cst[:, 0:128])
        P.memset(onesb[:], 1.0)
        P.copy(blkb[:], cst[:, 128:256])
        mk1 = P.sb('mk1', [64, 128], F32)
        mk3 = P.sb('mk3', [64, 64], F32)
        id64b = P.sb('id64b', [64, 64], BF16)
        rmask = P.sb('rmask', [128, 512], F32)
        P.copy(mk1[:], cst[0:64, 256:384])
        P.copy(mk3[:], cst[0:64, 384:448])
        P.copy(id64b[:], cst[0:64, 0:64])
        P.memset(rmask[:], 1.0)
        P.memset(rmask[:].rearrange("p (c t) -> p c t", t=64)[:, :, 0:1], 0.0)
        pow2 = cst[:, 448:480]
        gn = P.sb('gn', [128, 4, L, 8], F32)
        P.dma('sp', gn[:], gains[:, :, :, :])
        abT = P.sb('abT', [128, L, 48], F32)
        P.dma('sp', abT[:], ada_bT[:, :, :])
        mu = P.sb('mu', [128, L, 19], F32)
        P.dma('sp', mu[:], muT[:, :, :])
        qg = P.sb('qg', [128, L, 2], F32)
        P.dma('sp', qg[:], qng[:, :, :])
        kg = P.sb('kg', [128, L], F32)
        P.dma('sp', kg[:], kvg[:, :])
        kl = P.sb('kl', [128, L, 2], F32)
        P.dma('sp', kl[:], kiln[:, :, :])
        rp = P.sb('rp', [128, 7, L, 4], F32)
        P.dma('sp', rp[:], rwp[:, :, :, :])
        v0s = P.sb('v0s', [128, L, 4], F32)
        P.dma('sp', v0s[:], v0T[:, :, :])
        mod = P.sb('mod', [128, L, 48, NB], F32)
        A1 = P.sb('A1', [128, L, 8, NB], F32)
        A2 = P.sb('A2', [128, L, 8, NB], F32)
        G1 = P.sb('G1', [128, L, 8, NB], F32)
        G2 = P.sb('G2', [128, L, 8, NB], F32)

        PSB = [psC, psD, psE, psF]

        P.push()
        ct = P.sb('ct', [128, 8, NB], F32)
        cond = P.sb('cond', [128, 8, NB], F32)
        P.dma('sp', ct[:], cT[:, :, :])
        P.act(cond[:], ct[:], AF.Silu)
        wa = [P.sb('wa%d' % i, [128, 8, 768], F32) for i in range(2)]
        it = 0
        for l in range(n_layers):
            for grp in range(8):
                w = wa[it % 2]
                ps = PSB[it % 2]
                it += 1
                for kk in range(8):
                    P.dma('sp', w[:, kk, :], ada_w[l, kk * 128:(kk + 1) * 128, grp * 768:(grp + 1) * 768])
                for mi in range(6):
                    for kk in range(8):
                        P.mm(ps[:, mi * 2:(mi + 1) * 2], lhsT=w[:, kk, mi * 128:(mi + 1) * 128], rhs=cond[:, kk, :],
                             start=(kk == 0), stop=(kk == 7))
                P.tt(mod[:, l, grp * 6:(grp + 1) * 6, :], ps[:, 0:12].rearrange("p (m b) -> p m b", b=NB),
                     bc(abT[:, l, grp * 6:(grp + 1) * 6].unsqueeze(2), [128, 6, NB]), ALU.add)
            for (dst, gi, sc0) in ((A1, 0, 8), (A2, 2, 32)):
                P.stt(dst[:, l, :, :], mod[:, l, sc0:sc0 + 8, :], 1.0,
                      bc(gn[:, gi, l, :].unsqueeze(2), [128, 8, NB]), ALU.add, ALU.mult)
            for (dst, gi, g0) in ((G1, 1, 16), (G2, 3, 40)):
                P.tt(dst[:, l, :, :], mod[:, l, g0:g0 + 8, :], bc(gn[:, gi, l, :].unsqueeze(2), [128, 8, NB]), ALU.mult)
        P.pop()

        P.push()
        xc = [P.sb('xc%d' % i, [128, 2048], F32) for i in range(2)]
        it = 0
        for kk in range(8):
            for hf in range(2):
                t_ = xc[it % 2]
                it += 1
                P.dma('sp', t_[:], xT[kk * 128:(kk + 1) * 128, hf * 2048:(hf + 1) * 2048])
                P.dma('sp', XT[kk * 128:(kk + 1) * 128, hf * 2048:(hf + 1) * 2048], t_[:])
        P.pop()

        def rstd_of(x, K_, N, inv_dim, eps, sq, ps, rs, ones=None, np_=128):
            P.act(sq[0:np_, 0:K_, 0:N], x, AF.Square)
            for kk in range(K_):
                P.mm(ps[:, 0:N], lhsT=(ones if ones is not None else onesb[0:np_, :]), rhs=sq[0:np_, kk, 0:N],
                     start=(kk == 0), stop=(kk == K_ - 1))
            P.act(rs[:, 0:N], ps[:, 0:N], AF.Sqrt, bias=eps_ap(eps), scale=inv_dim)
            P.recip(rs[:, 0:N], rs[:, 0:N])

        epst = P.sb('epst', [128, 4], F32)
        P.memset(epst[:, 0:1], 1e-6)
        P.memset(epst[:, 1:2], 64e-5)
        P.memset(epst[:, 2:3], 0.0)

        def eps_ap(eps):
            if eps == 1e-6:
                return epst[:, 0:1]
            if eps == 64e-5:
                return epst[:, 1:2]
            return epst[:, 2:3]

        stg = [P.sb('stg%d' % i, [128, 1024], F32) for i in range(2)]
        stg_i = [0]

        def ldw(dst, src):
            if len(dst.shape) == 3:
                dst = dst.rearrange("p a b -> p (a b)")
            if len(src.shape) == 3:
                src = src.rearrange("p a b -> p (a b)")
            rows, cols = dst.shape[0], dst.shape[1]
            for c0 in range(0, cols, 1024):
                cw = min(1024, cols - c0)
                t_ = stg[stg_i[0] % 2]
                stg_i[0] += 1
                P.dma('sp', t_[0:rows, 0:cw], src[:, c0:c0 + cw])
                P.copy(dst[:, c0:c0 + cw], t_[0:rows, 0:cw], e='pool')

        k.__dict__.update(locals())
        for l in range(n_layers):
            stage_proj(k, l)
            if 'cols' in dbg_out and l == 0:
                dump(k, COLS, dbg_out['cols'], PC)
            for s in range(NB):
                stage_rwkv(k, l, s)
                stage_dsa(k, l, s)
            if 'ymix' in dbg_out and l == 0:
                dump(k, YMIX, dbg_out['ymix'], D, bf=True)
            stage_out_ffn(k, l)
            if 'xl0' in dbg_out and l == 0:
                dump(k, XT, dbg_out['xl0'], D)
        P.push()
        xc = [P.sb('xo%d' % i, [128, 2048], F32) for i in range(2)]
        it = 0
        for kk in range(8):
            for hf in range(2):
                t_ = xc[it % 2]
                it += 1
                P.dma('sp', t_[:], XT[kk * 128:(kk + 1) * 128, hf * 2048:(hf + 1) * 2048])
                P.dma('sp', outT[kk * 128:(kk + 1) * 128, hf * 2048:(hf + 1) * 2048], t_[:])
        P.pop()
        P.emit()
    return nc


def dump(k, src, dst, rows, bf=False):
    P = k.P
    P.push()
    nchunk = (rows + 127) // 128
    bufs = [P.sb('dmp%d' % i, [128, 2048], BF16 if bf else F32) for i in range(2)]
    bufs2 = [P.sb('dmq%d' % i, [128, 2048], F32) for i in range(2)] if bf else None
    it = 0
    for m in range(nchunk):
        r = min(128, rows - m * 128)
        for hf in range(T // 2048):
            b = bufs[it % 2]
            P.dma('sp', b[0:r, :], src[m * 128:m * 128 + r, hf * 2048:(hf + 1) * 2048])
            if bf:
                b2 = bufs2[it % 2]
                P.copy(b2[0:r, :], b[0:r, :])
                b = b2
            P.dma('sp', dst[m * 128:m * 128 + r, hf * 2048:(hf + 1) * 2048], b[0:r, :])
            it += 1
    P.pop()


def stage_proj(k, l):
    P = k.P
    P.push()
    win = P.sb('win', [128, 8, PC], BF16)
    for kk in range(8):
        k.ldw(win[:, kk, :], k.w_in[l, kk * 128:(kk + 1) * 128, :])
    xt = P.sb('xt', [128, 8, 512], F32)
    sq = P.sb('sq', [128, 8, 512], BF16)
    rs = P.sb('rs', [128, 512], F32)
    hb = P.sb('hb', [128, 8, 512], BF16)
    co = [P.sb('co%d' % i, [128, 512], F32) for i in range(4)]
    it = 0
    for n in range(T // 512):
        b = n // 4
        tok = slice(n * 512, (n + 1) * 512)
        P.dma('sp', xt[:], k.XT[:, tok].rearrange("(k p) n -> p k n", p=128))
        k.rstd_of(xt[:], 8, 512, 1.0 / D, 1e-6, sq, k.psC, rs)
        P.tt(xt[:], xt[:], bc(rs[:].unsqueeze(1), [128, 8, 512]), ALU.mult)
        for kk in range(8):
            P.act(hb[:, kk, :], xt[:, kk, :], AF.Identity, bias=k.mod[:, l, kk, b:b + 1], scale=k.A1[:, l, kk, b:b + 1])
        for m in range(19):
            ps = k.PSB[1 + (it % 3)]
            c = co[it % 4]
            for kk in range(8):
                P.mm(ps[:, :], lhsT=win[:, kk, m * 128:(m + 1) * 128], rhs=hb[:, kk, :], start=(kk == 0), stop=(kk == 7))
            if it % 2 == 0:
                P.act(c[:], ps[:, :], AF.Copy)
            else:
                P.copy(c[:], ps[:, :])
            P.dma('sp', k.COLS[m * 128:(m + 1) * 128, tok], c[:], writes=['COLS:%d:%d' % (m, n)])
            it += 1
    P.pop()


def cols_res(m, s):
    return ['COLS:%d:%d' % (m, n) for n in range(s * 4, s * 4 + 4)]


EXPH = 0.6065306597126334


SKIP = set()


def stage_rwkv(k, l, s):
    if 'rwkv' in SKIP:
        return
    P = k.P
    rp, mu = k.rp, k.mu
    psA, psB, psC, psD, psE, psF = k.psA, k.psB, k.psC, k.psD, k.psE, k.psF
    t0 = s * S
    P.push()
    w2a2b = P.sb('w2a2b', [128, 512], BF16)
    g2ab = P.sb('g2ab', [128, 512], BF16)
    g2bv = P.sb('g2bv', [64, 512], BF16)
    k.ldw(w2a2b[:], k.w2a2[l, :, :])
    k.ldw(g2ab[:], k.g2a[l, :, :])
    k.ldw(g2bv[:], k.g2bv2[l, :, :])
    KR = P.sb('KR', [128, 4, 32, 2, 64], BF16)
    BT = P.sb('BT', [128, 4, 2048], BF16)
    KT = P.sb('KT', [128, 4, 2048], BF16)
    VB = P.sb('VB', [128, 4, 2048], BF16)
    WC = P.sb('WC', [128, 4, 32], F32)
    P.push()
    ush = P.sb('ush', [128, 2049], F32)
    shd = P.sb('shd', [128, 2048], F32)
    lt = P.sb('lt', [128, 2048], F32)
    lwb = P.sb('lwb', [128, 2048], BF16)
    sg1 = P.sb('sg1', [128, 2048], BF16)
    lg2 = P.sb('lg2', [64, 2048], BF16)
    P.memset(ush[:, 0:1], 0.0)

    def shift_load(m, dst, rows=128):
        P.dma('sp', ush[0:rows, 1:2049], k.COLS[m * 128:m * 128 + rows, t0:t0 + S], reads=cols_res(m, s))
        P.tt(shd[0:rows, :], ush[0:rows, 0:2048], ush[0:rows, 1:2049], ALU.subtract)
        P.stt(dst, shd[0:rows, :], mu[0:rows, l, m:m + 1], ush[0:rows, 1:2049], ALU.mult, ALU.add)

    shift_load(16, lt[:, :])
    P.act(lwb[0:64, :], lt[0:64, :], AF.Tanh)
    P.copy(lwb[64:128, :], lt[64:128, :])
    shift_load(17, lt[:, :])
    P.act(sg1[:, :], lt[:, :], AF.Sigmoid)
    shift_load(18, lt[0:64, :], rows=64)
    P.act(lg2[0:32, :], lt[0:32, :], AF.Sigmoid)
    P.copy(lg2[32:64, :], lt[32:64, :])
    rj = P.sb('rj', [128, 2048], F32)
    kj = P.sb('kj', [128, 2048], F32)
    vj = P.sb('vj', [128, 2048], F32)
    names = ['sig', 'aa', 'gg', 'vm', 'vf', 'kk', 'rn', 't1', 'kp', 'be', 'cs', 'Wt', 'Wi', 'csm', 'Wp', 'bon']
    W_ = {nm: P.sb(nm, [128, 512], F32) for nm in names}
    kq = P.sb('kq', [128, 512], BF16)
    rk = P.sb('rk', [128, 512], BF16)
    c3 = lambda ap: ap.rearrange("p (c t) -> p c t", t=64)
    for j in range(4):
        jc = slice(j * 128, (j + 1) * 128)
        shift_load(4 + j, rj[:, :])
        shift_load(8 + j, kj[:, :])
        shift_load(12 + j, vj[:, :])
        for n in range(4):
            tk = slice(n * 512, (n + 1) * 512)
            gt = slice(t0 + n * 512, t0 + (n + 1) * 512)
            sig, aa, gg, vm, vf, kk, rn, t1, kp, be, cs, Wt, Wi, csm, Wp, bon = [W_[nm] for nm in names]
            P.mm(psD[:, :], lhsT=w2a2b[0:64, jc], rhs=lwb[0:64, tk])
            P.act(sig[:], psD[:, :], AF.Sigmoid, bias=rp[:, 0, l, j:j + 1])
            P.mm(psE[:, :], lhsT=w2a2b[64:128, jc], rhs=lwb[64:128, tk])
            P.act(aa[:], psE[:, :], AF.Sigmoid, bias=rp[:, 1, l, j:j + 1])
            P.mm(psF[:, :], lhsT=g2ab[:, jc], rhs=sg1[:, tk], start=True, stop=False)
            P.mm(psF[:, :], lhsT=g2bv[0:32, jc], rhs=lg2[0:32, tk], start=False, stop=True)
            P.copy(gg[:], psF[:, :])
            P.dma('sp', k.GS[jc, gt], gg[:])
            if l > 0:
                P.mm(psC[:, :], lhsT=g2bv[32:64, jc], rhs=lg2[32:64, tk])
                P.act(vm[:], psC[:, :], AF.Sigmoid, bias=k.v0s[:, l, j:j + 1])
                P.dma('sp', vf[:], k.VF[jc, gt])
                P.tt(vf[:], vf[:], vj[:, tk], ALU.subtract)
                P.tt(vf[:], vf[:], vm[:], ALU.mult)
                P.tt(vj[:, tk], vj[:, tk], vf[:], ALU.add)
            else:
                P.dma('sp', k.VF[jc, gt], vj[:, tk])
            P.copy(VB[:, j, tk], vj[:, tk], e='pool')
            P.ts(kk[:], kj[:, tk], rp[:, 2, l, j:j + 1], ALU.mult)
            P.act(kq[:], kk[:], AF.Square)
            P.mm(psD[:, :], lhsT=k.blkb[:, :], rhs=kq[:])
            P.act(rn[:], psD[:, :], AF.Sqrt)
            P.ts(rn[:], rn[:], 1e-12, ALU.max)
            P.recip(rn[:], rn[:])
            P.tt(kk[:], kk[:], rn[:], ALU.mult)
            P.ts(t1[:], aa[:], -1.0, ALU.add, rp[:, 3, l, j:j + 1], ALU.mult)
            P.stt(kp[:], t1[:], 1.0, kj[:, tk], ALU.add, ALU.mult)
            P.tt(be[:], kk[:], aa[:], ALU.mult, e='pool')
            P.op('dve', lambda eng, cs=cs, sig=sig: eng.tensor_tensor_scan(out=cs[:], data0=k.rmask[:], data1=sig[:], initial=0.0, op0=ALU.mult, op1=ALU.add),
                 reads=[k.rmask[:], sig[:]], writes=[cs[:]])
            P.act(Wt[:], cs[:], AF.Exp, scale=-EXPH)
            P.act(Wi[:], cs[:], AF.Exp, scale=EXPH)
            P.tt(csm[:], cs[:], sig[:], ALU.subtract, e='pool')
            P.act(Wp[:], csm[:], AF.Exp, scale=-EXPH)
            P.copy(WC[:, j, n * 8:(n + 1) * 8], c3(Wt[:])[:, :, 63])
            P.tt(KR[:, j, n * 8:(n + 1) * 8, 0, :], c3(kk[:]), c3(Wp[:]), ALU.mult)
            P.tt(KR[:, j, n * 8:(n + 1) * 8, 1, :], c3(rj[:, tk]), c3(Wt[:]), ALU.mult)
            P.tt(BT[:, j, tk], be[:], Wi[:], ALU.mult, e='pool')
            P.tt(KT[:, j, tk], kp[:], Wi[:], ALU.mult)
            P.stt(rk[:], rj[:, tk], rp[:, 6, l, j:j + 1], kp[:], ALU.mult, ALU.mult)
            P.mm(psE[:, :], lhsT=k.blkb[:, :], rhs=rk[:])
            P.tt(bon[:], psE[:, :], vj[:, tk], ALU.mult)
            P.dma('sp', k.BON[jc, gt], bon[:])
    P.pop()
    P.push()
    Mf = P.sb('Mf', [128, 4, 128], F32)
    Mb = P.sb('Mb', [128, 4, 128], BF16)
    Mt = P.sb('Mt', [128, 4, 128], F32)
    blk32 = P.sb('blk32', [128, 128], F32)
    P.copy(blk32[:], k.cst[:, 128:256])
    P.memset(Mf[:], 0.0)
    P.memset(Mb[:], 0.0)
    tok3 = P.sb('tok3', [64, 3, 512], BF16)
    m1 = P.sb('m1', [64, 8, 128], BF16)
    m2 = P.sb('m2', [64, 8, 128], BF16)
    am = P.sb('am', [64, 8, 64], BF16)
    xs = [P.sb('xs%d' % i, [64, 8, 64], BF16) for i in range(2)]
    zs = [P.sb('zs%d' % i, [64, 8, 64], BF16) for i in range(2)]
    pps = [P.sb('pp%d' % i, [64, 8, 64], BF16) for i in range(2)]
    nr = P.sb('nr', [64, 8, 64], BF16)
    ub = P.sb('ub', [64, 8, 64], BF16)
    yt8 = P.sb('yt8', [64, 8, 8, 64], F32)
    ysq = P.sb('ysq', [64, 64, 64], F32)
    st_ = {nm: P.sb(nm, [64, 64], F32) for nm in ('s1', 's2', 'mean', 'msq', 'var', 'rstd')}
    yfm = P.sb('yfm', [128, 4, 512], F32)
    ye = P.sb('ye', [128, 512], F32)
    bo2 = P.sb('bo2', [128, 512], F32)
    gg2 = P.sb('gg2', [128, 512], F32)
    yo16 = P.sb('yo16', [128, 512], BF16)
    pAb = psA[:, :].bitcast(BF16)
    h8 = lambda ap, w: ap.rearrange("p (h c) -> p h c", c=w)
    for c in range(0 if 'rec' in SKIP else 32):
        cs_ = slice(c * 64, (c + 1) * 64)
        for i, src in enumerate((VB, BT, KT)):
            for j in range(4):
                P.tr(pAb[0:64, i * 512 + j * 128:i * 512 + (j + 1) * 128], src[:, j, cs_], k.idb[:, :])
        P.act(tok3[:].rearrange("p a b -> p (a b)"), pAb[0:64, 0:1536], AF.Copy)
        Vt = tok3[:, 0, :]
        Bt = tok3[:, 1, :]
        Kt = tok3[:, 2, :]
        hp = lambda h: slice((h % 2) * 64, (h % 2) * 64 + 64)
        pos = lambda h: (h % 2) * 4 + h // 2
        for h in range(8):
            P.mm(psB[0:64, pos(h) * 128:(pos(h) + 1) * 128], lhsT=BT[hp(h), h // 2, cs_],
                 rhs=KR[hp(h), h // 2, c, :, :].rearrange("p a b -> p (a b)"))
        P.tt(m1[:], h8(psB[0:64, :], 128), bc(k.mk1[:].unsqueeze(1), [64, 8, 128]), ALU.mult)
        for h in range(8):
            P.mm(psA[0:64, pos(h) * 128:(pos(h) + 1) * 128], lhsT=KT[hp(h), h // 2, cs_],
                 rhs=KR[hp(h), h // 2, c, :, :].rearrange("p a b -> p (a b)"))
        P.tt(m2[:], h8(psA[0:64, :], 128), bc(k.mk1[:].unsqueeze(1), [64, 8, 128]), ALU.mult)
        for h in range(8):
            o0 = (h % 2) * 512 + (h // 2) * 64
            P.mm(psB[0:64, o0:o0 + 64], lhsT=KR[hp(h), h // 2, c, 0, :], rhs=BT[hp(h), h // 2, cs_])
        P.tt(am[:].rearrange("p (q j) c -> p q j c", q=2),
             psB[0:64, :].rearrange("p (q x) -> p q x", q=2)[:, :, 0:256].rearrange("p q (j c) -> p q j c", c=64),
             bc(k.mk3[:].unsqueeze(1).unsqueeze(1), [64, 2, 4, 64]), ALU.mult)
        P.tt(pps[0][:], bc(k.id64b[:].unsqueeze(1), [64, 8, 64]), m1[:, :, 0:64], ALU.subtract)
        X = m1[:, :, 0:64]
        Z = am[:]
        Pc = pps[0]
        for lev in range(1, 6):
            Xn = xs[lev % 2]
            Zn = zs[lev % 2]
            Pn = pps[lev % 2]
            if lev < 5:
                for h in range(8):
                    P.mm(psD[0:64, h * 64:(h + 1) * 64], lhsT=Z[:, h, :], rhs=X[:, h, :])
            for h in range(8):
                P.mm(psE[0:64, h * 64:(h + 1) * 64], lhsT=X[:, h, :], rhs=Z[:, h, :])
            if lev < 5:
                P.act(Xn[:], h8(psD[0:64, :], 64), AF.Copy)
            P.copy(Zn[:], h8(psE[0:64, :], 64))
            for h in range(8):
                P.mm(psF[0:64, h * 64:(h + 1) * 64], lhsT=Zn[:, h, :], rhs=Pc[:, h, :])
            P.tt(Pn[:], h8(psF[0:64, :], 64), Pc[:], ALU.add)
            X, Z, Pc = Xn[:], Zn[:], Pn
        TT = Pc
        if 'ph2' in SKIP:
            continue
        for j in range(4):
            jc = slice(j * 128, (j + 1) * 128)
            P.mm(psD[0:64, jc], lhsT=KR[:, j, c, 0, :], rhs=Mb[:, j, :], start=True, stop=False)
            for h in (2 * j, 2 * j + 1):
                P.mm(psD[0:64, h * 64:(h + 1) * 64], lhsT=m2[:, pos(h), 0:64], rhs=Vt[:, h * 64:(h + 1) * 64],
                     start=False, stop=(h == 2 * j + 1))
        P.act(nr[:], h8(psD[0:64, :], 64), AF.Copy, scale=-1.0)
        for h in range(8):
            P.mm(psE[0:64, h * 64:(h + 1) * 64], lhsT=TT[:, pos(h), :], rhs=nr[:, h, :])
        P.copy(ub[:], h8(psE[0:64, :], 64))
        ubf = ub[:].rearrange("p h c -> p (h c)")
        for j in range(4):
            jc = slice(j * 128, (j + 1) * 128)
            P.mm(psF[0:64, jc], lhsT=KR[:, j, c, 1, :], rhs=Mb[:, j, :], start=True, stop=False)
            for h in (2 * j, 2 * j + 1):
                o = psF[0:64, h * 64:(h + 1) * 64]
                P.mm(o, lhsT=m1[:, pos(h), 64:128], rhs=ub[:, h, :], start=False, stop=False)
                P.mm(o, lhsT=m2[:, pos(h), 64:128], rhs=Vt[:, h * 64:(h + 1) * 64], start=False, stop=(h == 2 * j + 1))
        P.act(yt8[:, c % 8, :, :], h8(psF[0:64, :], 64), AF.Copy)
        for j in range(4):
            jc = slice(j * 128, (j + 1) * 128)
            P.mm(psC[:, jc], lhsT=Bt[:, jc], rhs=ubf[:, jc], start=True, stop=False)
            P.mm(psC[:, jc], lhsT=Kt[:, jc], rhs=Vt[:, jc], start=False, stop=True)
        P.tt(Mt[:], h8(psC[:, :], 128), bc(blk32[:].unsqueeze(1), [128, 4, 128]), ALU.mult)
        P.tt(Mf[:], Mt[:], Mf[:], ALU.add)
        P.tt(Mf[:], Mf[:], bc(WC[:, :, c:c + 1], [128, 4, 128]), ALU.mult)
        P.copy(Mb[:], Mf[:], e='pool')
        if c % 8 == 7:
            s1, s2, mean, msq, var, rstd = [st_[nm] for nm in ('s1', 's2', 'mean', 'msq', 'var', 'rstd')]
            y3 = yt8[:].rearrange("p a h c -> p (a h) c")
            P.reduce(s1[:], y3, ALU.add)
            P.act(ysq[:], y3, AF.Square)
            P.reduce(s2[:], ysq[:], ALU.add)
            P.ts(mean[:], s1[:], 1.0 / 64, ALU.mult)
            P.tt(msq[:], mean[:], mean[:], ALU.mult)
            P.stt(var[:], s2[:], 1.0 / 64, msq[:], ALU.mult, ALU.subtract)
            P.act(rstd[:], var[:], AF.Sqrt, bias=k.epst[0:64, 1:2])
            P.recip(rstd[:], rstd[:])
            P.tt(y3, y3, bc(mean[:].unsqueeze(2), [64, 64, 64]), ALU.subtract)
            P.tt(y3, y3, bc(rstd[:].unsqueeze(2), [64, 64, 64]), ALU.mult)
            for half in range(2):
                for a in range(4):
                    ca = half * 4 + a
                    for j in range(4):
                        P.tr(psB[:, (a * 4 + j) * 64:(a * 4 + j + 1) * 64],
                             yt8[:, ca, 2 * j:2 * j + 2, :].rearrange("p h c -> p (h c)"), k.idf[0:64, 0:64])
                P.copy(yfm[:, :, half * 256:(half + 1) * 256].rearrange("p j (a t) -> p a j t", t=64),
                       psB[:, :].rearrange("p (a j t) -> p a j t", j=4, t=64))
        if c % 8 == 7:
            n = c // 8
            gt = slice(t0 + n * 512, t0 + (n + 1) * 512)
            for j in range(4):
                jc = slice(j * 128, (j + 1) * 128)
                P.ts(ye[:], yfm[:, j, :], rp[:, 4, l, j:j + 1], ALU.mult, rp[:, 5, l, j:j + 1], ALU.add)
                P.dma('sp', bo2[:], k.BON[jc, gt])
                P.dma('sp', gg2[:], k.GS[jc, gt])
                P.tt(ye[:], ye[:], bo2[:], ALU.add)
                P.tt(yo16[:], ye[:], gg2[:], ALU.mult)
                P.dma('sp', k.YMIX[512 + j * 128:512 + (j + 1) * 128, gt], yo16[:])
    P.pop()
    P.pop()


NIT = 13


def stage_dsa(k, l, s):
    if 'dsa' in SKIP:
        return
    P = k.P
    psA, psB, psC, psD, psE, psF = k.psA, k.psB, k.psC, k.psD, k.psE, k.psF
    t0 = s * S
    P.push()
    wq = P.sb('wq', [128, 2, 512], BF16)
    wqi = P.sb('wqi', [128, 2, 256], BF16)
    wk = P.sb('wk', [128, 4, 128], BF16)
    wv = P.sb('wv', [128, 8, 128], BF16)
    for kk in range(2):
        k.ldw(wq[:, kk, :], k.w_q[l, kk * 128:(kk + 1) * 128, :])
        k.ldw(wqi[:, kk, :], k.w_qi[l, kk * 128:(kk + 1) * 128, :])
    k.ldw(wk[:], k.wkT[l, :, :, :])
    k.ldw(wv[:], k.wvP[l, :, :, :])
    blkf = P.sb('blkf', [128, 128], F32)
    P.copy(blkf[:], k.cst[:, 128:256])
    cqb = P.sb('cqb', [128, 2, 2048], BF16)
    ckvT = P.sb('ckvT', [128, 2048], BF16)
    ckvTok = P.sb('ckvTok', [128, 16, 128], BF16)
    kix = P.sb('kix', [128, 2048], BF16)
    qab = P.sb('qab', [128, 8, 2048], BF16)
    qib = P.sb('qib', [128, 2, 2048], BF16)
    wht = P.sb('wht', [128, 16, 4], F32)
    pAb = psA[:, :].bitcast(BF16)
    P.push()
    cq32 = P.sb('cq32', [128, 2, 512], F32)
    sq = P.sb('sq', [128, 2, 512], BF16)
    rs = P.sb('rs', [128, 512], F32)
    kv32 = P.sb('kv32', [128, 512], F32)
    ki32 = P.sb('ki32', [128, 512], F32)
    sqf = P.sb('sqf', [128, 512], F32)
    mean = P.sb('mean', [128, 512], F32)
    msq = P.sb('msq', [128, 512], F32)
    var = P.sb('var', [128, 512], F32)
    wi = P.sb('wi', [4, 512], F32)
    qb = P.sb('qb', [128, 4, 512], BF16)
    for n in range(4):
        tk = slice(n * 512, (n + 1) * 512)
        gt = slice(t0 + n * 512, t0 + (n + 1) * 512)
        nn = s * 4 + n
        for kk in range(2):
            P.dma('sp', cq32[:, kk, :], k.COLS[kk * 128:(kk + 1) * 128, gt], reads=['COLS:%d:%d' % (kk, nn)])
        k.rstd_of(cq32[:], 2, 512, 1.0 / 256, 1e-6, sq, psC, rs)
        P.tt(cq32[:], cq32[:], bc(rs[:].unsqueeze(1), [128, 2, 512]), ALU.mult)
        for kk in range(2):
            P.act(cqb[:, kk, tk], cq32[:, kk, :], AF.Copy, scale=k.qg[:, l, kk:kk + 1])
        P.dma('sp', kv32[:], k.COLS[256:384, gt], reads=['COLS:2:%d' % nn])
        k.rstd_of(kv32[:].unsqueeze(1), 1, 512, 1.0 / 128, 1e-6, sq, psC, rs)
        P.tt(kv32[:], kv32[:], rs[:], ALU.mult)
        P.act(ckvT[:, tk], kv32[:], AF.Copy, scale=k.kg[:, l:l + 1])
        for i in range(4):
            P.tr(pAb[:, i * 128:(i + 1) * 128], ckvT[:, n * 512 + i * 128:n * 512 + (i + 1) * 128], k.idb[:, :])
        P.copy(ckvTok[:, n * 4:(n + 1) * 4, :], pAb[:, 0:512].rearrange("p (a b) -> p a b", b=128))
        P.dma('sp', ki32[0:64, :], k.COLS[384:448, gt], reads=['COLS:3:%d' % nn])
        P.dma('sp', ki32[64:128, :], k.COLS[384:448, gt], reads=['COLS:3:%d' % nn])
        P.mm(psD[:, :], lhsT=blkf[:, :], rhs=ki32[:, :])
        P.act(sqf[:], ki32[:], AF.Square)
        P.mm(psE[:, :], lhsT=blkf[:, :], rhs=sqf[:, :])
        P.ts(mean[:], psD[:, :], 1.0 / 64, ALU.mult)
        P.tt(msq[:], mean[:], mean[:], ALU.mult)
        P.stt(var[:], psE[:, :], 1.0 / 64, msq[:], ALU.mult, ALU.subtract)
        P.act(var[:], var[:], AF.Sqrt, bias=k.epst[:, 0:1])
        P.recip(var[:], var[:])
        P.tt(ki32[:], ki32[:], mean[:], ALU.subtract)
        P.tt(ki32[:], ki32[:], var[:], ALU.mult)
        P.act(kix[:, tk], ki32[:], AF.Identity, bias=k.kl[:, l, 1:2], scale=k.kl[:, l, 0:1])
        P.dma('sp', wi[:, :], k.COLS[448:452, gt], reads=['COLS:3:%d' % nn])
        for i in range(4):
            P.tr(psD[:, i * 4:(i + 1) * 4], wi[0:4, i * 128:(i + 1) * 128], k.idf[0:4, 0:4])
        P.ts(wht[:, n * 4:(n + 1) * 4, :], psD[:, 0:16].rearrange("p (a b) -> p a b", b=4), 0.0625, ALU.mult)
        for m in range(4):
            for kk in range(2):
                P.mm(psE[:, :], lhsT=wq[:, kk, m * 128:(m + 1) * 128], rhs=cqb[:, kk, tk], start=(kk == 0), stop=(kk == 1))
            P.copy(qb[:, m, :], psE[:, :])
        for h in range(8):
            hp = slice((h % 2) * 64, (h % 2) * 64 + 64)
            P.mm(psF[:, :], lhsT=wk[hp, h // 2, :], rhs=qb[hp, h // 2, :])
            P.act(qab[:, h, tk], psF[:, :], AF.Copy, scale=0.125)
        for m in range(2):
            for kk in range(2):
                P.mm(psE[:, :], lhsT=wqi[:, kk, m * 128:(m + 1) * 128], rhs=cqb[:, kk, tk], start=(kk == 0), stop=(kk == 1))
            P.copy(qib[:, m, tk], psE[:, :])
    P.pop()
    P.push()
    sc = P.sb('sc', [128, 2048], F32)
    tmp = P.sb('tmp', [128, 2048], F32)
    junk = P.sb('junk', [128, 2048], BF16)
    maskb = P.sb('maskb', [128, 2048], BF16)
    mT = P.sb('mT', [128, 2048], BF16)
    E = P.sb('E', [128, 1024], BF16)
    Pm = P.sb('Pm', [128, 1024], BF16)
    rd = P.sb('rd', [128, 1024], F32)
    olat = P.sb('olat', [128, 1024], BF16)
    yd = P.sb('yd', [128, 4, 128], BF16)
    sm = {nm: P.sb(nm, [128, 1], F32) for nm in ('hi', 'lo', 'w0', 'mid', 'cnt', 'gh')}
    Hs = P.sb('Hs', [128, 32], F32)
    for qt in range(16):
        q_ = slice(qt * 128, (qt + 1) * 128)
        N = (qt + 1) * 128
        for hi in range(4):
            hp = slice((hi % 2) * 64, (hi % 2) * 64 + 64)
            for half, pst in enumerate((psA, psB)):
                cols_h = min(1024, N - half * 1024)
                if cols_h <= 0:
                    continue
                for bnk in range((cols_h + 511) // 512):
                    cw = min(512, cols_h - bnk * 512)
                    k0 = half * 1024 + bnk * 512
                    P.mm(pst[:, bnk * 512:bnk * 512 + cw], lhsT=qib[hp, hi // 2, q_], rhs=kix[hp, k0:k0 + cw])
                dst = sc if hi == 0 else tmp
                seg = slice(half * 1024, half * 1024 + cols_h)
                P.ts(dst[:, seg], pst[:, 0:cols_h], 0.0, ALU.max, wht[:, qt, hi:hi + 1], ALU.mult)
                if hi > 0:
                    P.tt(sc[:, seg], sc[:, seg], tmp[:, seg], ALU.add, e='pool')
        P.memset(sc[0:64, qt * 128 + 64:(qt + 1) * 128], NEG)
        lo = sm['lo']
        if qt >= 2:
            P.reduce(sm['hi'][:], sc[:, 0:N], ALU.max)
            P.reduce(lo[:], sc[:, 0:qt * 128 + 64], ALU.min)
            P.tt(sm['w0'][:], sm['hi'][:], lo[:], ALU.subtract)
            P.ts(Hs[:, 0:NIT + 1], k.cst[:, 448:448 + NIT + 1], sm['w0'][:, 0:1], ALU.mult)
            P.tt(lo[:], lo[:], Hs[:, 0:1], ALU.add)
            for i in range(NIT):
                P.ts(junk[:, 0:N], sc[:, 0:N], lo[:, 0:1], ALU.is_gt, None, ALU.add, accum=sm['cnt'][:])
                P.ts(sm['gh'][:], sm['cnt'][:], 255.5, ALU.is_gt, Hs[:, i:i + 1], ALU.mult)
                sub = Hs[:, i + 1:i + 2] if i < NIT - 1 else Hs[:, i:i + 1]
                P.stt(lo[:], sm['gh'][:], sub, lo[:], ALU.subtract, ALU.add)
        else:
            P.memset(lo[:], -1.0e29, e='dve')
        P.ts(maskb[:, 0:N], sc[:, 0:N], lo[:, 0:1], ALU.is_gt)
        for kt in range(qt + 1):
            P.tr(pAb[:, kt * 128:(kt + 1) * 128], maskb[:, kt * 128:(kt + 1) * 128], k.idb[:, :])
        P.copy(mT[:, 0:N], pAb[:, 0:N])
        for kt in range(qt + 1):
            kk_ = slice(kt * 128, (kt + 1) * 128)
            for g in range(2):
                P.mm(psA[:, g * 512:(g + 1) * 512], lhsT=ckvT[:, kk_], rhs=qab[:, g * 4:(g + 1) * 4, q_])
            P.act(E[:, :], psA[:, :], AF.Exp)
            P.tt(Pm[:].rearrange("p (h q) -> p h q", q=128), E[:].rearrange("p (h q) -> p h q", q=128),
                 bc(mT[:, kk_].unsqueeze(1), [128, 8, 128]), ALU.mult, e=('pool' if kt % 2 else 'dve'))
            for g in range(2):
                P.mm(psB[:, g * 512:(g + 1) * 512], lhsT=ckvTok[:, kt, :], rhs=Pm[:, g * 512:(g + 1) * 512],
                     start=(kt == 0), stop=(kt == qt))
            P.mm(psC[:, :], lhsT=k.onesb[:, :], rhs=Pm[:, 0:512], start=(kt == 0), stop=(kt == qt))
            P.mm(psD[:, :], lhsT=k.onesb[:, :], rhs=Pm[:, 512:1024], start=(kt == 0), stop=(kt == qt))
        P.recip(rd[:, 0:512], psC[:, :])
        P.recip(rd[:, 512:1024], psD[:, :])
        P.tt(olat[:, :], psB[:, :], rd[:, :], ALU.mult)
        for j in range(4):
            o = psE[:, j * 128:(j + 1) * 128]
            P.mm(o, lhsT=wv[:, 2 * j, :], rhs=olat[:, 2 * j * 128:(2 * j + 1) * 128], start=True, stop=False)
            P.mm(o, lhsT=wv[:, 2 * j + 1, :], rhs=olat[:, (2 * j + 1) * 128:(2 * j + 2) * 128], start=False, stop=True)
        P.copy(yd[:], psE[:, :].rearrange("p (j q) -> p j q", q=128))
        P.dma('sp', k.YMIX[0:512, t0 + qt * 128:t0 + (qt + 1) * 128].rearrange("(j p) q -> p j q", p=128), yd[:])
    P.pop()
    P.pop()


def stage_out_ffn(k, l):
    if 'ffn' in SKIP:
        return
    P = k.P
    psC, psD, psE, psF = k.psC, k.psD, k.psE, k.psF
    P.push()
    wo = P.sb('wo', [128, 8, 1024], BF16)
    for kk in range(8):
        k.ldw(wo[:, kk, :], k.w_out[l, kk * 128:(kk + 1) * 128, :])
    ym = P.sb('ym', [128, 8, 512], BF16)
    yo = P.sb('yo', [128, 8, 512], F32)
    xt = P.sb('xt', [128, 8, 512], F32)
    sq = P.sb('sq', [128, 8, 512], BF16)
    rs = P.sb('rs', [128, 512], F32)
    hb = P.sb('hb', [128, 8, 512], BF16)
    it = 0
    for n in range(T // 512):
        b = n // 4
        tok = slice(n * 512, (n + 1) * 512)
        P.dma('sp', ym[:], k.YMIX[:, tok].rearrange("(k p) n -> p k n", p=128))
        P.dma('sp', xt[:], k.XT[:, tok].rearrange("(k p) n -> p k n", p=128))
        for m in range(8):
            ps = k.PSB[1 + (it % 3)]
            it += 1
            for kk in range(8):
                P.mm(ps[:, :], lhsT=wo[:, kk, m * 128:(m + 1) * 128], rhs=ym[:, kk, :], start=(kk == 0), stop=(kk == 7))
            if m % 2 == 0:
                P.act(yo[:, m, :], ps[:, :], AF.Copy)
            else:
                P.copy(yo[:, m, :], ps[:, :])
        k.rstd_of(yo[:], 8, 512, 1.0 / D, 1e-6, sq, psC, rs)
        P.tt(yo[:], yo[:], bc(rs[:].unsqueeze(1), [128, 8, 512]), ALU.mult)
        for m in range(8):
            P.stt(xt[:, m, :], yo[:, m, :], k.G1[:, l, m, b:b + 1], xt[:, m, :], ALU.mult, ALU.add)
        P.dma('sp', k.XT[:, tok].rearrange("(k p) n -> p k n", p=128), xt[:])
        k.rstd_of(xt[:], 8, 512, 1.0 / D, 1e-6, sq, psC, rs)
        P.tt(yo[:], xt[:], bc(rs[:].unsqueeze(1), [128, 8, 512]), ALU.mult)
        for kk in range(8):
            P.act(hb[:, kk, :], yo[:, kk, :], AF.Identity, bias=k.mod[:, l, 24 + kk, b:b + 1], scale=k.A2[:, l, kk, b:b + 1])
        P.dma('sp', k.H2[:, tok].rearrange("(k p) n -> p k n", p=128), hb[:])
    P.pop()
    P.push()
    wg = [P.sb('wg%d' % i, [128, 8, 512], BF16) for i in range(2)]
    wu = [P.sb('wu%d' % i, [128, 8, 512], BF16) for i in range(2)]
    h2 = [P.sb('h2%d' % i, [128, 8, 512], BF16) for i in range(2)]
    sgl = [P.sb('sgl%d' % i, [128, 512], F32) for i in range(2)]
    u16 = [P.sb('u16%d' % i, [128, 512], BF16) for i in range(2)]
    def load_w(grp):
        nch = min(4, 22 - grp * 4)
        g_, u_ = wg[grp % 2], wu[grp % 2]
        for kk in range(8):
            k.ldw(g_[:, kk, 0:nch * 128], k.w_fc[l, kk * 128:(kk + 1) * 128, grp * 512:grp * 512 + nch * 128])
            k.ldw(u_[:, kk, 0:nch * 128], k.w_fc[l, kk * 128:(kk + 1) * 128, DFF + grp * 512:DFF + grp * 512 + nch * 128])

    iters = [(grp, n) for grp in range(6) for n in range(T // 512)]

    def load_h(i):
        n = iters[i][1]
        P.dma('sp', h2[i % 2][:], k.H2[:, n * 512:(n + 1) * 512].rearrange("(k p) n -> p k n", p=128))

    load_w(0)
    load_h(0)
    it = 0
    for i, (grp, n) in enumerate(iters):
        nch = min(4, 22 - grp * 4)
        g_, u_ = wg[grp % 2], wu[grp % 2]
        if n == 0 and grp + 1 < 6:
            load_w(grp + 1)
        if i + 1 < len(iters):
            load_h(i + 1)
        tok = slice(n * 512, (n + 1) * 512)
        hh = h2[i % 2]
        for c in range(nch):
            pg = psC if it % 2 == 0 else psE
            pu = psD if it % 2 == 0 else psF
            sg_, uu = sgl[it % 2], u16[it % 2]
            it += 1
            for kk in range(8):
                P.mm(pg[:, :], lhsT=g_[:, kk, c * 128:(c + 1) * 128], rhs=hh[:, kk, :], start=(kk == 0), stop=(kk == 7))
            for kk in range(8):
                P.mm(pu[:, :], lhsT=u_[:, kk, c * 128:(c + 1) * 128], rhs=hh[:, kk, :], start=(kk == 0), stop=(kk == 7))
            P.act(sg_[:], pg[:, :], AF.Silu)
            P.tt(uu[:], sg_[:], pu[:, :], ALU.mult)
            r0 = (grp * 4 + c) * 128
            P.dma('sp', k.UT[r0:r0 + 128, tok], uu[:], writes=['UT:%d:%d' % (grp * 4 + c, n)])
    P.pop()
    P.push()
    wd = P.sb('wd', [128, 22, 1024], BF16)
    for kk in range(22):
        k.ldw(wd[:, kk, :], k.w_down[l, kk * 128:(kk + 1) * 128, :])
    uts = [P.sb('ut%d' % i, [128, 22, 512], BF16) for i in range(2)]
    xts = [P.sb('xtd%d' % i, [128, 8, 512], F32) for i in range(2)]
    yo = P.sb('yo', [128, 8, 512], F32)
    sq = P.sb('sq', [128, 8, 512], BF16)
    rs = P.sb('rs', [128, 512], F32)

    def load_t(n):
        tok = slice(n * 512, (n + 1) * 512)
        P.dma('sp', uts[n % 2][:], k.UT[:, tok].rearrange("(k p) n -> p k n", p=128), reads=['UT:%d:%d' % (c, n) for c in range(22)])
        P.dma('sp', xts[n % 2][:], k.XT[:, tok].rearrange("(k p) n -> p k n", p=128), reads=['XT:%d' % n])

    load_t(0)
    it = 0
    for n in range(T // 512):
        b = n // 4
        tok = slice(n * 512, (n + 1) * 512)
        ut, xt = uts[n % 2], xts[n % 2]
        if n + 1 < T // 512:
            load_t(n + 1)
        for m in range(8):
            ps = k.PSB[1 + (it % 3)]
            it += 1
            for kk in range(22):
                P.mm(ps[:, :], lhsT=wd[:, kk, m * 128:(m + 1) * 128], rhs=ut[:, kk, :], start=(kk == 0), stop=(kk == 21))
            if m % 2 == 0:
                P.act(yo[:, m, :], ps[:, :], AF.Copy)
            else:
                P.copy(yo[:, m, :], ps[:, :])
        k.rstd_of(yo[:], 8, 512, 1.0 / D, 1e-6, sq, psC, rs)
        P.tt(yo[:], yo[:], bc(rs[:].unsqueeze(1), [128, 8, 512]), ALU.mult)
        for m in range(8):
            P.stt(xt[:, m, :], yo[:, m, :], k.G2[:, l, m, b:b + 1], xt[:, m, :], ALU.mult, ALU.add)
        P.dma('sp', k.XT[:, tok].rearrange("(k p) n -> p k n", p=128), xt[:], writes=['XT:%d' % n])
    P.pop()


def prep_shared(inp):
    f = np.float32
    sh = {}
    sh['ada_w'] = np.ascontiguousarray(inp['ada_w'], f)
    sh['ada_bT'] = np.ascontiguousarray(inp['ada_b'].reshape(L, 48, 128).transpose(2, 0, 1), f)
    g = np.stack([inp['pre_g_mix'], inp['post_g_mix'], inp['pre_g_ffn'], inp['post_g_ffn']], 0)
    sh['gains'] = np.ascontiguousarray(g.reshape(4, L, 8, 128).transpose(3, 0, 1, 2), f)
    W = np.zeros((L, D, PC), f)
    M = np.zeros((L, PC), f)
    wi = inp['w_in']
    ms = inp['mu_shift']
    W[:, :, 0:452] = wi[:, :, 0:452]
    W[:, :, 512:2048] = wi[:, :, 452:1988]
    M[:, 512:2048] = ms[:, 0:1536]
    W[:, :, 2048:2176] = wi[:, :, 1988:2116]
    M[:, 2048:2176] = ms[:, 1536:1664]
    W[:, :, 2176:2336] = wi[:, :, 2116:2276]
    M[:, 2176:2336] = ms[:, 1664:1824]
    W[1:, :, 2336:2368] = inp['w_in_vres']
    M[1:, 2336:2368] = inp['mu_vres']
    sh['w_in'] = W
    sh['muT'] = np.ascontiguousarray(M.reshape(L, 19, 128).transpose(2, 0, 1), f)
    sh['w_out'] = np.ascontiguousarray(inp['w_out'], f)
    sh['qng'] = np.ascontiguousarray(inp['q_norm_g'].reshape(L, 2, 128).transpose(2, 0, 1), f)
    sh['kvg'] = np.ascontiguousarray(inp['kv_norm_g'].T, f)
    sh['w_q'] = np.ascontiguousarray(inp['w_q_up'].reshape(L, 256, 512), f)
    sh['w_qi'] = np.ascontiguousarray(inp['w_qi_up'].reshape(L, 256, 256), f)
    wk = inp['w_k_up'].reshape(L, 128, 4, 2, 64)
    sh['wkT'] = np.ascontiguousarray(wk.transpose(0, 3, 4, 2, 1).reshape(L, 128, 4, 128), f)
    wv = np.zeros((L, 128, 8, 128), f)
    for h in range(8):
        wv[:, :, h, (h % 2) * 64:(h % 2) * 64 + 64] = inp['w_v_up'][:, :, h, :]
    sh['wvP'] = wv
    kl = np.stack([inp['kidx_ln_g'], inp['kidx_ln_b']], -1)
    kl = np.concatenate([kl, kl], 1)
    sh['kiln'] = np.ascontiguousarray(kl.transpose(1, 0, 2), f)
    rw = np.stack([inp['w0'], inp['a0'], inp['k_k'], inp['k_a'], inp['lnx_g'], inp['lnx_b'],
                   inp['r_k'].reshape(L, 512)], 0)
    sh['rwp'] = np.ascontiguousarray(rw.reshape(7, L, 4, 128).transpose(3, 0, 1, 2), f)
    v0 = np.zeros((L, 512), f)
    v0[1:] = inp['v0']
    sh['v0T'] = np.ascontiguousarray(v0.reshape(L, 4, 128).transpose(2, 0, 1), f)
    sh['w2a2'] = np.ascontiguousarray(np.concatenate([inp['w2'], inp['a2']], 1), f)
    sh['g2a'] = np.ascontiguousarray(inp['g2'][:, 0:128], f)
    gb = np.zeros((L, 64, 512), f)
    gb[:, 0:32] = inp['g2'][:, 128:160]
    gb[1:, 32:64] = inp['v2']
    sh['g2bv2'] = gb
    sh['w_fc'] = np.ascontiguousarray(inp['w_fc'], f)
    sh['w_down'] = np.ascontiguousarray(inp['w_down'], f)
    c = np.zeros((128, 1024), f)
    c[:, 0:128] = np.eye(128)
    c[0:64, 128:192] = 1.0
    c[64:128, 192:256] = 1.0
    si = np.arange(64)[:, None]
    ti = np.arange(64)[None, :]
    c[0:64, 256:320] = (si < ti)
    c[0:64, 320:384] = (si <= ti)
    c[0:64, 384:448] = (si > ti)
    c[:, 448:480] = 2.0 ** -(np.arange(32) + 1.0)
    sh['consts'] = c
    return sh


def prep_core(inp, ci):
    f = np.float32
    xb = np.asarray(inp['x'][ci * NB:(ci + 1) * NB], f).reshape(T, D)
    cb = np.asarray(inp['c'][ci * NB:(ci + 1) * NB], f)
    return {'xT': np.ascontiguousarray(xb.T),
            'cT': np.ascontiguousarray(cb.reshape(NB, 8, 128).transpose(2, 1, 0))}


_NC = None


def kernel(**inputs):
    global _NC
    inp = {k_: np.asarray(v) for k_, v in inputs.items()}
    if _NC is None:
        _NC = build()
    sh = prep_shared(inp)
    ncores = 8
    in_maps = []
    for ci in range(ncores):
        m = dict(sh)
        m.update(prep_core(inp, ci))
        in_maps.append(m)
    res = run_bass_kernel_spmd(_NC, in_maps, core_ids=list(range(ncores)))
    out = np.empty((16, S, D), np.float32)
    for ci in range(ncores):
        o = np.asarray(res.results[ci]['outT'])
        out[ci * NB:(ci + 1) * NB] = o.T.reshape(NB, S, D)
    return out
```

```python
import contextlib
import numpy as np
import concourse.bass as bass
import concourse.mybir as mybir
from concourse.bass_utils import run_bass_kernel_spmd
from concourse.alu_op_type import AluOpType as ALU

F32 = mybir.dt.float32
BF16 = mybir.dt.bfloat16
AF = mybir.ActivationFunctionType
AX = mybir.AxisListType

N_DMA_SLOTS = 32
D = 1024
L = 4
S = 2048
NB = 2
T = NB * S
PC = 2432
DFF = 2816
NEG = -1.0e30


class Prog:
    ENGS = ('pe', 'dve', 'act', 'pool', 'sp')

    def __init__(self, nc, stack):
        self.nc = nc
        self.stack = stack
        self.q = {e: [] for e in self.ENGS}
        self.cnt = {e: 0 for e in self.ENGS}
        self.waited = {e: {} for e in self.ENGS}
        self.lastw = {}
        self.readers = {}
        self.sems = {}
        for e in ('pe', 'dve', 'act', 'pool'):
            self.sems[e] = stack.enter_context(nc.semaphore('s_' + e))
        self.slot_cnt = []
        for i in range(N_DMA_SLOTS):
            self.sems['d%d' % i] = stack.enter_context(nc.semaphore('s_d%d' % i))
            self.slot_cnt.append(0)
        self.rr = 0
        self.n_ops = 0
        self.tid = 0
        self.scopes = []

    def push(self):
        st = contextlib.ExitStack()
        st.__enter__()
        self.scopes.append(st)

    def pop(self):
        self.barrier()
        self.scopes.pop().__exit__(None, None, None)

    def _st(self):
        return self.scopes[-1] if self.scopes else self.stack

    def sb(self, name, shape, dt):
        self.tid += 1
        return self._st().enter_context(self.nc.sbuf_tensor('%s_%d' % (name, self.tid), list(shape), dt))

    def ps(self, name, shape, dt):
        return self.stack.enter_context(self.nc.psum_tensor(name, list(shape), dt))

    @staticmethod
    def _res(items):
        out = []
        for it in items:
            if it is None:
                continue
            if isinstance(it, str):
                out.append(it)
            elif isinstance(it, (int, float)):
                continue
            else:
                out.append(it.tensor.name)
        return out

    def _deps(self, e, reads, writes):
        deps = []
        for r in reads:
            ev = self.lastw.get(r)
            if ev is not None:
                deps.append(ev)
        for w in writes:
            ev = self.lastw.get(w)
            if ev is not None:
                deps.append(ev)
            deps.extend(self.readers.get(w, ()))
        need = {}
        for (sk, v) in deps:
            if sk == e and e == 'pe':
                continue
            if self.waited[e].get(sk, 0) >= v:
                continue
            if need.get(sk, 0) < v:
                need[sk] = v
        return need

    def _emit_waits(self, e, need):
        for sk, v in need.items():
            self.waited[e][sk] = v
            self.q[e].append(('w', self.sems[sk], v))

    def _record(self, ev, reads, writes):
        for r in reads:
            self.readers.setdefault(r, []).append(ev)
        for w in writes:
            self.lastw[w] = ev
            self.readers[w] = []

    def op(self, e, fn, reads=(), writes=()):
        reads = self._res(reads)
        writes = self._res(writes)
        need = self._deps(e, reads, writes)
        self._emit_waits(e, need)
        self.cnt[e] += 1
        ev = (e, self.cnt[e])
        self.q[e].append(('i', fn, self.sems[e], 1))
        self._record(ev, reads, writes)
        self.n_ops += 1

    def dma(self, e, out, in_, reads=None, writes=None):
        reads = self._res(reads if reads is not None else [in_])
        writes = self._res(writes if writes is not None else [out])
        need = self._deps(e, reads, writes)
        slot = self.rr
        self.rr = (self.rr + 1) % N_DMA_SLOTS
        sk = 'd%d' % slot
        prev = self.slot_cnt[slot]
        if prev > 0 and self.waited[e].get(sk, 0) < prev and need.get(sk, 0) < prev:
            need[sk] = prev
        self._emit_waits(e, need)
        self.slot_cnt[slot] = prev + 16
        ev = (sk, prev + 16)
        self.q[e].append(('i', lambda eng: eng.dma_start(out=out, in_=in_), self.sems[sk], 16))
        self._record(ev, reads, writes)
        self.n_ops += 1

    def barrier(self):
        evs = {}
        for e in ('pe', 'dve', 'act', 'pool'):
            if self.cnt[e] > 0:
                evs[e] = self.cnt[e]
        for i, c in enumerate(self.slot_cnt):
            if c > 0:
                evs['d%d' % i] = c
        for e in self.ENGS:
            need = {}
            for sk, v in evs.items():
                if sk == e and e == 'pe':
                    continue
                if self.waited[e].get(sk, 0) < v:
                    need[sk] = v
            self._emit_waits(e, need)
        self.lastw = {}
        self.readers = {}

    def emit(self):
        nc = self.nc
        q = self.q
        with nc.Block() as block:
            def run(eng, items):
                for it in items:
                    if it[0] == 'w':
                        eng.wait_ge(it[1], it[2])
                    else:
                        it[1](eng).then_inc(it[2], it[3])

            @block.tensor
            def _(eng):
                run(eng, q['pe'])

            @block.vector
            def _(eng):
                run(eng, q['dve'])

            @block.scalar
            def _(eng):
                run(eng, q['act'])

            @block.gpsimd
            def _(eng):
                run(eng, q['pool'])

            @block.sync
            def _(eng):
                run(eng, q['sp'])

    def mm(self, out, lhsT, rhs, start=True, stop=True):
        self.op('pe', lambda e: e.matmul(out, lhsT=lhsT, rhs=rhs, start=start, stop=stop),
                reads=[lhsT, rhs], writes=[out])

    def tr(self, out, in_, ident):
        self.op('pe', lambda e: e.transpose(out=out, in_=in_, identity=ident),
                reads=[in_, ident], writes=[out])

    def act(self, out, in_, func, bias=None, scale=None, e='act'):
        kw = {}
        if bias is not None:
            kw['bias'] = bias
        if scale is not None:
            kw['scale'] = scale
        self.op('act', lambda eng: eng.activation(out=out, in_=in_, func=func, **kw),
                reads=[in_, bias, scale], writes=[out])

    def tt(self, out, in0, in1, op, e='dve'):
        self.op(e, lambda eng: eng.tensor_tensor(out=out, in0=in0, in1=in1, op=op),
                reads=[in0, in1], writes=[out])

    def ts(self, out, in0, s1, op0, s2=None, op1=None, e='dve', accum=None):
        kw = {}
        if op1 is not None:
            kw['op1'] = op1
        if accum is not None:
            kw['accum_out'] = accum
        self.op(e, lambda eng: eng.tensor_scalar(out=out, in0=in0, scalar1=s1, scalar2=s2, op0=op0, **kw),
                reads=[in0, s1, s2], writes=[out, accum])

    def stt(self, out, in0, scalar, in1, op0, op1):
        self.op('dve', lambda eng: eng.scalar_tensor_tensor(out=out, in0=in0, scalar=scalar, in1=in1, op0=op0, op1=op1),
                reads=[in0, scalar, in1], writes=[out])

    def copy(self, out, in_, e='dve'):
        self.op(e, lambda eng: eng.tensor_copy(out=out, in_=in_), reads=[in_], writes=[out])

    def memset(self, ap, val, e='pool'):
        self.op(e, lambda eng: eng.memset(ap, val), writes=[ap])

    def recip(self, out, in_):
        self.op('dve', lambda eng: eng.reciprocal(out=out, in_=in_), reads=[in_], writes=[out])

    def reduce(self, out, in_, op, axis=AX.X):
        self.op('dve', lambda eng: eng.tensor_reduce(out=out, in_=in_, axis=axis, op=op), reads=[in_], writes=[out])


def bc(ap, shape):
    return ap.to_broadcast(list(shape))


class K:
    pass


def build(n_layers=L, dbg=()):
    nc = bass.Bass("TRN2", target_bir_lowering=False)
    k = K()
    k.nc = nc

    def din(name, shape, dt=F32):
        return nc.dram_tensor(name, list(shape), dt, kind="ExternalInput").ap()

    def dscr(name, shape, dt):
        return nc.dram_tensor(name, list(shape), dt, kind="Internal").ap()

    xT = din('xT', [D, T])
    cT = din('cT', [128, 8, NB])
    ada_w = din('ada_w', [L, D, 6 * D])
    ada_bT = din('ada_bT', [128, L, 48])
    gains = din('gains', [128, 4, L, 8])
    w_in = din('w_in', [L, D, PC])
    muT = din('muT', [128, L, 19])
    w_out = din('w_out', [L, D, D])
    qng = din('qng', [128, L, 2])
    kvg = din('kvg', [128, L])
    w_q = din('w_q', [L, 256, 512])
    w_qi = din('w_qi', [L, 256, 256])
    wkT = din('wkT', [L, 128, 4, 128])
    wvP = din('wvP', [L, 128, 8, 128])
    kiln = din('kiln', [128, L, 2])
    rwp = din('rwp', [128, 7, L, 4])
    v0T = din('v0T', [128, L, 4])
    w2a2 = din('w2a2', [L, 128, 512])
    g2a = din('g2a', [L, 128, 512])
    g2bv2 = din('g2bv2', [L, 64, 512])
    w_fc = din('w_fc', [L, D, 2 * DFF])
    w_down = din('w_down', [L, DFF, D])
    consts = din('consts', [128, 1024])
    outT = nc.dram_tensor('outT', [D, T], F32, kind="ExternalOutput").ap()

    XT = dscr('XT', [D, T], F32)
    COLS = dscr('COLS', [PC, T], F32)
    YMIX = dscr('YMIX', [D, T], BF16)
    VF = dscr('VF', [512, T], F32)
    GS = dscr('GS', [512, T], F32)
    BON = dscr('BON', [512, T], F32)
    H2 = dscr('H2', [D, T], BF16)
    UT = dscr('UT', [DFF, T], BF16)
    dbg_out = {}
    for nm, shp in dbg:
        dbg_out[nm] = nc.dram_tensor('dbg_' + nm, list(shp), F32, kind="ExternalOutput").ap()

    with contextlib.ExitStack() as st:
        P = Prog(nc, st)
        k.P = P
        psA = P.ps('psA', [128, 1024], F32)
        psB = P.ps('psB', [128, 1024], F32)
        psC = P.ps('psC', [128, 512], F32)
        psD = P.ps('psD', [128, 512], F32)
        psE = P.ps('psE', [128, 512], F32)
        psF = P.ps('psF', [128, 512], F32)
        cst = P.sb('cst', [128, 1024], F32)
        P.dma('sp', cst[:], consts[:, :])
        idb = P.sb('idb', [128, 128], BF16)
        idf = P.sb('idf', [128, 128], F32)
        onesb = P.sb('onesb', [128, 128], BF16)
        blkb = P.sb('blkb', [128, 128], BF16)
        P.copy(idb[:], cst[:, 0:128])
        P.copy(idf[:], cst[:, 0:128])
        P.memset(onesb[:], 1.0)
        P.copy(blkb[:], cst[:, 128:256])
        mk1 = P.sb('mk1', [64, 128], F32)
        mk3 = P.sb('mk3', [64, 64], F32)
        id64b = P.sb('id64b', [64, 64], BF16)
        rmask = P.sb('rmask', [128, 512], F32)
        P.copy(mk1[:], cst[0:64, 256:384])
        P.copy(mk3[:], cst[0:64, 384:448])
        P.copy(id64b[:], cst[0:64, 0:64])
        P.memset(rmask[:], 1.0)
        P.memset(rmask[:].rearrange("p (c t) -> p c t", t=64)[:, :, 0:1], 0.0)
        pow2 = cst[:, 448:480]
        gn = P.sb('gn', [128, 4, L, 8], F32)
        P.dma('sp', gn[:], gains[:, :, :, :])
        abT = P.sb('abT', [128, L, 48], F32)
        P.dma('sp', abT[:], ada_bT[:, :, :])
        mu = P.sb('mu', [128, L, 19], F32)
        P.dma('sp', mu[:], muT[:, :, :])
        qg = P.sb('qg', [128, L, 2], F32)
        P.dma('sp', qg[:], qng[:, :, :])
        kg = P.sb('kg', [128, L], F32)
        P.dma('sp', kg[:], kvg[:, :])
        kl = P.sb('kl', [128, L, 2], F32)
        P.dma('sp', kl[:], kiln[:, :, :])
        rp = P.sb('rp', [128, 7, L, 4], F32)
        P.dma('sp', rp[:], rwp[:, :, :, :])
        v0s = P.sb('v0s', [128, L, 4], F32)
        P.dma('sp', v0s[:], v0T[:, :, :])
        mod = P.sb('mod', [128, L, 48, NB], F32)
        A1 = P.sb('A1', [128, L, 8, NB], F32)
        A2 = P.sb('A2', [128, L, 8, NB], F32)
        G1 = P.sb('G1', [128, L, 8, NB], F32)
        G2 = P.sb('G2', [128, L, 8, NB], F32)

        PSB = [psC, psD, psE, psF]

        P.push()
        ct = P.sb('ct', [128, 8, NB], F32)
        cond = P.sb('cond', [128, 8, NB], F32)
        P.dma('sp', ct[:], cT[:, :, :])
        P.act(cond[:], ct[:], AF.Silu)
        wa = [P.sb('wa%d' % i, [128, 8, 768], F32) for i in range(2)]
        it = 0
        for l in range(n_layers):
            for grp in range(8):
                w = wa[it % 2]
                ps = PSB[it % 2]
                it += 1
                for kk in range(8):
                    P.dma('sp', w[:, kk, :], ada_w[l, kk * 128:(kk + 1) * 128, grp * 768:(grp + 1) * 768])
                for mi in range(6):
                    for kk in range(8):
                        P.mm(ps[:, mi * 2:(mi + 1) * 2], lhsT=w[:, kk, mi * 128:(mi + 1) * 128], rhs=cond[:, kk, :],
                             start=(kk == 0), stop=(kk == 7))
                P.tt(mod[:, l, grp * 6:(grp + 1) * 6, :], ps[:, 0:12].rearrange("p (m b) -> p m b", b=NB),
                     bc(abT[:, l, grp * 6:(grp + 1) * 6].unsqueeze(2), [128, 6, NB]), ALU.add)
            for (dst, gi, sc0) in ((A1, 0, 8), (A2, 2, 32)):
                P.stt(dst[:, l, :, :], mod[:, l, sc0:sc0 + 8, :], 1.0,
                      bc(gn[:, gi, l, :].unsqueeze(2), [128, 8, NB]), ALU.add, ALU.mult)
            for (dst, gi, g0) in ((G1, 1, 16), (G2, 3, 40)):
                P.tt(dst[:, l, :, :], mod[:, l, g0:g0 + 8, :], bc(gn[:, gi, l, :].unsqueeze(2), [128, 8, NB]), ALU.mult)
        P.pop()

        P.push()
        xc = [P.sb('xc%d' % i, [128, 2048], F32) for i in range(2)]
        it = 0
        for kk in range(8):
            for hf in range(2):
                t_ = xc[it % 2]
                it += 1
                P.dma('sp', t_[:], xT[kk * 128:(kk + 1) * 128, hf * 2048:(hf + 1) * 2048])
                P.dma('sp', XT[kk * 128:(kk + 1) * 128, hf * 2048:(hf + 1) * 2048], t_[:])
        P.pop()

        def rstd_of(x, K_, N, inv_dim, eps, sq, ps, rs, ones=None, np_=128):
            P.act(sq[0:np_, 0:K_, 0:N], x, AF.Square)
            for kk in range(K_):
                P.mm(ps[:, 0:N], lhsT=(ones if ones is not None else onesb[0:np_, :]), rhs=sq[0:np_, kk, 0:N],
                     start=(kk == 0), stop=(kk == K_ - 1))
            P.act(rs[:, 0:N], ps[:, 0:N], AF.Sqrt, bias=eps_ap(eps), scale=inv_dim)
            P.recip(rs[:, 0:N], rs[:, 0:N])

        epst = P.sb('epst', [128, 4], F32)
        P.memset(epst[:, 0:1], 1e-6)
        P.memset(epst[:, 1:2], 64e-5)
        P.memset(epst[:, 2:3], 0.0)

        def eps_ap(eps):
            if eps == 1e-6:
                return epst[:, 0:1]
            if eps == 64e-5:
                return epst[:, 1:2]
            return epst[:, 2:3]

        stg = [P.sb('stg%d' % i, [128, 1024], F32) for i in range(2)]
        stg_i = [0]

        def ldw(dst, src):
            if len(dst.shape) == 3:
                dst = dst.rearrange("p a b -> p (a b)")
            if len(src.shape) == 3:
                src = src.rearrange("p a b -> p (a b)")
            rows, cols = dst.shape[0], dst.shape[1]
            for c0 in range(0, cols, 1024):
                cw = min(1024, cols - c0)
                t_ = stg[stg_i[0] % 2]
                stg_i[0] += 1
                P.dma('sp', t_[0:rows, 0:cw], src[:, c0:c0 + cw])
                P.copy(dst[:, c0:c0 + cw], t_[0:rows, 0:cw], e='pool')

        k.__dict__.update(locals())
        for l in range(n_layers):
            stage_proj(k, l)
            if 'cols' in dbg_out and l == 0:
                dump(k, COLS, dbg_out['cols'], PC)
            for s in range(NB):
                stage_rwkv(k, l, s)
                stage_dsa(k, l, s)
            if 'ymix' in dbg_out and l == 0:
                dump(k, YMIX, dbg_out['ymix'], D, bf=True)
            stage_out_ffn(k, l)
            if 'xl0' in dbg_out and l == 0:
                dump(k, XT, dbg_out['xl0'], D)
        P.push()
        xc = [P.sb('xo%d' % i, [128, 2048], F32) for i in range(2)]
        it = 0
        for kk in range(8):
            for hf in range(2):
                t_ = xc[it % 2]
                it += 1
                P.dma('sp', t_[:], XT[kk * 128:(kk + 1) * 128, hf * 2048:(hf + 1) * 2048])
                P.dma('sp', outT[kk * 128:(kk + 1) * 128, hf * 2048:(hf + 1) * 2048], t_[:])
        P.pop()
        P.emit()
    return nc


def dump(k, src, dst, rows, bf=False):
    P = k.P
    P.push()
    nchunk = (rows + 127) // 128
    bufs = [P.sb('dmp%d' % i, [128, 2048], BF16 if bf else F32) for i in range(2)]
    bufs2 = [P.sb('dmq%d' % i, [128, 2048], F32) for i in range(2)] if bf else None
    it = 0
    for m in range(nchunk):
        r = min(128, rows - m * 128)
        for hf in range(T // 2048):
            b = bufs[it % 2]
            P.dma('sp', b[0:r, :], src[m * 128:m * 128 + r, hf * 2048:(hf + 1) * 2048])
            if bf:
                b2 = bufs2[it % 2]
                P.copy(b2[0:r, :], b[0:r, :])
                b = b2
            P.dma('sp', dst[m * 128:m * 128 + r, hf * 2048:(hf + 1) * 2048], b[0:r, :])
            it += 1
    P.pop()


def stage_proj(k, l):
    P = k.P
    P.push()
    win = P.sb('win', [128, 8, PC], BF16)
    for kk in range(8):
        k.ldw(win[:, kk, :], k.w_in[l, kk * 128:(kk + 1) * 128, :])
    xt = P.sb('xt', [128, 8, 512], F32)
    sq = P.sb('sq', [128, 8, 512], BF16)
    rs = P.sb('rs', [128, 512], F32)
    hb = P.sb('hb', [128, 8, 512], BF16)
    co = [P.sb('co%d' % i, [128, 512], F32) for i in range(4)]
    it = 0
    for n in range(T // 512):
        b = n // 4
        tok = slice(n * 512, (n + 1) * 512)
        P.dma('sp', xt[:], k.XT[:, tok].rearrange("(k p) n -> p k n", p=128))
        k.rstd_of(xt[:], 8, 512, 1.0 / D, 1e-6, sq, k.psC, rs)
        P.tt(xt[:], xt[:], bc(rs[:].unsqueeze(1), [128, 8, 512]), ALU.mult)
        for kk in range(8):
            P.act(hb[:, kk, :], xt[:, kk, :], AF.Identity, bias=k.mod[:, l, kk, b:b + 1], scale=k.A1[:, l, kk, b:b + 1])
        for m in range(19):
            ps = k.PSB[1 + (it % 3)]
            c = co[it % 4]
            for kk in range(8):
                P.mm(ps[:, :], lhsT=win[:, kk, m * 128:(m + 1) * 128], rhs=hb[:, kk, :], start=(kk == 0), stop=(kk == 7))
            if it % 2 == 0:
                P.act(c[:], ps[:, :], AF.Copy)
            else:
                P.copy(c[:], ps[:, :])
            P.dma('sp', k.COLS[m * 128:(m + 1) * 128, tok], c[:], writes=['COLS:%d:%d' % (m, n)])
            it += 1
    P.pop()


def cols_res(m, s):
    return ['COLS:%d:%d' % (m, n) for n in range(s * 4, s * 4 + 4)]


EXPH = 0.6065306597126334


SKIP = set()


def stage_rwkv(k, l, s):
    if 'rwkv' in SKIP:
        return
    P = k.P
    rp, mu = k.rp, k.mu
    psA, psB, psC, psD, psE, psF = k.psA, k.psB, k.psC, k.psD, k.psE, k.psF
    t0 = s * S
    P.push()
    w2a2b = P.sb('w2a2b', [128, 512], BF16)
    g2ab = P.sb('g2ab', [128, 512], BF16)
    g2bv = P.sb('g2bv', [64, 512], BF16)
    k.ldw(w2a2b[:], k.w2a2[l, :, :])
    k.ldw(g2ab[:], k.g2a[l, :, :])
    k.ldw(g2bv[:], k.g2bv2[l, :, :])
    KR = P.sb('KR', [128, 4, 32, 2, 64], BF16)
    BT = P.sb('BT', [128, 4, 2048], BF16)
    KT = P.sb('KT', [128, 4, 2048], BF16)
    VB = P.sb('VB', [128, 4, 2048], BF16)
    WC = P.sb('WC', [128, 4, 32], F32)
    P.push()
    ush = P.sb('ush', [128, 2049], F32)
    shd = P.sb('shd', [128, 2048], F32)
    lt = P.sb('lt', [128, 2048], F32)
    lwb = P.sb('lwb', [128, 2048], BF16)
    sg1 = P.sb('sg1', [128, 2048], BF16)
    lg2 = P.sb('lg2', [64, 2048], BF16)
    P.memset(ush[:, 0:1], 0.0)

    def shift_load(m, dst, rows=128):
        P.dma('sp', ush[0:rows, 1:2049], k.COLS[m * 128:m * 128 + rows, t0:t0 + S], reads=cols_res(m, s))
        P.tt(shd[0:rows, :], ush[0:rows, 0:2048], ush[0:rows, 1:2049], ALU.subtract)
        P.stt(dst, shd[0:rows, :], mu[0:rows, l, m:m + 1], ush[0:rows, 1:2049], ALU.mult, ALU.add)

    shift_load(16, lt[:, :])
    P.act(lwb[0:64, :], lt[0:64, :], AF.Tanh)
    P.copy(lwb[64:128, :], lt[64:128, :])
    shift_load(17, lt[:, :])
    P.act(sg1[:, :], lt[:, :], AF.Sigmoid)
    shift_load(18, lt[0:64, :], rows=64)
    P.act(lg2[0:32, :], lt[0:32, :], AF.Sigmoid)
    P.copy(lg2[32:64, :], lt[32:64, :])
    rj = P.sb('rj', [128, 2048], F32)
    kj = P.sb('kj', [128, 2048], F32)
    vj = P.sb('vj', [128, 2048], F32)
    names = ['sig', 'aa', 'gg', 'vm', 'vf', 'kk', 'rn', 't1', 'kp', 'be', 'cs', 'Wt', 'Wi', 'csm', 'Wp', 'bon']
    W_ = {nm: P.sb(nm, [128, 512], F32) for nm in names}
    kq = P.sb('kq', [128, 512], BF16)
    rk = P.sb('rk', [128, 512], BF16)
    c3 = lambda ap: ap.rearrange("p (c t) -> p c t", t=64)
    for j in range(4):
        jc = slice(j * 128, (j + 1) * 128)
        shift_load(4 + j, rj[:, :])
        shift_load(8 + j, kj[:, :])
        shift_load(12 + j, vj[:, :])
        for n in range(4):
            tk = slice(n * 512, (n + 1) * 512)
            gt = slice(t0 + n * 512, t0 + (n + 1) * 512)
            sig, aa, gg, vm, vf, kk, rn, t1, kp, be, cs, Wt, Wi, csm, Wp, bon = [W_[nm] for nm in names]
            P.mm(psD[:, :], lhsT=w2a2b[0:64, jc], rhs=lwb[0:64, tk])
            P.act(sig[:], psD[:, :], AF.Sigmoid, bias=rp[:, 0, l, j:j + 1])
            P.mm(psE[:, :], lhsT=w2a2b[64:128, jc], rhs=lwb[64:128, tk])
            P.act(aa[:], psE[:, :], AF.Sigmoid, bias=rp[:, 1, l, j:j + 1])
            P.mm(psF[:, :], lhsT=g2ab[:, jc], rhs=sg1[:, tk], start=True, stop=False)
            P.mm(psF[:, :], lhsT=g2bv[0:32, jc], rhs=lg2[0:32, tk], start=False, stop=True)
            P.copy(gg[:], psF[:, :])
            P.dma('sp', k.GS[jc, gt], gg[:])
            if l > 0:
                P.mm(psC[:, :], lhsT=g2bv[32:64, jc], rhs=lg2[32:64, tk])
                P.act(vm[:], psC[:, :], AF.Sigmoid, bias=k.v0s[:, l, j:j + 1])
                P.dma('sp', vf[:], k.VF[jc, gt])
                P.tt(vf[:], vf[:], vj[:, tk], ALU.subtract)
                P.tt(vf[:], vf[:], vm[:], ALU.mult)
                P.tt(vj[:, tk], vj[:, tk], vf[:], ALU.add)
            else:
                P.dma('sp', k.VF[jc, gt], vj[:, tk])
            P.copy(VB[:, j, tk], vj[:, tk], e='pool')
            P.ts(kk[:], kj[:, tk], rp[:, 2, l, j:j + 1], ALU.mult)
            P.act(kq[:], kk[:], AF.Square)
            P.mm(psD[:, :], lhsT=k.blkb[:, :], rhs=kq[:])
            P.act(rn[:], psD[:, :], AF.Sqrt)
            P.ts(rn[:], rn[:], 1e-12, ALU.max)
            P.recip(rn[:], rn[:])
            P.tt(kk[:], kk[:], rn[:], ALU.mult)
            P.ts(t1[:], aa[:], -1.0, ALU.add, rp[:, 3, l, j:j + 1], ALU.mult)
            P.stt(kp[:], t1[:], 1.0, kj[:, tk], ALU.add, ALU.mult)
            P.tt(be[:], kk[:], aa[:], ALU.mult, e='pool')
            P.op('dve', lambda eng, cs=cs, sig=sig: eng.tensor_tensor_scan(out=cs[:], data0=k.rmask[:], data1=sig[:], initial=0.0, op0=ALU.mult, op1=ALU.add),
                 reads=[k.rmask[:], sig[:]], writes=[cs[:]])
            P.act(Wt[:], cs[:], AF.Exp, scale=-EXPH)
            P.act(Wi[:], cs[:], AF.Exp, scale=EXPH)
            P.tt(csm[:], cs[:], sig[:], ALU.subtract, e='pool')
            P.act(Wp[:], csm[:], AF.Exp, scale=-EXPH)
            P.copy(WC[:, j, n * 8:(n + 1) * 8], c3(Wt[:])[:, :, 63])
            P.tt(KR[:, j, n * 8:(n + 1) * 8, 0, :], c3(kk[:]), c3(Wp[:]), ALU.mult)
            P.tt(KR[:, j, n * 8:(n + 1) * 8, 1, :], c3(rj[:, tk]), c3(Wt[:]), ALU.mult)
            P.tt(BT[:, j, tk], be[:], Wi[:], ALU.mult, e='pool')
            P.tt(KT[:, j, tk], kp[:], Wi[:], ALU.mult)
            P.stt(rk[:], rj[:, tk], rp[:, 6, l, j:j + 1], kp[:], ALU.mult, ALU.mult)
            P.mm(psE[:, :], lhsT=k.blkb[:, :], rhs=rk[:])
            P.tt(bon[:], psE[:, :], vj[:, tk], ALU.mult)
            P.dma('sp', k.BON[jc, gt], bon[:])
    P.pop()
    P.push()
    Mf = P.sb('Mf', [128, 4, 128], F32)
    Mb = P.sb('Mb', [128, 4, 128], BF16)
    Mt = P.sb('Mt', [128, 4, 128], F32)
    blk32 = P.sb('blk32', [128, 128], F32)
    P.copy(blk32[:], k.cst[:, 128:256])
    P.memset(Mf[:], 0.0)
    P.memset(Mb[:], 0.0)
    tok3 = P.sb('tok3', [64, 3, 512], BF16)
    m1 = P.sb('m1', [64, 8, 128], BF16)
    m2 = P.sb('m2', [64, 8, 128], BF16)
    am = P.sb('am', [64, 8, 64], BF16)
    xs = [P.sb('xs%d' % i, [64, 8, 64], BF16) for i in range(2)]
    zs = [P.sb('zs%d' % i, [64, 8, 64], BF16) for i in range(2)]
    pps = [P.sb('pp%d' % i, [64, 8, 64], BF16) for i in range(2)]
    nr = P.sb('nr', [64, 8, 64], BF16)
    ub = P.sb('ub', [64, 8, 64], BF16)
    yt8 = P.sb('yt8', [64, 8, 8, 64], F32)
    ysq = P.sb('ysq', [64, 64, 64], F32)
    st_ = {nm: P.sb(nm, [64, 64], F32) for nm in ('s1', 's2', 'mean', 'msq', 'var', 'rstd')}
    yfm = P.sb('yfm', [128, 4, 512], F32)
    ye = P.sb('ye', [128, 512], F32)
    bo2 = P.sb('bo2', [128, 512], F32)
    gg2 = P.sb('gg2', [128, 512], F32)
    yo16 = P.sb('yo16', [128, 512], BF16)
    pAb = psA[:, :].bitcast(BF16)
    h8 = lambda ap, w: ap.rearrange("p (h c) -> p h c", c=w)
    for c in range(0 if 'rec' in SKIP else 32):
        cs_ = slice(c * 64, (c + 1) * 64)
        for i, src in enumerate((VB, BT, KT)):
            for j in range(4):
                P.tr(pAb[0:64, i * 512 + j * 128:i * 512 + (j + 1) * 128], src[:, j, cs_], k.idb[:, :])
        P.act(tok3[:].rearrange("p a b -> p (a b)"), pAb[0:64, 0:1536], AF.Copy)
        Vt = tok3[:, 0, :]
        Bt = tok3[:, 1, :]
        Kt = tok3[:, 2, :]
        hp = lambda h: slice((h % 2) * 64, (h % 2) * 64 + 64)
        pos = lambda h: (h % 2) * 4 + h // 2
        for h in range(8):
            P.mm(psB[0:64, pos(h) * 128:(pos(h) + 1) * 128], lhsT=BT[hp(h), h // 2, cs_],
                 rhs=KR[hp(h), h // 2, c, :, :].rearrange("p a b -> p (a b)"))
        P.tt(m1[:], h8(psB[0:64, :], 128), bc(k.mk1[:].unsqueeze(1), [64, 8, 128]), ALU.mult)
        for h in range(8):
            P.mm(psA[0:64, pos(h) * 128:(pos(h) + 1) * 128], lhsT=KT[hp(h), h // 2, cs_],
                 rhs=KR[hp(h), h // 2, c, :, :].rearrange("p a b -> p (a b)"))
        P.tt(m2[:], h8(psA[0:64, :], 128), bc(k.mk1[:].unsqueeze(1), [64, 8, 128]), ALU.mult)
        for h in range(8):
            o0 = (h % 2) * 512 + (h // 2) * 64
            P.mm(psB[0:64, o0:o0 + 64], lhsT=KR[hp(h), h // 2, c, 0, :], rhs=BT[hp(h), h // 2, cs_])
        P.tt(am[:].rearrange("p (q j) c -> p q j c", q=2),
             psB[0:64, :].rearrange("p (q x) -> p q x", q=2)[:, :, 0:256].rearrange("p q (j c) -> p q j c", c=64),
             bc(k.mk3[:].unsqueeze(1).unsqueeze(1), [64, 2, 4, 64]), ALU.mult)
        P.tt(pps[0][:], bc(k.id64b[:].unsqueeze(1), [64, 8, 64]), m1[:, :, 0:64], ALU.subtract)
        X = m1[:, :, 0:64]
        Z = am[:]
        Pc = pps[0]
        for lev in range(1, 6):
            Xn = xs[lev % 2]
            Zn = zs[lev % 2]
            Pn = pps[lev % 2]
            if lev < 5:
                for h in range(8):
                    P.mm(psD[0:64, h * 64:(h + 1) * 64], lhsT=Z[:, h, :], rhs=X[:, h, :])
            for h in range(8):
                P.mm(psE[0:64, h * 64:(h + 1) * 64], lhsT=X[:, h, :], rhs=Z[:, h, :])
            if lev < 5:
                P.act(Xn[:], h8(psD[0:64, :], 64), AF.Copy)
            P.copy(Zn[:], h8(psE[0:64, :], 64))
            for h in range(8):
                P.mm(psF[0:64, h * 64:(h + 1) * 64], lhsT=Zn[:, h, :], rhs=Pc[:, h, :])
            P.tt(Pn[:], h8(psF[0:64, :], 64), Pc[:], ALU.add)
            X, Z, Pc = Xn[:], Zn[:], Pn
        TT = Pc
        if 'ph2' in SKIP:
            continue
        for j in range(4):
            jc = slice(j * 128, (j + 1) * 128)
            P.mm(psD[0:64, jc], lhsT=KR[:, j, c, 0, :], rhs=Mb[:, j, :], start=True, stop=False)
            for h in (2 * j, 2 * j + 1):
                P.mm(psD[0:64, h * 64:(h + 1) * 64], lhsT=m2[:, pos(h), 0:64], rhs=Vt[:, h * 64:(h + 1) * 64],
                     start=False, stop=(h == 2 * j + 1))
        P.act(nr[:], h8(psD[0:64, :], 64), AF.Copy, scale=-1.0)
        for h in range(8):
            P.mm(psE[0:64, h * 64:(h + 1) * 64], lhsT=TT[:, pos(h), :], rhs=nr[:, h, :])
        P.copy(ub[:], h8(psE[0:64, :], 64))
        ubf = ub[:].rearrange("p h c -> p (h c)")
        for j in range(4):
            jc = slice(j * 128, (j + 1) * 128)
            P.mm(psF[0:64, jc], lhsT=KR[:, j, c, 1, :], rhs=Mb[:, j, :], start=True, stop=False)
            for h in (2 * j, 2 * j + 1):
                o = psF[0:64, h * 64:(h + 1) * 64]
                P.mm(o, lhsT=m1[:, pos(h), 64:128], rhs=ub[:, h, :], start=False, stop=False)
                P.mm(o, lhsT=m2[:, pos(h), 64:128], rhs=Vt[:, h * 64:(h + 1) * 64], start=False, stop=(h == 2 * j + 1))
        P.act(yt8[:, c % 8, :, :], h8(psF[0:64, :], 64), AF.Copy)
        for j in range(4):
            jc = slice(j * 128, (j + 1) * 128)
            P.mm(psC[:, jc], lhsT=Bt[:, jc], rhs=ubf[:, jc], start=True, stop=False)
            P.mm(psC[:, jc], lhsT=Kt[:, jc], rhs=Vt[:, jc], start=False, stop=True)
        P.tt(Mt[:], h8(psC[:, :], 128), bc(blk32[:].unsqueeze(1), [128, 4, 128]), ALU.mult)
        P.tt(Mf[:], Mt[:], Mf[:], ALU.add)
        P.tt(Mf[:], Mf[:], bc(WC[:, :, c:c + 1], [128, 4, 128]), ALU.mult)
        P.copy(Mb[:], Mf[:], e='pool')
        if c % 8 == 7:
            s1, s2, mean, msq, var, rstd = [st_[nm] for nm in ('s1', 's2', 'mean', 'msq', 'var', 'rstd')]
            y3 = yt8[:].rearrange("p a h c -> p (a h) c")
            P.reduce(s1[:], y3, ALU.add)
            P.act(ysq[:], y3, AF.Square)
            P.reduce(s2[:], ysq[:], ALU.add)
            P.ts(mean[:], s1[:], 1.0 / 64, ALU.mult)
            P.tt(msq[:], mean[:], mean[:], ALU.mult)
            P.stt(var[:], s2[:], 1.0 / 64, msq[:], ALU.mult, ALU.subtract)
            P.act(rstd[:], var[:], AF.Sqrt, bias=k.epst[0:64, 1:2])
            P.recip(rstd[:], rstd[:])
            P.tt(y3, y3, bc(mean[:].unsqueeze(2), [64, 64, 64]), ALU.subtract)
            P.tt(y3, y3, bc(rstd[:].unsqueeze(2), [64, 64, 64]), ALU.mult)
            for half in range(2):
                for a in range(4):
                    ca = half * 4 + a
                    for j in range(4):
                        P.tr(psB[:, (a * 4 + j) * 64:(a * 4 + j + 1) * 64],
                             yt8[:, ca, 2 * j:2 * j + 2, :].rearrange("p h c -> p (h c)"), k.idf[0:64, 0:64])
                P.copy(yfm[:, :, half * 256:(half + 1) * 256].rearrange("p j (a t) -> p a j t", t=64),
                       psB[:, :].rearrange("p (a j t) -> p a j t", j=4, t=64))
        if c % 8 == 7:
            n = c // 8
            gt = slice(t0 + n * 512, t0 + (n + 1) * 512)
            for j in range(4):
                jc = slice(j * 128, (j + 1) * 128)
                P.ts(ye[:], yfm[:, j, :], rp[:, 4, l, j:j + 1], ALU.mult, rp[:, 5, l, j:j + 1], ALU.add)
                P.dma('sp', bo2[:], k.BON[jc, gt])
                P.dma('sp', gg2[:], k.GS[jc, gt])
                P.tt(ye[:], ye[:], bo2[:], ALU.add)
                P.tt(yo16[:], ye[:], gg2[:], ALU.mult)
                P.dma('sp', k.YMIX[512 + j * 128:512 + (j + 1) * 128, gt], yo16[:])
    P.pop()
    P.pop()


NIT = 13


def stage_dsa(k, l, s):
    if 'dsa' in SKIP:
        return
    P = k.P
    psA, psB, psC, psD, psE, psF = k.psA, k.psB, k.psC, k.psD, k.psE, k.psF
    t0 = s * S
    P.push()
    wq = P.sb('wq', [128, 2, 512], BF16)
    wqi = P.sb('wqi', [128, 2, 256], BF16)
    wk = P.sb('wk', [128, 4, 128], BF16)
    wv = P.sb('wv', [128, 8, 128], BF16)
    for kk in range(2):
        k.ldw(wq[:, kk, :], k.w_q[l, kk * 128:(kk + 1) * 128, :])
        k.ldw(wqi[:, kk, :], k.w_qi[l, kk * 128:(kk + 1) * 128, :])
    k.ldw(wk[:], k.wkT[l, :, :, :])
    k.ldw(wv[:], k.wvP[l, :, :, :])
    blkf = P.sb('blkf', [128, 128], F32)
    P.copy(blkf[:], k.cst[:, 128:256])
    cqb = P.sb('cqb', [128, 2, 2048], BF16)
    ckvT = P.sb('ckvT', [128, 2048], BF16)
    ckvTok = P.sb('ckvTok', [128, 16, 128], BF16)
    kix = P.sb('kix', [128, 2048], BF16)
    qab = P.sb('qab', [128, 8, 2048], BF16)
    qib = P.sb('qib', [128, 2, 2048], BF16)
    wht = P.sb('wht', [128, 16, 4], F32)
    pAb = psA[:, :].bitcast(BF16)
    P.push()
    cq32 = P.sb('cq32', [128, 2, 512], F32)
    sq = P.sb('sq', [128, 2, 512], BF16)
    rs = P.sb('rs', [128, 512], F32)
    kv32 = P.sb('kv32', [128, 512], F32)
    ki32 = P.sb('ki32', [128, 512], F32)
    sqf = P.sb('sqf', [128, 512], F32)
    mean = P.sb('mean', [128, 512], F32)
    msq = P.sb('msq', [128, 512], F32)
    var = P.sb('var', [128, 512], F32)
    wi = P.sb('wi', [4, 512], F32)
    qb = P.sb('qb', [128, 4, 512], BF16)
    for n in range(4):
        tk = slice(n * 512, (n + 1) * 512)
        gt = slice(t0 + n * 512, t0 + (n + 1) * 512)
        nn = s * 4 + n
        for kk in range(2):
            P.dma('sp', cq32[:, kk, :], k.COLS[kk * 128:(kk + 1) * 128, gt], reads=['COLS:%d:%d' % (kk, nn)])
        k.rstd_of(cq32[:], 2, 512, 1.0 / 256, 1e-6, sq, psC, rs)
        P.tt(cq32[:], cq32[:], bc(rs[:].unsqueeze(1), [128, 2, 512]), ALU.mult)
        for kk in range(2):
            P.act(cqb[:, kk, tk], cq32[:, kk, :], AF.Copy, scale=k.qg[:, l, kk:kk + 1])
        P.dma('sp', kv32[:], k.COLS[256:384, gt], reads=['COLS:2:%d' % nn])
        k.rstd_of(kv32[:].unsqueeze(1), 1, 512, 1.0 / 128, 1e-6, sq, psC, rs)
        P.tt(kv32[:], kv32[:], rs[:], ALU.mult)
        P.act(ckvT[:, tk], kv32[:], AF.Copy, scale=k.kg[:, l:l + 1])
        for i in range(4):
            P.tr(pAb[:, i * 128:(i + 1) * 128], ckvT[:, n * 512 + i * 128:n * 512 + (i + 1) * 128], k.idb[:, :])
        P.copy(ckvTok[:, n * 4:(n + 1) * 4, :], pAb[:, 0:512].rearrange("p (a b) -> p a b", b=128))
        P.dma('sp', ki32[0:64, :], k.COLS[384:448, gt], reads=['COLS:3:%d' % nn])
        P.dma('sp', ki32[64:128, :], k.COLS[384:448, gt], reads=['COLS:3:%d' % nn])
        P.mm(psD[:, :], lhsT=blkf[:, :], rhs=ki32[:, :])
        P.act(sqf[:], ki32[:], AF.Square)
        P.mm(psE[:, :], lhsT=blkf[:, :], rhs=sqf[:, :])
        P.ts(mean[:], psD[:, :], 1.0 / 64, ALU.mult)
        P.tt(msq[:], mean[:], mean[:], ALU.mult)
        P.stt(var[:], psE[:, :], 1.0 / 64, msq[:], ALU.mult, ALU.subtract)
        P.act(var[:], var[:], AF.Sqrt, bias=k.epst[:, 0:1])
        P.recip(var[:], var[:])
        P.tt(ki32[:], ki32[:], mean[:], ALU.subtract)
        P.tt(ki32[:], ki32[:], var[:], ALU.mult)
        P.act(kix[:, tk], ki32[:], AF.Identity, bias=k.kl[:, l, 1:2], scale=k.kl[:, l, 0:1])
        P.dma('sp', wi[:, :], k.COLS[448:452, gt], reads=['COLS:3:%d' % nn])
        for i in range(4):
            P.tr(psD[:, i * 4:(i + 1) * 4], wi[0:4, i * 128:(i + 1) * 128], k.idf[0:4, 0:4])
        P.ts(wht[:, n * 4:(n + 1) * 4, :], psD[:, 0:16].rearrange("p (a b) -> p a b", b=4), 0.0625, ALU.mult)
        for m in range(4):
            for kk in range(2):
                P.mm(psE[:, :], lhsT=wq[:, kk, m * 128:(m + 1) * 128], rhs=cqb[:, kk, tk], start=(kk == 0), stop=(kk == 1))
            P.copy(qb[:, m, :], psE[:, :])
        for h in range(8):
            hp = slice((h % 2) * 64, (h % 2) * 64 + 64)
            P.mm(psF[:, :], lhsT=wk[hp, h // 2, :], rhs=qb[hp, h // 2, :])
            P.act(qab[:, h, tk], psF[:, :], AF.Copy, scale=0.125)
        for m in range(2):
            for kk in range(2):
                P.mm(psE[:, :], lhsT=wqi[:, kk, m * 128:(m + 1) * 128], rhs=cqb[:, kk, tk], start=(kk == 0), stop=(kk == 1))
            P.copy(qib[:, m, tk], psE[:, :])
    P.pop()
    P.push()
    sc = P.sb('sc', [128, 2048], F32)
    tmp = P.sb('tmp', [128, 2048], F32)
    junk = P.sb('junk', [128, 2048], BF16)
    maskb = P.sb('maskb', [128, 2048], BF16)
    mT = P.sb('mT', [128, 2048], BF16)
    E = P.sb('E', [128, 1024], BF16)
    Pm = P.sb('Pm', [128, 1024], BF16)
    rd = P.sb('rd', [128, 1024], F32)
    olat = P.sb('olat', [128, 1024], BF16)
    yd = P.sb('yd', [128, 4, 128], BF16)
    sm = {nm: P.sb(nm, [128, 1], F32) for nm in ('hi', 'lo', 'w0', 'mid', 'cnt', 'gh')}
    Hs = P.sb('Hs', [128, 32], F32)
    mTs = [mT, P.sb('mT2', [128, 2048], BF16)]
    pEb = psE[:, :].bitcast(BF16)
    pFb = psF[:, :].bitcast(BF16)
    lo = sm['lo']

    def s_score(qt):
        q_ = slice(qt * 128, (qt + 1) * 128)
        N = (qt + 1) * 128
        ib = 0
        for hi in range(4):
            hp = slice((hi % 2) * 64, (hi % 2) * 64 + 64)
            for k0 in range(0, N, 512):
                cw = min(512, N - k0)
                pst = psE if ib % 2 == 0 else psF
                ib += 1
                P.mm(pst[:, 0:cw], lhsT=qib[hp, hi // 2, q_], rhs=kix[hp, k0:k0 + cw])
                dst = sc if hi == 0 else tmp
                P.ts(dst[:, k0:k0 + cw], pst[:, 0:cw], 0.0, ALU.max, wht[:, qt, hi:hi + 1], ALU.mult)
            if hi > 0:
                P.tt(sc[:, 0:N], sc[:, 0:N], tmp[:, 0:N], ALU.add)
        P.memset(sc[0:64, qt * 128 + 64:(qt + 1) * 128], NEG, e='dve')

    def s_select(qt):
        N = (qt + 1) * 128
        if qt >= 2:
            P.reduce(sm['hi'][:], sc[:, 0:N], ALU.max)
            P.reduce(lo[:], sc[:, 0:qt * 128 + 64], ALU.min)
            P.tt(sm['w0'][:], sm['hi'][:], lo[:], ALU.subtract)
            P.ts(Hs[:, 0:NIT + 1], k.cst[:, 448:448 + NIT + 1], sm['w0'][:, 0:1], ALU.mult)
            P.tt(lo[:], lo[:], Hs[:, 0:1], ALU.add)
            for i in range(NIT):
                P.ts(junk[:, 0:N], sc[:, 0:N], lo[:, 0:1], ALU.is_gt, None, ALU.add, accum=sm['cnt'][:])
                P.ts(sm['gh'][:], sm['cnt'][:], 255.5, ALU.is_gt, Hs[:, i:i + 1], ALU.mult)
                sub = Hs[:, i + 1:i + 2] if i < NIT - 1 else Hs[:, i:i + 1]
                P.stt(lo[:], sm['gh'][:], sub, lo[:], ALU.subtract, ALU.add)
        else:
            P.memset(lo[:], -1.0e29, e='dve')
        P.ts(maskb[:, 0:N], sc[:, 0:N], lo[:, 0:1], ALU.is_gt)
        mTq = mTs[qt % 2]
        for kt in range(qt + 1):
            pb_ = pEb if kt < 8 else pFb
            P.tr(pb_[:, (kt % 8) * 128:(kt % 8 + 1) * 128], maskb[:, kt * 128:(kt + 1) * 128], k.idb[:, :])
        P.copy(mTq[:, 0:min(N, 1024)], pEb[:, 0:min(N, 1024)])
        if N > 1024:
            P.copy(mTq[:, 1024:N], pFb[:, 0:N - 1024])

    def t_attn(qt):
        q_ = slice(qt * 128, (qt + 1) * 128)
        mTq = mTs[qt % 2]
        for kt in range(qt + 1):
            kk_ = slice(kt * 128, (kt + 1) * 128)
            for g in range(2):
                P.mm(psA[:, g * 512:(g + 1) * 512], lhsT=ckvT[:, kk_], rhs=qab[:, g * 4:(g + 1) * 4, q_])
            P.act(E[:, :], psA[:, :], AF.Exp)
            P.tt(Pm[:].rearrange("p (h q) -> p h q", q=128), E[:].rearrange("p (h q) -> p h q", q=128),
                 bc(mTq[:, kk_].unsqueeze(1), [128, 8, 128]), ALU.mult, e='pool')
            for g in range(2):
                P.mm(psB[:, g * 512:(g + 1) * 512], lhsT=ckvTok[:, kt, :], rhs=Pm[:, g * 512:(g + 1) * 512],
                     start=(kt == 0), stop=(kt == qt))
            P.mm(psC[:, :], lhsT=k.onesb[:, :], rhs=Pm[:, 0:512], start=(kt == 0), stop=(kt == qt))
            P.mm(psD[:, :], lhsT=k.onesb[:, :], rhs=Pm[:, 512:1024], start=(kt == 0), stop=(kt == qt))

    def t_fin(qt):
        P.recip(rd[:, 0:512], psC[:, :])
        P.recip(rd[:, 512:1024], psD[:, :])
        P.tt(olat[:, :], psB[:, :], rd[:, :], ALU.mult)
        for j in range(4):
            o = psA[:, j * 128:(j + 1) * 128]
            P.mm(o, lhsT=wv[:, 2 * j, :], rhs=olat[:, 2 * j * 128:(2 * j + 1) * 128], start=True, stop=False)
            P.mm(o, lhsT=wv[:, 2 * j + 1, :], rhs=olat[:, (2 * j + 1) * 128:(2 * j + 2) * 128], start=False, stop=True)
        P.act(yd[:], psA[:, 0:512].rearrange("p (j q) -> p j q", q=128), AF.Copy)
        P.dma('sp', k.YMIX[0:512, t0 + qt * 128:t0 + (qt + 1) * 128].rearrange("(j p) q -> p j q", p=128), yd[:])

    s_score(0)
    s_select(0)
    for qt in range(16):
        if qt + 1 < 16:
            s_score(qt + 1)
        t_attn(qt)
        if qt + 1 < 16:
            s_select(qt + 1)
        t_fin(qt)
    P.pop()
    P.pop()


def stage_out_ffn(k, l):
    if 'ffn' in SKIP:
        return
    P = k.P
    psC, psD, psE, psF = k.psC, k.psD, k.psE, k.psF
    P.push()
    wo = P.sb('wo', [128, 8, 1024], BF16)
    for kk in range(8):
        k.ldw(wo[:, kk, :], k.w_out[l, kk * 128:(kk + 1) * 128, :])
    ym = P.sb('ym', [128, 8, 512], BF16)
    yo = P.sb('yo', [128, 8, 512], F32)
    xt = P.sb('xt', [128, 8, 512], F32)
    sq = P.sb('sq', [128, 8, 512], BF16)
    rs = P.sb('rs', [128, 512], F32)
    hb = P.sb('hb', [128, 8, 512], BF16)
    it = 0
    for n in range(T // 512):
        b = n // 4
        tok = slice(n * 512, (n + 1) * 512)
        P.dma('sp', ym[:], k.YMIX[:, tok].rearrange("(k p) n -> p k n", p=128))
        P.dma('sp', xt[:], k.XT[:, tok].rearrange("(k p) n -> p k n", p=128))
        for m in range(8):
            ps = k.PSB[1 + (it % 3)]
            it += 1
            for kk in range(8):
                P.mm(ps[:, :], lhsT=wo[:, kk, m * 128:(m + 1) * 128], rhs=ym[:, kk, :], start=(kk == 0), stop=(kk == 7))
            if m % 2 == 0:
                P.act(yo[:, m, :], ps[:, :], AF.Copy)
            else:
                P.copy(yo[:, m, :], ps[:, :])
        k.rstd_of(yo[:], 8, 512, 1.0 / D, 1e-6, sq, psC, rs)
        P.tt(yo[:], yo[:], bc(rs[:].unsqueeze(1), [128, 8, 512]), ALU.mult)
        for m in range(8):
            P.stt(xt[:, m, :], yo[:, m, :], k.G1[:, l, m, b:b + 1], xt[:, m, :], ALU.mult, ALU.add)
        P.dma('sp', k.XT[:, tok].rearrange("(k p) n -> p k n", p=128), xt[:])
        k.rstd_of(xt[:], 8, 512, 1.0 / D, 1e-6, sq, psC, rs)
        P.tt(yo[:], xt[:], bc(rs[:].unsqueeze(1), [128, 8, 512]), ALU.mult)
        for kk in range(8):
            P.act(hb[:, kk, :], yo[:, kk, :], AF.Identity, bias=k.mod[:, l, 24 + kk, b:b + 1], scale=k.A2[:, l, kk, b:b + 1])
        P.dma('sp', k.H2[:, tok].rearrange("(k p) n -> p k n", p=128), hb[:])
    P.pop()
    P.push()
    wg = [P.sb('wg%d' % i, [128, 8, 512], BF16) for i in range(2)]
    wu = [P.sb('wu%d' % i, [128, 8, 512], BF16) for i in range(2)]
    h2 = [P.sb('h2%d' % i, [128, 8, 512], BF16) for i in range(2)]
    sgl = [P.sb('sgl%d' % i, [128, 512], F32) for i in range(2)]
    u16 = [P.sb('u16%d' % i, [128, 512], BF16) for i in range(2)]
    def load_w(grp):
        nch = min(4, 22 - grp * 4)
        g_, u_ = wg[grp % 2], wu[grp % 2]
        for kk in range(8):
            k.ldw(g_[:, kk, 0:nch * 128], k.w_fc[l, kk * 128:(kk + 1) * 128, grp * 512:grp * 512 + nch * 128])
            k.ldw(u_[:, kk, 0:nch * 128], k.w_fc[l, kk * 128:(kk + 1) * 128, DFF + grp * 512:DFF + grp * 512 + nch * 128])

    iters = [(grp, n) for grp in range(6) for n in range(T // 512)]

    def load_h(i):
        n = iters[i][1]
        P.dma('sp', h2[i % 2][:], k.H2[:, n * 512:(n + 1) * 512].rearrange("(k p) n -> p k n", p=128))

    load_w(0)
    load_h(0)
    it = 0
    for i, (grp, n) in enumerate(iters):
        nch = min(4, 22 - grp * 4)
        g_, u_ = wg[grp % 2], wu[grp % 2]
        if n == 0 and grp + 1 < 6:
            load_w(grp + 1)
        if i + 1 < len(iters):
            load_h(i + 1)
        tok = slice(n * 512, (n + 1) * 512)
        hh = h2[i % 2]
        for c in range(nch):
            pg = psC if it % 2 == 0 else psE
            pu = psD if it % 2 == 0 else psF
            sg_, uu = sgl[it % 2], u16[it % 2]
            it += 1
            for kk in range(8):
                P.mm(pg[:, :], lhsT=g_[:, kk, c * 128:(c + 1) * 128], rhs=hh[:, kk, :], start=(kk == 0), stop=(kk == 7))
            for kk in range(8):
                P.mm(pu[:, :], lhsT=u_[:, kk, c * 128:(c + 1) * 128], rhs=hh[:, kk, :], start=(kk == 0), stop=(kk == 7))
            P.act(sg_[:], pg[:, :], AF.Silu)
            P.tt(uu[:], sg_[:], pu[:, :], ALU.mult)
            r0 = (grp * 4 + c) * 128
            P.dma('sp', k.UT[r0:r0 + 128, tok], uu[:], writes=['UT:%d:%d' % (grp * 4 + c, n)])
    P.pop()
    P.push()
    wd = P.sb('wd', [128, 22, 1024], BF16)
    for kk in range(22):
        k.ldw(wd[:, kk, :], k.w_down[l, kk * 128:(kk + 1) * 128, :])
    uts = [P.sb('ut%d' % i, [128, 22, 512], BF16) for i in range(2)]
    xts = [P.sb('xtd%d' % i, [128, 8, 512], F32) for i in range(2)]
    yo = P.sb('yo', [128, 8, 512], F32)
    sq = P.sb('sq', [128, 8, 512], BF16)
    rs = P.sb('rs', [128, 512], F32)

    def load_t(n):
        tok = slice(n * 512, (n + 1) * 512)
        P.dma('sp', uts[n % 2][:], k.UT[:, tok].rearrange("(k p) n -> p k n", p=128), reads=['UT:%d:%d' % (c, n) for c in range(22)])
        P.dma('sp', xts[n % 2][:], k.XT[:, tok].rearrange("(k p) n -> p k n", p=128), reads=['XT:%d' % n])

    load_t(0)
    it = 0
    for n in range(T // 512):
        b = n // 4
        tok = slice(n * 512, (n + 1) * 512)
        ut, xt = uts[n % 2], xts[n % 2]
        if n + 1 < T // 512:
            load_t(n + 1)
        for m in range(8):
            ps = k.PSB[1 + (it % 3)]
            it += 1
            for kk in range(22):
                P.mm(ps[:, :], lhsT=wd[:, kk, m * 128:(m + 1) * 128], rhs=ut[:, kk, :], start=(kk == 0), stop=(kk == 21))
            if m % 2 == 0:
                P.act(yo[:, m, :], ps[:, :], AF.Copy)
            else:
                P.copy(yo[:, m, :], ps[:, :])
        k.rstd_of(yo[:], 8, 512, 1.0 / D, 1e-6, sq, psC, rs)
        P.tt(yo[:], yo[:], bc(rs[:].unsqueeze(1), [128, 8, 512]), ALU.mult)
        for m in range(8):
            P.stt(xt[:, m, :], yo[:, m, :], k.G2[:, l, m, b:b + 1], xt[:, m, :], ALU.mult, ALU.add)
        P.dma('sp', k.XT[:, tok].rearrange("(k p) n -> p k n", p=128), xt[:], writes=['XT:%d' % n])
    P.pop()


def prep_shared(inp):
    f = np.float32
    sh = {}
    sh['ada_w'] = np.ascontiguousarray(inp['ada_w'], f)
    sh['ada_bT'] = np.ascontiguousarray(inp['ada_b'].reshape(L, 48, 128).transpose(2, 0, 1), f)
    g = np.stack([inp['pre_g_mix'], inp['post_g_mix'], inp['pre_g_ffn'], inp['post_g_ffn']], 0)
    sh['gains'] = np.ascontiguousarray(g.reshape(4, L, 8, 128).transpose(3, 0, 1, 2), f)
    W = np.zeros((L, D, PC), f)
    M = np.zeros((L, PC), f)
    wi = inp['w_in']
    ms = inp['mu_shift']
    W[:, :, 0:452] = wi[:, :, 0:452]
    W[:, :, 512:2048] = wi[:, :, 452:1988]
    M[:, 512:2048] = ms[:, 0:1536]
    W[:, :, 2048:2176] = wi[:, :, 1988:2116]
    M[:, 2048:2176] = ms[:, 1536:1664]
    W[:, :, 2176:2336] = wi[:, :, 2116:2276]
    M[:, 2176:2336] = ms[:, 1664:1824]
    W[1:, :, 2336:2368] = inp['w_in_vres']
    M[1:, 2336:2368] = inp['mu_vres']
    sh['w_in'] = W
    sh['muT'] = np.ascontiguousarray(M.reshape(L, 19, 128).transpose(2, 0, 1), f)
    sh['w_out'] = np.ascontiguousarray(inp['w_out'], f)
    sh['qng'] = np.ascontiguousarray(inp['q_norm_g'].reshape(L, 2, 128).transpose(2, 0, 1), f)
    sh['kvg'] = np.ascontiguousarray(inp['kv_norm_g'].T, f)
    sh['w_q'] = np.ascontiguousarray(inp['w_q_up'].reshape(L, 256, 512), f)
    sh['w_qi'] = np.ascontiguousarray(inp['w_qi_up'].reshape(L, 256, 256), f)
    wk = inp['w_k_up'].reshape(L, 128, 4, 2, 64)
    sh['wkT'] = np.ascontiguousarray(wk.transpose(0, 3, 4, 2, 1).reshape(L, 128, 4, 128), f)
    wv = np.zeros((L, 128, 8, 128), f)
    for h in range(8):
        wv[:, :, h, (h % 2) * 64:(h % 2) * 64 + 64] = inp['w_v_up'][:, :, h, :]
    sh['wvP'] = wv
    kl = np.stack([inp['kidx_ln_g'], inp['kidx_ln_b']], -1)
    kl = np.concatenate([kl, kl], 1)
    sh['kiln'] = np.ascontiguousarray(kl.transpose(1, 0, 2), f)
    rw = np.stack([inp['w0'], inp['a0'], inp['k_k'], inp['k_a'], inp['lnx_g'], inp['lnx_b'],
                   inp['r_k'].reshape(L, 512)], 0)
    sh['rwp'] = np.ascontiguousarray(rw.reshape(7, L, 4, 128).transpose(3, 0, 1, 2), f)
    v0 = np.zeros((L, 512), f)
    v0[1:] = inp['v0']
    sh['v0T'] = np.ascontiguousarray(v0.reshape(L, 4, 128).transpose(2, 0, 1), f)
    sh['w2a2'] = np.ascontiguousarray(np.concatenate([inp['w2'], inp['a2']], 1), f)
    sh['g2a'] = np.ascontiguousarray(inp['g2'][:, 0:128], f)
    gb = np.zeros((L, 64, 512), f)
    gb[:, 0:32] = inp['g2'][:, 128:160]
    gb[1:, 32:64] = inp['v2']
    sh['g2bv2'] = gb
    sh['w_fc'] = np.ascontiguousarray(inp['w_fc'], f)
    sh['w_down'] = np.ascontiguousarray(inp['w_down'], f)
    c = np.zeros((128, 1024), f)
    c[:, 0:128] = np.eye(128)
    c[0:64, 128:192] = 1.0
    c[64:128, 192:256] = 1.0
    si = np.arange(64)[:, None]
    ti = np.arange(64)[None, :]
    c[0:64, 256:320] = (si < ti)
    c[0:64, 320:384] = (si <= ti)
    c[0:64, 384:448] = (si > ti)
    c[:, 448:480] = 2.0 ** -(np.arange(32) + 1.0)
    sh['consts'] = c
    return sh


def prep_core(inp, ci):
    f = np.float32
    xb = np.asarray(inp['x'][ci * NB:(ci + 1) * NB], f).reshape(T, D)
    cb = np.asarray(inp['c'][ci * NB:(ci + 1) * NB], f)
    return {'xT': np.ascontiguousarray(xb.T),
            'cT': np.ascontiguousarray(cb.reshape(NB, 8, 128).transpose(2, 1, 0))}


_NC = None


def kernel(**inputs):
    global _NC
    inp = {k_: np.asarray(v) for k_, v in inputs.items()}
    if _NC is None:
        _NC = build()
    sh = prep_shared(inp)
    ncores = 8
    in_maps = []
    for ci in range(ncores):
        m = dict(sh)
        m.update(prep_core(inp, ci))
        in_maps.append(m)
    res = run_bass_kernel_spmd(_NC, in_maps, core_ids=list(range(ncores)))
    out = np.empty((16, S, D), np.float32)
    for ci in range(ncores):
        o = np.asarray(res.results[ci]['outT'])
        out[ci * NB:(ci + 1) * NB] = o.T.reshape(NB, S, D)
    return out
```

```python
import contextlib
import numpy as np
import concourse.bass as bass
import concourse.mybir as mybir
from concourse.bass_utils import run_bass_kernel_spmd
from concourse.alu_op_type import AluOpType as ALU

F32 = mybir.dt.float32
BF16 = mybir.dt.bfloat16
AF = mybir.ActivationFunctionType
AX = mybir.AxisListType

N_DMA_SLOTS = 32
D = 1024
L = 4
S = 2048
NB = 2
T = NB * S
PC = 2432
DFF = 2816
NEG = -1.0e30


class Prog:
    ENGS = ('pe', 'dve', 'act', 'pool', 'sp')

    def __init__(self, nc, stack):
        self.nc = nc
        self.stack = stack
        self.q = {e: [] for e in self.ENGS}
        self.cnt = {e: 0 for e in self.ENGS}
        self.waited = {e: {} for e in self.ENGS}
        self.lastw = {}
        self.readers = {}
        self.sems = {}
        for e in ('pe', 'dve', 'act', 'pool'):
            self.sems[e] = stack.enter_context(nc.semaphore('s_' + e))
        self.slot_cnt = []
        for i in range(N_DMA_SLOTS):
            self.sems['d%d' % i] = stack.enter_context(nc.semaphore('s_d%d' % i))
            self.slot_cnt.append(0)
        self.rr = 0
        self.n_ops = 0
        self.tid = 0
        self.scopes = []

    def push(self):
        st = contextlib.ExitStack()
        st.__enter__()
        self.scopes.append(st)

    def pop(self):
        self.barrier()
        self.scopes.pop().__exit__(None, None, None)

    def _st(self):
        return self.scopes[-1] if self.scopes else self.stack

    def sb(self, name, shape, dt):
        self.tid += 1
        return self._st().enter_context(self.nc.sbuf_tensor('%s_%d' % (name, self.tid), list(shape), dt))

    def ps(self, name, shape, dt):
        return self.stack.enter_context(self.nc.psum_tensor(name, list(shape), dt))

    @staticmethod
    def _res(items):
        out = []
        for it in items:
            if it is None:
                continue
            if isinstance(it, str):
                out.append(it)
            elif isinstance(it, (int, float)):
                continue
            else:
                out.append(it.tensor.name)
        return out

    def _deps(self, e, reads, writes):
        deps = []
        for r in reads:
            ev = self.lastw.get(r)
            if ev is not None:
                deps.append(ev)
        for w in writes:
            ev = self.lastw.get(w)
            if ev is not None:
                deps.append(ev)
            deps.extend(self.readers.get(w, ()))
        need = {}
        for (sk, v) in deps:
            if sk == e and e == 'pe':
                continue
            if self.waited[e].get(sk, 0) >= v:
                continue
            if need.get(sk, 0) < v:
                need[sk] = v
        return need

    def _emit_waits(self, e, need):
        for sk, v in need.items():
            self.waited[e][sk] = v
            self.q[e].append(('w', self.sems[sk], v))

    def _record(self, ev, reads, writes):
        for r in reads:
            self.readers.setdefault(r, []).append(ev)
        for w in writes:
            self.lastw[w] = ev
            self.readers[w] = []

    def op(self, e, fn, reads=(), writes=()):
        reads = self._res(reads)
        writes = self._res(writes)
        need = self._deps(e, reads, writes)
        self._emit_waits(e, need)
        self.cnt[e] += 1
        ev = (e, self.cnt[e])
        self.q[e].append(('i', fn, self.sems[e], 1))
        self._record(ev, reads, writes)
        self.n_ops += 1

    def dma(self, e, out, in_, reads=None, writes=None):
        reads = self._res(reads if reads is not None else [in_])
        writes = self._res(writes if writes is not None else [out])
        need = self._deps(e, reads, writes)
        slot = self.rr
        self.rr = (self.rr + 1) % N_DMA_SLOTS
        sk = 'd%d' % slot
        prev = self.slot_cnt[slot]
        if prev > 0 and self.waited[e].get(sk, 0) < prev and need.get(sk, 0) < prev:
            need[sk] = prev
        self._emit_waits(e, need)
        self.slot_cnt[slot] = prev + 16
        ev = (sk, prev + 16)
        self.q[e].append(('i', lambda eng: eng.dma_start(out=out, in_=in_), self.sems[sk], 16))
        self._record(ev, reads, writes)
        self.n_ops += 1

    def barrier(self):
        evs = {}
        for e in ('pe', 'dve', 'act', 'pool'):
            if self.cnt[e] > 0:
                evs[e] = self.cnt[e]
        for i, c in enumerate(self.slot_cnt):
            if c > 0:
                evs['d%d' % i] = c
        for e in self.ENGS:
            need = {}
            for sk, v in evs.items():
                if sk == e and e == 'pe':
                    continue
                if self.waited[e].get(sk, 0) < v:
                    need[sk] = v
            self._emit_waits(e, need)
        self.lastw = {}
        self.readers = {}

    def emit(self):
        nc = self.nc
        q = self.q
        with nc.Block() as block:
            def run(eng, items):
                for it in items:
                    if it[0] == 'w':
                        eng.wait_ge(it[1], it[2])
                    else:
                        it[1](eng).then_inc(it[2], it[3])

            @block.tensor
            def _(eng):
                run(eng, q['pe'])

            @block.vector
            def _(eng):
                run(eng, q['dve'])

            @block.scalar
            def _(eng):
                run(eng, q['act'])

            @block.gpsimd
            def _(eng):
                run(eng, q['pool'])

            @block.sync
            def _(eng):
                run(eng, q['sp'])

    def mm(self, out, lhsT, rhs, start=True, stop=True):
        self.op('pe', lambda e: e.matmul(out, lhsT=lhsT, rhs=rhs, start=start, stop=stop),
                reads=[lhsT, rhs], writes=[out])

    def tr(self, out, in_, ident):
        self.op('pe', lambda e: e.transpose(out=out, in_=in_, identity=ident),
                reads=[in_, ident], writes=[out])

    def act(self, out, in_, func, bias=None, scale=None, e='act'):
        kw = {}
        if bias is not None:
            kw['bias'] = bias
        if scale is not None:
            kw['scale'] = scale
        self.op('act', lambda eng: eng.activation(out=out, in_=in_, func=func, **kw),
                reads=[in_, bias, scale], writes=[out])

    def tt(self, out, in0, in1, op, e='dve'):
        self.op(e, lambda eng: eng.tensor_tensor(out=out, in0=in0, in1=in1, op=op),
                reads=[in0, in1], writes=[out])

    def ts(self, out, in0, s1, op0, s2=None, op1=None, e='dve', accum=None):
        kw = {}
        if op1 is not None:
            kw['op1'] = op1
        if accum is not None:
            kw['accum_out'] = accum
        self.op(e, lambda eng: eng.tensor_scalar(out=out, in0=in0, scalar1=s1, scalar2=s2, op0=op0, **kw),
                reads=[in0, s1, s2], writes=[out, accum])

    def stt(self, out, in0, scalar, in1, op0, op1):
        self.op('dve', lambda eng: eng.scalar_tensor_tensor(out=out, in0=in0, scalar=scalar, in1=in1, op0=op0, op1=op1),
                reads=[in0, scalar, in1], writes=[out])

    def copy(self, out, in_, e='dve'):
        self.op(e, lambda eng: eng.tensor_copy(out=out, in_=in_), reads=[in_], writes=[out])

    def memset(self, ap, val, e='pool'):
        self.op(e, lambda eng: eng.memset(ap, val), writes=[ap])

    def recip(self, out, in_):
        self.op('dve', lambda eng: eng.reciprocal(out=out, in_=in_), reads=[in_], writes=[out])

    def reduce(self, out, in_, op, axis=AX.X):
        self.op('dve', lambda eng: eng.tensor_reduce(out=out, in_=in_, axis=axis, op=op), reads=[in_], writes=[out])


def bc(ap, shape):
    return ap.to_broadcast(list(shape))


class K:
    pass


def build(n_layers=L, dbg=()):
    nc = bass.Bass("TRN2", target_bir_lowering=False)
    k = K()
    k.nc = nc

    def din(name, shape, dt=F32):
        return nc.dram_tensor(name, list(shape), dt, kind="ExternalInput").ap()

    def dscr(name, shape, dt):
        return nc.dram_tensor(name, list(shape), dt, kind="Internal").ap()

    xT = din('xT', [D, T])
    cT = din('cT', [128, 8, NB])
    ada_w = din('ada_w', [L, D, 6 * D])
    ada_bT = din('ada_bT', [128, L, 48])
    gains = din('gains', [128, 4, L, 8])
    w_in = din('w_in', [L, D, PC])
    muT = din('muT', [128, L, 19])
    w_out = din('w_out', [L, D, D])
    qng = din('qng', [128, L, 2])
    kvg = din('kvg', [128, L])
    w_q = din('w_q', [L, 256, 512])
    w_qi = din('w_qi', [L, 256, 256])
    wkT = din('wkT', [L, 128, 4, 128])
    wvP = din('wvP', [L, 128, 8, 128])
    kiln = din('kiln', [128, L, 2])
    rwp = din('rwp', [128, 7, L, 4])
    v0T = din('v0T', [128, L, 4])
    w2a2 = din('w2a2', [L, 128, 512])
    g2a = din('g2a', [L, 128, 512])
    g2bv2 = din('g2bv2', [L, 64, 512])
    w_fc = din('w_fc', [L, D, 2 * DFF])
    w_down = din('w_down', [L, DFF, D])
    consts = din('consts', [128, 1024])
    outT = nc.dram_tensor('outT', [D, T], F32, kind="ExternalOutput").ap()

    XT = dscr('XT', [D, T], F32)
    COLS = dscr('COLS', [PC, T], F32)
    YMIX = dscr('YMIX', [D, T], BF16)
    VF = dscr('VF', [512, T], F32)
    GS = dscr('GS', [512, T], F32)
    BON = dscr('BON', [512, T], F32)
    H2 = dscr('H2', [D, T], BF16)
    UT = dscr('UT', [DFF, T], BF16)
    dbg_out = {}
    for nm, shp in dbg:
        dbg_out[nm] = nc.dram_tensor('dbg_' + nm, list(shp), F32, kind="ExternalOutput").ap()

    with contextlib.ExitStack() as st:
        P = Prog(nc, st)
        k.P = P
        psA = P.ps('psA', [128, 1024], F32)
        psB = P.ps('psB', [128, 1024], F32)
        psC = P.ps('psC', [128, 512], F32)
        psD = P.ps('psD', [128, 512], F32)
        psE = P.ps('psE', [128, 512], F32)
        psF = P.ps('psF', [128, 512], F32)
        cst = P.sb('cst', [128, 1024], F32)
        P.dma('sp', cst[:], consts[:, :])
        idb = P.sb('idb', [128, 128], BF16)
        idf = P.sb('idf', [128, 128], F32)
        onesb = P.sb('onesb', [128, 128], BF16)
        blkb = P.sb('blkb', [128, 128], BF16)
        P.copy(idb[:], cst[:, 0:128])
        P.copy(idf[:], cst[:, 0:128])
        P.memset(onesb[:], 1.0)
        P.copy(blkb[:], cst[:, 128:256])
        mk1 = P.sb('mk1', [64, 128], F32)
        mk3 = P.sb('mk3', [64, 64], F32)
        id64b = P.sb('id64b', [64, 64], BF16)
        rmask = P.sb('rmask', [128, 512], F32)
        P.copy(mk1[:], cst[0:64, 256:384])
        P.copy(mk3[:], cst[0:64, 384:448])
        P.copy(id64b[:], cst[0:64, 0:64])
        P.memset(rmask[:], 1.0)
        P.memset(rmask[:].rearrange("p (c t) -> p c t", t=64)[:, :, 0:1], 0.0)
        pow2 = cst[:, 448:480]
        gn = P.sb('gn', [128, 4, L, 8], F32)
        P.dma('sp', gn[:], gains[:, :, :, :])
        abT = P.sb('abT', [128, L, 48], F32)
        P.dma('sp', abT[:], ada_bT[:, :, :])
        mu = P.sb('mu', [128, L, 19], F32)
        P.dma('sp', mu[:], muT[:, :, :])
        qg = P.sb('qg', [128, L, 2], F32)
        P.dma('sp', qg[:], qng[:, :, :])
        kg = P.sb('kg', [128, L], F32)
        P.dma('sp', kg[:], kvg[:, :])
        kl = P.sb('kl', [128, L, 2], F32)
        P.dma('sp', kl[:], kiln[:, :, :])
        rp = P.sb('rp', [128, 7, L, 4], F32)
        P.dma('sp', rp[:], rwp[:, :, :, :])
        v0s = P.sb('v0s', [128, L, 4], F32)
        P.dma('sp', v0s[:], v0T[:, :, :])
        mod = P.sb('mod', [128, L, 48, NB], F32)
        A1 = P.sb('A1', [128, L, 8, NB], F32)
        A2 = P.sb('A2', [128, L, 8, NB], F32)
        G1 = P.sb('G1', [128, L, 8, NB], F32)
        G2 = P.sb('G2', [128, L, 8, NB], F32)

        PSB = [psC, psD, psE, psF]

        P.push()
        ct = P.sb('ct', [128, 8, NB], F32)
        cond = P.sb('cond', [128, 8, NB], F32)
        P.dma('sp', ct[:], cT[:, :, :])
        P.act(cond[:], ct[:], AF.Silu)
        wa = [P.sb('wa%d' % i, [128, 8, 768], F32) for i in range(2)]
        it = 0
        for l in range(n_layers):
            for grp in range(8):
                w = wa[it % 2]
                ps = PSB[it % 2]
                it += 1
                for kk in range(8):
                    P.dma('sp', w[:, kk, :], ada_w[l, kk * 128:(kk + 1) * 128, grp * 768:(grp + 1) * 768])
                for mi in range(6):
                    for kk in range(8):
                        P.mm(ps[:, mi * 2:(mi + 1) * 2], lhsT=w[:, kk, mi * 128:(mi + 1) * 128], rhs=cond[:, kk, :],
                             start=(kk == 0), stop=(kk == 7))
                P.tt(mod[:, l, grp * 6:(grp + 1) * 6, :], ps[:, 0:12].rearrange("p (m b) -> p m b", b=NB),
                     bc(abT[:, l, grp * 6:(grp + 1) * 6].unsqueeze(2), [128, 6, NB]), ALU.add)
            for (dst, gi, sc0) in ((A1, 0, 8), (A2, 2, 32)):
                P.stt(dst[:, l, :, :], mod[:, l, sc0:sc0 + 8, :], 1.0,
                      bc(gn[:, gi, l, :].unsqueeze(2), [128, 8, NB]), ALU.add, ALU.mult)
            for (dst, gi, g0) in ((G1, 1, 16), (G2, 3, 40)):
                P.tt(dst[:, l, :, :], mod[:, l, g0:g0 + 8, :], bc(gn[:, gi, l, :].unsqueeze(2), [128, 8, NB]), ALU.mult)
        P.pop()

        P.push()
        xc = [P.sb('xc%d' % i, [128, 2048], F32) for i in range(2)]
        it = 0
        for kk in range(8):
            for hf in range(2):
                t_ = xc[it % 2]
                it += 1
                P.dma('sp', t_[:], xT[kk * 128:(kk + 1) * 128, hf * 2048:(hf + 1) * 2048])
                P.dma('sp', XT[kk * 128:(kk + 1) * 128, hf * 2048:(hf + 1) * 2048], t_[:])
        P.pop()

        def rstd_of(x, K_, N, inv_dim, eps, sq, ps, rs, ones=None, np_=128):
            P.act(sq[0:np_, 0:K_, 0:N], x, AF.Square)
            for kk in range(K_):
                P.mm(ps[:, 0:N], lhsT=(ones if ones is not None else onesb[0:np_, :]), rhs=sq[0:np_, kk, 0:N],
                     start=(kk == 0), stop=(kk == K_ - 1))
            P.act(rs[:, 0:N], ps[:, 0:N], AF.Sqrt, bias=eps_ap(eps), scale=inv_dim)
            P.recip(rs[:, 0:N], rs[:, 0:N])

        epst = P.sb('epst', [128, 4], F32)
        P.memset(epst[:, 0:1], 1e-6)
        P.memset(epst[:, 1:2], 64e-5)
        P.memset(epst[:, 2:3], 0.0)

        def eps_ap(eps):
            if eps == 1e-6:
                return epst[:, 0:1]
            if eps == 64e-5:
                return epst[:, 1:2]
            return epst[:, 2:3]

        stg = [P.sb('stg%d' % i, [128, 1024], F32) for i in range(2)]
        stg_i = [0]

        def ldw(dst, src):
            if len(dst.shape) == 3:
                dst = dst.rearrange("p a b -> p (a b)")
            if len(src.shape) == 3:
                src = src.rearrange("p a b -> p (a b)")
            rows, cols = dst.shape[0], dst.shape[1]
            for c0 in range(0, cols, 1024):
                cw = min(1024, cols - c0)
                t_ = stg[stg_i[0] % 2]
                stg_i[0] += 1
                P.dma('sp', t_[0:rows, 0:cw], src[:, c0:c0 + cw])
                P.copy(dst[:, c0:c0 + cw], t_[0:rows, 0:cw], e='pool')

        k.__dict__.update(locals())
        for l in range(n_layers):
            stage_proj(k, l)
            if 'cols' in dbg_out and l == 0:
                dump(k, COLS, dbg_out['cols'], PC)
            for s in range(NB):
                stage_rwkv(k, l, s)
                stage_dsa(k, l, s)
            if 'ymix' in dbg_out and l == 0:
                dump(k, YMIX, dbg_out['ymix'], D, bf=True)
            stage_out_ffn(k, l)
            if 'xl0' in dbg_out and l == 0:
                dump(k, XT, dbg_out['xl0'], D)
        P.push()
        xc = [P.sb('xo%d' % i, [128, 2048], F32) for i in range(2)]
        it = 0
        for kk in range(8):
            for hf in range(2):
                t_ = xc[it % 2]
                it += 1
                P.dma('sp', t_[:], XT[kk * 128:(kk + 1) * 128, hf * 2048:(hf + 1) * 2048])
                P.dma('sp', outT[kk * 128:(kk + 1) * 128, hf * 2048:(hf + 1) * 2048], t_[:])
        P.pop()
        P.emit()
    return nc


def dump(k, src, dst, rows, bf=False):
    P = k.P
    P.push()
    nchunk = (rows + 127) // 128
    bufs = [P.sb('dmp%d' % i, [128, 2048], BF16 if bf else F32) for i in range(2)]
    bufs2 = [P.sb('dmq%d' % i, [128, 2048], F32) for i in range(2)] if bf else None
    it = 0
    for m in range(nchunk):
        r = min(128, rows - m * 128)
        for hf in range(T // 2048):
            b = bufs[it % 2]
            P.dma('sp', b[0:r, :], src[m * 128:m * 128 + r, hf * 2048:(hf + 1) * 2048])
            if bf:
                b2 = bufs2[it % 2]
                P.copy(b2[0:r, :], b[0:r, :])
                b = b2
            P.dma('sp', dst[m * 128:m * 128 + r, hf * 2048:(hf + 1) * 2048], b[0:r, :])
            it += 1
    P.pop()


def stage_proj(k, l):
    P = k.P
    P.push()
    win = P.sb('win', [128, 8, PC], BF16)
    for kk in range(8):
        k.ldw(win[:, kk, :], k.w_in[l, kk * 128:(kk + 1) * 128, :])
    xt = P.sb('xt', [128, 8, 512], F32)
    sq = P.sb('sq', [128, 8, 512], BF16)
    rs = P.sb('rs', [128, 512], F32)
    hb = P.sb('hb', [128, 8, 512], BF16)
    co = [P.sb('co%d' % i, [128, 512], F32) for i in range(4)]
    it = 0
    for n in range(T // 512):
        b = n // 4
        tok = slice(n * 512, (n + 1) * 512)
        P.dma('sp', xt[:], k.XT[:, tok].rearrange("(k p) n -> p k n", p=128))
        k.rstd_of(xt[:], 8, 512, 1.0 / D, 1e-6, sq, k.psC, rs)
        P.tt(xt[:], xt[:], bc(rs[:].unsqueeze(1), [128, 8, 512]), ALU.mult)
        for kk in range(8):
            P.act(hb[:, kk, :], xt[:, kk, :], AF.Identity, bias=k.mod[:, l, kk, b:b + 1], scale=k.A1[:, l, kk, b:b + 1])
        for m in range(19):
            ps = k.PSB[1 + (it % 3)]
            c = co[it % 4]
            for kk in range(8):
                P.mm(ps[:, :], lhsT=win[:, kk, m * 128:(m + 1) * 128], rhs=hb[:, kk, :], start=(kk == 0), stop=(kk == 7))
            if it % 2 == 0:
                P.act(c[:], ps[:, :], AF.Copy)
            else:
                P.copy(c[:], ps[:, :])
            P.dma('sp', k.COLS[m * 128:(m + 1) * 128, tok], c[:], writes=['COLS:%d:%d' % (m, n)])
            it += 1
    P.pop()


def cols_res(m, s):
    return ['COLS:%d:%d' % (m, n) for n in range(s * 4, s * 4 + 4)]


EXPH = 0.6065306597126334


SKIP = set()


def stage_rwkv(k, l, s):
    if 'rwkv' in SKIP:
        return
    P = k.P
    rp, mu = k.rp, k.mu
    psA, psB, psC, psD, psE, psF = k.psA, k.psB, k.psC, k.psD, k.psE, k.psF
    t0 = s * S
    P.push()
    w2a2b = P.sb('w2a2b', [128, 512], BF16)
    g2ab = P.sb('g2ab', [128, 512], BF16)
    g2bv = P.sb('g2bv', [64, 512], BF16)
    k.ldw(w2a2b[:], k.w2a2[l, :, :])
    k.ldw(g2ab[:], k.g2a[l, :, :])
    k.ldw(g2bv[:], k.g2bv2[l, :, :])
    KR = P.sb('KR', [128, 4, 32, 2, 64], BF16)
    BT = P.sb('BT', [128, 4, 2048], BF16)
    KT = P.sb('KT', [128, 4, 2048], BF16)
    VB = P.sb('VB', [128, 4, 2048], BF16)
    WC = P.sb('WC', [128, 4, 32], F32)
    P.push()
    ush = P.sb('ush', [128, 2049], F32)
    shd = P.sb('shd', [128, 2048], F32)
    lt = P.sb('lt', [128, 2048], F32)
    lwb = P.sb('lwb', [128, 2048], BF16)
    sg1 = P.sb('sg1', [128, 2048], BF16)
    lg2 = P.sb('lg2', [64, 2048], BF16)
    P.memset(ush[:, 0:1], 0.0)

    def shift_load(m, dst, rows=128):
        P.dma('sp', ush[0:rows, 1:2049], k.COLS[m * 128:m * 128 + rows, t0:t0 + S], reads=cols_res(m, s))
        P.tt(shd[0:rows, :], ush[0:rows, 0:2048], ush[0:rows, 1:2049], ALU.subtract)
        P.stt(dst, shd[0:rows, :], mu[0:rows, l, m:m + 1], ush[0:rows, 1:2049], ALU.mult, ALU.add)

    shift_load(16, lt[:, :])
    P.act(lwb[0:64, :], lt[0:64, :], AF.Tanh)
    P.copy(lwb[64:128, :], lt[64:128, :])
    shift_load(17, lt[:, :])
    P.act(sg1[:, :], lt[:, :], AF.Sigmoid)
    shift_load(18, lt[0:64, :], rows=64)
    P.act(lg2[0:32, :], lt[0:32, :], AF.Sigmoid)
    P.copy(lg2[32:64, :], lt[32:64, :])
    rj = P.sb('rj', [128, 2048], F32)
    kj = P.sb('kj', [128, 2048], F32)
    vj = P.sb('vj', [128, 2048], F32)
    names = ['sig', 'aa', 'gg', 'vm', 'vf', 'kk', 'rn', 't1', 'kp', 'be', 'cs', 'Wt', 'Wi', 'csm', 'Wp', 'bon']
    W_ = {nm: P.sb(nm, [128, 512], F32) for nm in names}
    kq = P.sb('kq', [128, 512], BF16)
    rk = P.sb('rk', [128, 512], BF16)
    c3 = lambda ap: ap.rearrange("p (c t) -> p c t", t=64)
    for j in range(4):
        jc = slice(j * 128, (j + 1) * 128)
        shift_load(4 + j, rj[:, :])
        shift_load(8 + j, kj[:, :])
        shift_load(12 + j, vj[:, :])
        for n in range(4):
            tk = slice(n * 512, (n + 1) * 512)
            gt = slice(t0 + n * 512, t0 + (n + 1) * 512)
            sig, aa, gg, vm, vf, kk, rn, t1, kp, be, cs, Wt, Wi, csm, Wp, bon = [W_[nm] for nm in names]
            P.mm(psD[:, :], lhsT=w2a2b[0:64, jc], rhs=lwb[0:64, tk])
            P.act(sig[:], psD[:, :], AF.Sigmoid, bias=rp[:, 0, l, j:j + 1])
            P.mm(psE[:, :], lhsT=w2a2b[64:128, jc], rhs=lwb[64:128, tk])
            P.act(aa[:], psE[:, :], AF.Sigmoid, bias=rp[:, 1, l, j:j + 1])
            P.mm(psF[:, :], lhsT=g2ab[:, jc], rhs=sg1[:, tk], start=True, stop=False)
            P.mm(psF[:, :], lhsT=g2bv[0:32, jc], rhs=lg2[0:32, tk], start=False, stop=True)
            P.copy(gg[:], psF[:, :])
            P.dma('sp', k.GS[jc, gt], gg[:])
            if l > 0:
                P.mm(psC[:, :], lhsT=g2bv[32:64, jc], rhs=lg2[32:64, tk])
                P.act(vm[:], psC[:, :], AF.Sigmoid, bias=k.v0s[:, l, j:j + 1])
                P.dma('sp', vf[:], k.VF[jc, gt])
                P.tt(vf[:], vf[:], vj[:, tk], ALU.subtract)
                P.tt(vf[:], vf[:], vm[:], ALU.mult)
                P.tt(vj[:, tk], vj[:, tk], vf[:], ALU.add)
            else:
                P.dma('sp', k.VF[jc, gt], vj[:, tk])
            P.copy(VB[:, j, tk], vj[:, tk], e='pool')
            P.ts(kk[:], kj[:, tk], rp[:, 2, l, j:j + 1], ALU.mult)
            P.act(kq[:], kk[:], AF.Square)
            P.mm(psD[:, :], lhsT=k.blkb[:, :], rhs=kq[:])
            P.act(rn[:], psD[:, :], AF.Sqrt)
            P.ts(rn[:], rn[:], 1e-12, ALU.max)
            P.recip(rn[:], rn[:])
            P.tt(kk[:], kk[:], rn[:], ALU.mult)
            P.ts(t1[:], aa[:], -1.0, ALU.add, rp[:, 3, l, j:j + 1], ALU.mult)
            P.stt(kp[:], t1[:], 1.0, kj[:, tk], ALU.add, ALU.mult)
            P.tt(be[:], kk[:], aa[:], ALU.mult, e='pool')
            P.op('dve', lambda eng, cs=cs, sig=sig: eng.tensor_tensor_scan(out=cs[:], data0=k.rmask[:], data1=sig[:], initial=0.0, op0=ALU.mult, op1=ALU.add),
                 reads=[k.rmask[:], sig[:]], writes=[cs[:]])
            P.act(Wt[:], cs[:], AF.Exp, scale=-EXPH)
            P.act(Wi[:], cs[:], AF.Exp, scale=EXPH)
            P.tt(csm[:], cs[:], sig[:], ALU.subtract, e='pool')
            P.act(Wp[:], csm[:], AF.Exp, scale=-EXPH)
            P.copy(WC[:, j, n * 8:(n + 1) * 8], c3(Wt[:])[:, :, 63])
            P.tt(KR[:, j, n * 8:(n + 1) * 8, 0, :], c3(kk[:]), c3(Wp[:]), ALU.mult)
            P.tt(KR[:, j, n * 8:(n + 1) * 8, 1, :], c3(rj[:, tk]), c3(Wt[:]), ALU.mult)
            P.tt(BT[:, j, tk], be[:], Wi[:], ALU.mult, e='pool')
            P.tt(KT[:, j, tk], kp[:], Wi[:], ALU.mult)
            P.stt(rk[:], rj[:, tk], rp[:, 6, l, j:j + 1], kp[:], ALU.mult, ALU.mult)
            P.mm(psE[:, :], lhsT=k.blkb[:, :], rhs=rk[:])
            P.tt(bon[:], psE[:, :], vj[:, tk], ALU.mult)
            P.dma('sp', k.BON[jc, gt], bon[:])
    P.pop()
    P.push()
    Mf = P.sb('Mf', [128, 4, 128], F32)
    Mb = P.sb('Mb', [128, 4, 128], BF16)
    Mt = P.sb('Mt', [128, 4, 128], F32)
    blk32 = P.sb('blk32', [128, 128], F32)
    P.copy(blk32[:], k.cst[:, 128:256])
    P.memset(Mf[:], 0.0)
    P.memset(Mb[:], 0.0)
    tok3 = P.sb('tok3', [64, 3, 512], BF16)
    m1 = P.sb('m1', [64, 8, 128], BF16)
    m2 = P.sb('m2', [64, 8, 128], BF16)
    am = P.sb('am', [64, 8, 64], BF16)
    xs = [P.sb('xs%d' % i, [64, 8, 64], BF16) for i in range(2)]
    zs = [P.sb('zs%d' % i, [64, 8, 64], BF16) for i in range(2)]
    pps = [P.sb('pp%d' % i, [64, 8, 64], BF16) for i in range(2)]
    nr = P.sb('nr', [64, 8, 64], BF16)
    ub = P.sb('ub', [64, 8, 64], BF16)
    yt8 = P.sb('yt8', [64, 8, 8, 64], F32)
    ysq = P.sb('ysq', [64, 64, 64], F32)
    st_ = {nm: P.sb(nm, [64, 64], F32) for nm in ('s1', 's2', 'mean', 'msq', 'var', 'rstd')}
    yfm = P.sb('yfm', [128, 4, 512], F32)
    ye = P.sb('ye', [128, 512], F32)
    bo2 = P.sb('bo2', [128, 512], F32)
    gg2 = P.sb('gg2', [128, 512], F32)
    yo16 = P.sb('yo16', [128, 512], BF16)
    pAb = psA[:, :].bitcast(BF16)
    h8 = lambda ap, w: ap.rearrange("p (h c) -> p h c", c=w)
    hp = lambda h: slice((h % 2) * 64, (h % 2) * 64 + 64)
    pos = lambda h: (h % 2) * 4 + h // 2
    tok3s = [tok3, P.sb('tok3b', [64, 3, 512], BF16)]
    m1s = [m1, P.sb('m1b', [64, 8, 128], BF16)]
    m2s = [m2, P.sb('m2b', [64, 8, 128], BF16)]
    ppss = [pps, [P.sb('ppb%d' % i, [64, 8, 64], BF16) for i in range(2)]]
    TTs = [None, None]

    def phase1(c):
        b_ = c % 2
        cs_ = slice(c * 64, (c + 1) * 64)
        tk3, m1_, m2_, pp_ = tok3s[b_], m1s[b_], m2s[b_], ppss[b_]
        for i, src in enumerate((VB, BT, KT)):
            for j in range(4):
                P.tr(pAb[0:64, i * 512 + j * 128:i * 512 + (j + 1) * 128], src[:, j, cs_], k.idb[:, :])
        P.act(tk3[:].rearrange("p a b -> p (a b)"), pAb[0:64, 0:1536], AF.Copy)
        yield
        for h in range(8):
            P.mm(psB[0:64, pos(h) * 128:(pos(h) + 1) * 128], lhsT=BT[hp(h), h // 2, cs_],
                 rhs=KR[hp(h), h // 2, c, :, :].rearrange("p a b -> p (a b)"))
        P.tt(m1_[:], h8(psB[0:64, :], 128), bc(k.mk1[:].unsqueeze(1), [64, 8, 128]), ALU.mult)
        yield
        for h in range(8):
            P.mm(psA[0:64, pos(h) * 128:(pos(h) + 1) * 128], lhsT=KT[hp(h), h // 2, cs_],
                 rhs=KR[hp(h), h // 2, c, :, :].rearrange("p a b -> p (a b)"))
        P.tt(m2_[:], h8(psA[0:64, :], 128), bc(k.mk1[:].unsqueeze(1), [64, 8, 128]), ALU.mult)
        yield
        for h in range(8):
            o0 = (h % 2) * 512 + (h // 2) * 64
            P.mm(psB[0:64, o0:o0 + 64], lhsT=KR[hp(h), h // 2, c, 0, :], rhs=BT[hp(h), h // 2, cs_])
        P.tt(am[:].rearrange("p (q j) c -> p q j c", q=2),
             psB[0:64, :].rearrange("p (q x) -> p q x", q=2)[:, :, 0:256].rearrange("p q (j c) -> p q j c", c=64),
             bc(k.mk3[:].unsqueeze(1).unsqueeze(1), [64, 2, 4, 64]), ALU.mult)
        P.tt(pp_[0][:], bc(k.id64b[:].unsqueeze(1), [64, 8, 64]), m1_[:, :, 0:64], ALU.subtract)
        yield
        X = m1_[:, :, 0:64]
        Z = am[:]
        Pc = pp_[0]
        for lev in range(1, 6):
            Xn = xs[lev % 2]
            Zn = zs[lev % 2]
            Pn = pp_[lev % 2]
            if lev < 5:
                for h in range(8):
                    P.mm(psE[0:64, h * 64:(h + 1) * 64], lhsT=Z[:, h, :], rhs=X[:, h, :])
            for h in range(8):
                P.mm(psF[0:64, h * 64:(h + 1) * 64], lhsT=X[:, h, :], rhs=Z[:, h, :])
            if lev < 5:
                P.act(Xn[:], h8(psE[0:64, :], 64), AF.Copy)
            P.copy(Zn[:], h8(psF[0:64, :], 64))
            yield
            for h in range(8):
                P.mm(psA[0:64, h * 64:(h + 1) * 64], lhsT=Zn[:, h, :], rhs=Pc[:, h, :])
            P.tt(Pn[:], h8(psA[0:64, 0:512], 64), Pc[:], ALU.add)
            yield
            X, Z, Pc = Xn[:], Zn[:], Pn
        TTs[b_] = Pc

    def phase2(c):
        b_ = c % 2
        tk3, m1_, m2_, TT = tok3s[b_], m1s[b_], m2s[b_], TTs[b_]
        Vt = tk3[:, 0, :]
        Bt = tk3[:, 1, :]
        Kt = tk3[:, 2, :]
        for j in range(4):
            jc = slice(j * 128, (j + 1) * 128)
            P.mm(psD[0:64, jc], lhsT=KR[:, j, c, 0, :], rhs=Mb[:, j, :], start=True, stop=False)
            for h in (2 * j, 2 * j + 1):
                P.mm(psD[0:64, h * 64:(h + 1) * 64], lhsT=m2_[:, pos(h), 0:64], rhs=Vt[:, h * 64:(h + 1) * 64],
                     start=False, stop=(h == 2 * j + 1))
        P.act(nr[:], h8(psD[0:64, :], 64), AF.Copy, scale=-1.0)
        yield
        for h in range(8):
            P.mm(psC[0:64, h * 64:(h + 1) * 64], lhsT=TT[:, pos(h), :], rhs=nr[:, h, :])
        P.copy(ub[:], h8(psC[0:64, :], 64))
        yield
        ubf = ub[:].rearrange("p h c -> p (h c)")
        for j in range(4):
            jc = slice(j * 128, (j + 1) * 128)
            P.mm(psD[0:64, jc], lhsT=KR[:, j, c, 1, :], rhs=Mb[:, j, :], start=True, stop=False)
            for h in (2 * j, 2 * j + 1):
                o = psD[0:64, h * 64:(h + 1) * 64]
                P.mm(o, lhsT=m1_[:, pos(h), 64:128], rhs=ub[:, h, :], start=False, stop=False)
                P.mm(o, lhsT=m2_[:, pos(h), 64:128], rhs=Vt[:, h * 64:(h + 1) * 64], start=False, stop=(h == 2 * j + 1))
        P.act(yt8[:, c % 8, :, :], h8(psD[0:64, :], 64), AF.Copy)
        yield
        for j in range(4):
            jc = slice(j * 128, (j + 1) * 128)
            P.mm(psC[:, jc], lhsT=Bt[:, jc], rhs=ubf[:, jc], start=True, stop=False)
            P.mm(psC[:, jc], lhsT=Kt[:, jc], rhs=Vt[:, jc], start=False, stop=True)
        P.tt(Mt[:], h8(psC[:, :], 128), bc(blk32[:].unsqueeze(1), [128, 4, 128]), ALU.mult)
        yield
        P.tt(Mf[:], Mt[:], Mf[:], ALU.add)
        P.tt(Mf[:], Mf[:], bc(WC[:, :, c:c + 1], [128, 4, 128]), ALU.mult)
        P.copy(Mb[:], Mf[:], e='pool')
        yield

    def gn_block(c):
        s1, s2, mean, msq, var, rstd = [st_[nm] for nm in ('s1', 's2', 'mean', 'msq', 'var', 'rstd')]
        y3 = yt8[:].rearrange("p a h c -> p (a h) c")
        P.reduce(s1[:], y3, ALU.add)
        P.act(ysq[:], y3, AF.Square)
        P.reduce(s2[:], ysq[:], ALU.add)
        P.ts(mean[:], s1[:], 1.0 / 64, ALU.mult)
        P.tt(msq[:], mean[:], mean[:], ALU.mult)
        P.stt(var[:], s2[:], 1.0 / 64, msq[:], ALU.mult, ALU.subtract)
        P.act(rstd[:], var[:], AF.Sqrt, bias=k.epst[0:64, 1:2])
        P.recip(rstd[:], rstd[:])
        P.tt(y3, y3, bc(mean[:].unsqueeze(2), [64, 64, 64]), ALU.subtract)
        P.tt(y3, y3, bc(rstd[:].unsqueeze(2), [64, 64, 64]), ALU.mult)
        for half in range(2):
            for a in range(4):
                ca = half * 4 + a
                for j in range(4):
                    P.tr(psB[:, (a * 4 + j) * 64:(a * 4 + j + 1) * 64],
                         yt8[:, ca, 2 * j:2 * j + 2, :].rearrange("p h c -> p (h c)"), k.idf[0:64, 0:64])
            P.copy(yfm[:, :, half * 256:(half + 1) * 256].rearrange("p j (a t) -> p a j t", t=64),
                   psB[:, :].rearrange("p (a j t) -> p a j t", j=4, t=64))
        n = c // 8
        gt = slice(t0 + n * 512, t0 + (n + 1) * 512)
        for j in range(4):
            jc = slice(j * 128, (j + 1) * 128)
            P.ts(ye[:], yfm[:, j, :], rp[:, 4, l, j:j + 1], ALU.mult, rp[:, 5, l, j:j + 1], ALU.add)
            P.dma('sp', bo2[:], k.BON[jc, gt])
            P.dma('sp', gg2[:], k.GS[jc, gt])
            P.tt(ye[:], ye[:], bo2[:], ALU.add)
            P.tt(yo16[:], ye[:], gg2[:], ALU.mult)
            P.dma('sp', k.YMIX[512 + j * 128:512 + (j + 1) * 128, gt], yo16[:])

    def drain(g):
        for _ in g:
            pass

    NCH = 0 if 'rec' in SKIP else 32
    if NCH:
        drain(phase1(0))
    for c in range(NCH):
        g2 = phase2(c)
        g1 = phase1(c + 1) if c + 1 < NCH else iter(())
        a1 = a2 = True
        while a1 or a2:
            if a2:
                a2 = next(g2, 'end') != 'end'
            if a1:
                a1 = next(g1, 'end') != 'end'
        if c % 8 == 7:
            gn_block(c)
    P.pop()
    P.pop()


NIT = 13


def stage_dsa(k, l, s):
    if 'dsa' in SKIP:
        return
    P = k.P
    psA, psB, psC, psD, psE, psF = k.psA, k.psB, k.psC, k.psD, k.psE, k.psF
    t0 = s * S
    P.push()
    wq = P.sb('wq', [128, 2, 512], BF16)
    wqi = P.sb('wqi', [128, 2, 256], BF16)
    wk = P.sb('wk', [128, 4, 128], BF16)
    wv = P.sb('wv', [128, 8, 128], BF16)
    for kk in range(2):
        k.ldw(wq[:, kk, :], k.w_q[l, kk * 128:(kk + 1) * 128, :])
        k.ldw(wqi[:, kk, :], k.w_qi[l, kk * 128:(kk + 1) * 128, :])
    k.ldw(wk[:], k.wkT[l, :, :, :])
    k.ldw(wv[:], k.wvP[l, :, :, :])
    blkf = P.sb('blkf', [128, 128], F32)
    P.copy(blkf[:], k.cst[:, 128:256])
    cqb = P.sb('cqb', [128, 2, 2048], BF16)
    ckvT = P.sb('ckvT', [128, 2048], BF16)
    ckvTok = P.sb('ckvTok', [128, 16, 128], BF16)
    kix = P.sb('kix', [128, 2048], BF16)
    qab = P.sb('qab', [128, 8, 2048], BF16)
    qib = P.sb('qib', [128, 2, 2048], BF16)
    wht = P.sb('wht', [128, 16, 4], F32)
    pAb = psA[:, :].bitcast(BF16)
    P.push()
    cq32 = P.sb('cq32', [128, 2, 512], F32)
    sq = P.sb('sq', [128, 2, 512], BF16)
    rs = P.sb('rs', [128, 512], F32)
    kv32 = P.sb('kv32', [128, 512], F32)
    ki32 = P.sb('ki32', [128, 512], F32)
    sqf = P.sb('sqf', [128, 512], F32)
    mean = P.sb('mean', [128, 512], F32)
    msq = P.sb('msq', [128, 512], F32)
    var = P.sb('var', [128, 512], F32)
    wi = P.sb('wi', [4, 512], F32)
    qb = P.sb('qb', [128, 4, 512], BF16)
    for n in range(4):
        tk = slice(n * 512, (n + 1) * 512)
        gt = slice(t0 + n * 512, t0 + (n + 1) * 512)
        nn = s * 4 + n
        for kk in range(2):
            P.dma('sp', cq32[:, kk, :], k.COLS[kk * 128:(kk + 1) * 128, gt], reads=['COLS:%d:%d' % (kk, nn)])
        k.rstd_of(cq32[:], 2, 512, 1.0 / 256, 1e-6, sq, psC, rs)
        P.tt(cq32[:], cq32[:], bc(rs[:].unsqueeze(1), [128, 2, 512]), ALU.mult)
        for kk in range(2):
            P.act(cqb[:, kk, tk], cq32[:, kk, :], AF.Copy, scale=k.qg[:, l, kk:kk + 1])
        P.dma('sp', kv32[:], k.COLS[256:384, gt], reads=['COLS:2:%d' % nn])
        k.rstd_of(kv32[:].unsqueeze(1), 1, 512, 1.0 / 128, 1e-6, sq, psC, rs)
        P.tt(kv32[:], kv32[:], rs[:], ALU.mult)
        P.act(ckvT[:, tk], kv32[:], AF.Copy, scale=k.kg[:, l:l + 1])
        for i in range(4):
            P.tr(pAb[:, i * 128:(i + 1) * 128], ckvT[:, n * 512 + i * 128:n * 512 + (i + 1) * 128], k.idb[:, :])
        P.copy(ckvTok[:, n * 4:(n + 1) * 4, :], pAb[:, 0:512].rearrange("p (a b) -> p a b", b=128))
        P.dma('sp', ki32[0:64, :], k.COLS[384:448, gt], reads=['COLS:3:%d' % nn])
        P.dma('sp', ki32[64:128, :], k.COLS[384:448, gt], reads=['COLS:3:%d' % nn])
        P.mm(psD[:, :], lhsT=blkf[:, :], rhs=ki32[:, :])
        P.act(sqf[:], ki32[:], AF.Square)
        P.mm(psE[:, :], lhsT=blkf[:, :], rhs=sqf[:, :])
        P.ts(mean[:], psD[:, :], 1.0 / 64, ALU.mult)
        P.tt(msq[:], mean[:], mean[:], ALU.mult)
        P.stt(var[:], psE[:, :], 1.0 / 64, msq[:], ALU.mult, ALU.subtract)
        P.act(var[:], var[:], AF.Sqrt, bias=k.epst[:, 0:1])
        P.recip(var[:], var[:])
        P.tt(ki32[:], ki32[:], mean[:], ALU.subtract)
        P.tt(ki32[:], ki32[:], var[:], ALU.mult)
        P.act(kix[:, tk], ki32[:], AF.Identity, bias=k.kl[:, l, 1:2], scale=k.kl[:, l, 0:1])
        P.dma('sp', wi[:, :], k.COLS[448:452, gt], reads=['COLS:3:%d' % nn])
        for i in range(4):
            P.tr(psD[:, i * 4:(i + 1) * 4], wi[0:4, i * 128:(i + 1) * 128], k.idf[0:4, 0:4])
        P.ts(wht[:, n * 4:(n + 1) * 4, :], psD[:, 0:16].rearrange("p (a b) -> p a b", b=4), 0.0625, ALU.mult)
        for m in range(4):
            for kk in range(2):
                P.mm(psE[:, :], lhsT=wq[:, kk, m * 128:(m + 1) * 128], rhs=cqb[:, kk, tk], start=(kk == 0), stop=(kk == 1))
            P.copy(qb[:, m, :], psE[:, :])
        for h in range(8):
            hp = slice((h % 2) * 64, (h % 2) * 64 + 64)
            P.mm(psF[:, :], lhsT=wk[hp, h // 2, :], rhs=qb[hp, h // 2, :])
            P.act(qab[:, h, tk], psF[:, :], AF.Copy, scale=0.125)
        for m in range(2):
            for kk in range(2):
                P.mm(psE[:, :], lhsT=wqi[:, kk, m * 128:(m + 1) * 128], rhs=cqb[:, kk, tk], start=(kk == 0), stop=(kk == 1))
            P.copy(qib[:, m, tk], psE[:, :])
    P.pop()
    P.push()
    sc = P.sb('sc', [128, 2048], F32)
    tmp = P.sb('tmp', [128, 2048], F32)
    junk = P.sb('junk', [128, 2048], BF16)
    maskb = P.sb('maskb', [128, 2048], BF16)
    mT = P.sb('mT', [128, 2048], BF16)
    E = P.sb('E', [128, 1024], BF16)
    Pm = P.sb('Pm', [128, 1024], BF16)
    rd = P.sb('rd', [128, 1024], F32)
    olat = P.sb('olat', [128, 1024], BF16)
    yd = P.sb('yd', [128, 4, 128], BF16)
    sm = {nm: P.sb(nm, [128, 1], F32) for nm in ('hi', 'lo', 'w0', 'mid', 'cnt', 'gh')}
    Hs = P.sb('Hs', [128, 32], F32)
    mTs = [mT, P.sb('mT2', [128, 2048], BF16)]
    pEb = psE[:, :].bitcast(BF16)
    pFb = psF[:, :].bitcast(BF16)
    lo = sm['lo']

    def s_score(qt):
        q_ = slice(qt * 128, (qt + 1) * 128)
        N = (qt + 1) * 128
        ib = 0
        for hi in range(4):
            hp = slice((hi % 2) * 64, (hi % 2) * 64 + 64)
            for k0 in range(0, N, 512):
                cw = min(512, N - k0)
                pst = psE if ib % 2 == 0 else psF
                ib += 1
                P.mm(pst[:, 0:cw], lhsT=qib[hp, hi // 2, q_], rhs=kix[hp, k0:k0 + cw])
                dst = sc if hi == 0 else tmp
                P.ts(dst[:, k0:k0 + cw], pst[:, 0:cw], 0.0, ALU.max, wht[:, qt, hi:hi + 1], ALU.mult)
            if hi > 0:
                P.tt(sc[:, 0:N], sc[:, 0:N], tmp[:, 0:N], ALU.add)
        P.memset(sc[0:64, qt * 128 + 64:(qt + 1) * 128], NEG, e='dve')

    def s_select(qt):
        N = (qt + 1) * 128
        if qt >= 2:
            P.reduce(sm['hi'][:], sc[:, 0:N], ALU.max)
            P.reduce(lo[:], sc[:, 0:qt * 128 + 64], ALU.min)
            P.tt(sm['w0'][:], sm['hi'][:], lo[:], ALU.subtract)
            P.ts(Hs[:, 0:NIT + 1], k.cst[:, 448:448 + NIT + 1], sm['w0'][:, 0:1], ALU.mult)
            P.tt(lo[:], lo[:], Hs[:, 0:1], ALU.add)
            for i in range(NIT):
                P.ts(junk[:, 0:N], sc[:, 0:N], lo[:, 0:1], ALU.is_gt, None, ALU.add, accum=sm['cnt'][:])
                P.ts(sm['gh'][:], sm['cnt'][:], 255.5, ALU.is_gt, Hs[:, i:i + 1], ALU.mult)
                sub = Hs[:, i + 1:i + 2] if i < NIT - 1 else Hs[:, i:i + 1]
                P.stt(lo[:], sm['gh'][:], sub, lo[:], ALU.subtract, ALU.add)
        else:
            P.memset(lo[:], -1.0e29, e='dve')
        P.ts(maskb[:, 0:N], sc[:, 0:N], lo[:, 0:1], ALU.is_gt)
        mTq = mTs[qt % 2]
        for kt in range(qt + 1):
            pb_ = pEb if kt < 8 else pFb
            P.tr(pb_[:, (kt % 8) * 128:(kt % 8 + 1) * 128], maskb[:, kt * 128:(kt + 1) * 128], k.idb[:, :])
        P.copy(mTq[:, 0:min(N, 1024)], pEb[:, 0:min(N, 1024)])
        if N > 1024:
            P.copy(mTq[:, 1024:N], pFb[:, 0:N - 1024])

    def t_attn(qt):
        q_ = slice(qt * 128, (qt + 1) * 128)
        mTq = mTs[qt % 2]
        for kt in range(qt + 1):
            kk_ = slice(kt * 128, (kt + 1) * 128)
            for g in range(2):
                P.mm(psA[:, g * 512:(g + 1) * 512], lhsT=ckvT[:, kk_], rhs=qab[:, g * 4:(g + 1) * 4, q_])
            P.act(E[:, :], psA[:, :], AF.Exp)
            P.tt(Pm[:].rearrange("p (h q) -> p h q", q=128), E[:].rearrange("p (h q) -> p h q", q=128),
                 bc(mTq[:, kk_].unsqueeze(1), [128, 8, 128]), ALU.mult, e='pool')
            for g in range(2):
                P.mm(psB[:, g * 512:(g + 1) * 512], lhsT=ckvTok[:, kt, :], rhs=Pm[:, g * 512:(g + 1) * 512],
                     start=(kt == 0), stop=(kt == qt))
            P.mm(psC[:, :], lhsT=k.onesb[:, :], rhs=Pm[:, 0:512], start=(kt == 0), stop=(kt == qt))
            P.mm(psD[:, :], lhsT=k.onesb[:, :], rhs=Pm[:, 512:1024], start=(kt == 0), stop=(kt == qt))

    def t_fin(qt):
        P.recip(rd[:, 0:512], psC[:, :])
        P.recip(rd[:, 512:1024], psD[:, :])
        P.tt(olat[:, :], psB[:, :], rd[:, :], ALU.mult)
        for j in range(4):
            o = psA[:, j * 128:(j + 1) * 128]
            P.mm(o, lhsT=wv[:, 2 * j, :], rhs=olat[:, 2 * j * 128:(2 * j + 1) * 128], start=True, stop=False)
            P.mm(o, lhsT=wv[:, 2 * j + 1, :], rhs=olat[:, (2 * j + 1) * 128:(2 * j + 2) * 128], start=False, stop=True)
        P.act(yd[:], psA[:, 0:512].rearrange("p (j q) -> p j q", q=128), AF.Copy)
        P.dma('sp', k.YMIX[0:512, t0 + qt * 128:t0 + (qt + 1) * 128].rearrange("(j p) q -> p j q", p=128), yd[:])

    s_score(0)
    s_select(0)
    for qt in range(16):
        if qt + 1 < 16:
            s_score(qt + 1)
        t_attn(qt)
        if qt + 1 < 16:
            s_select(qt + 1)
        t_fin(qt)
    P.pop()
    P.pop()


def stage_out_ffn(k, l):
    if 'ffn' in SKIP:
        return
    P = k.P
    psC, psD, psE, psF = k.psC, k.psD, k.psE, k.psF
    P.push()
    wo = P.sb('wo', [128, 8, 1024], BF16)
    for kk in range(8):
        k.ldw(wo[:, kk, :], k.w_out[l, kk * 128:(kk + 1) * 128, :])
    ym = P.sb('ym', [128, 8, 512], BF16)
    yo = P.sb('yo', [128, 8, 512], F32)
    xt = P.sb('xt', [128, 8, 512], F32)
    sq = P.sb('sq', [128, 8, 512], BF16)
    rs = P.sb('rs', [128, 512], F32)
    hb = P.sb('hb', [128, 8, 512], BF16)
    it = 0
    for n in range(T // 512):
        b = n // 4
        tok = slice(n * 512, (n + 1) * 512)
        P.dma('sp', ym[:], k.YMIX[:, tok].rearrange("(k p) n -> p k n", p=128))
        P.dma('sp', xt[:], k.XT[:, tok].rearrange("(k p) n -> p k n", p=128))
        for m in range(8):
            ps = k.PSB[1 + (it % 3)]
            it += 1
            for kk in range(8):
                P.mm(ps[:, :], lhsT=wo[:, kk, m * 128:(m + 1) * 128], rhs=ym[:, kk, :], start=(kk == 0), stop=(kk == 7))
            if m % 2 == 0:
                P.act(yo[:, m, :], ps[:, :], AF.Copy)
            else:
                P.copy(yo[:, m, :], ps[:, :])
        k.rstd_of(yo[:], 8, 512, 1.0 / D, 1e-6, sq, psC, rs)
        P.tt(yo[:], yo[:], bc(rs[:].unsqueeze(1), [128, 8, 512]), ALU.mult)
        for m in range(8):
            P.stt(xt[:, m, :], yo[:, m, :], k.G1[:, l, m, b:b + 1], xt[:, m, :], ALU.mult, ALU.add)
        P.dma('sp', k.XT[:, tok].rearrange("(k p) n -> p k n", p=128), xt[:])
        k.rstd_of(xt[:], 8, 512, 1.0 / D, 1e-6, sq, psC, rs)
        P.tt(yo[:], xt[:], bc(rs[:].unsqueeze(1), [128, 8, 512]), ALU.mult)
        for kk in range(8):
            P.act(hb[:, kk, :], yo[:, kk, :], AF.Identity, bias=k.mod[:, l, 24 + kk, b:b + 1], scale=k.A2[:, l, kk, b:b + 1])
        P.dma('sp', k.H2[:, tok].rearrange("(k p) n -> p k n", p=128), hb[:])
    P.pop()
    P.push()
    wg = [P.sb('wg%d' % i, [128, 8, 512], BF16) for i in range(2)]
    wu = [P.sb('wu%d' % i, [128, 8, 512], BF16) for i in range(2)]
    h2 = [P.sb('h2%d' % i, [128, 8, 512], BF16) for i in range(2)]
    sgl = [P.sb('sgl%d' % i, [128, 512], F32) for i in range(2)]
    u16 = [P.sb('u16%d' % i, [128, 512], BF16) for i in range(2)]
    def load_w(grp):
        nch = min(4, 22 - grp * 4)
        g_, u_ = wg[grp % 2], wu[grp % 2]
        for kk in range(8):
            k.ldw(g_[:, kk, 0:nch * 128], k.w_fc[l, kk * 128:(kk + 1) * 128, grp * 512:grp * 512 + nch * 128])
            k.ldw(u_[:, kk, 0:nch * 128], k.w_fc[l, kk * 128:(kk + 1) * 128, DFF + grp * 512:DFF + grp * 512 + nch * 128])

    iters = [(grp, n) for grp in range(6) for n in range(T // 512)]

    def load_h(i):
        n = iters[i][1]
        P.dma('sp', h2[i % 2][:], k.H2[:, n * 512:(n + 1) * 512].rearrange("(k p) n -> p k n", p=128))

    load_w(0)
    load_h(0)
    it = 0
    for i, (grp, n) in enumerate(iters):
        nch = min(4, 22 - grp * 4)
        g_, u_ = wg[grp % 2], wu[grp % 2]
        if n == 0 and grp + 1 < 6:
            load_w(grp + 1)
        if i + 1 < len(iters):
            load_h(i + 1)
        tok = slice(n * 512, (n + 1) * 512)
        hh = h2[i % 2]
        for c in range(nch):
            pg = psC if it % 2 == 0 else psE
            pu = psD if it % 2 == 0 else psF
            sg_, uu = sgl[it % 2], u16[it % 2]
            it += 1
            for kk in range(8):
                P.mm(pg[:, :], lhsT=g_[:, kk, c * 128:(c + 1) * 128], rhs=hh[:, kk, :], start=(kk == 0), stop=(kk == 7))
            for kk in range(8):
                P.mm(pu[:, :], lhsT=u_[:, kk, c * 128:(c + 1) * 128], rhs=hh[:, kk, :], start=(kk == 0), stop=(kk == 7))
            P.act(sg_[:], pg[:, :], AF.Silu)
            P.tt(uu[:], sg_[:], pu[:, :], ALU.mult)
            r0 = (grp * 4 + c) * 128
            P.dma('sp', k.UT[r0:r0 + 128, tok], uu[:], writes=['UT:%d:%d' % (grp * 4 + c, n)])
    P.pop()
    P.push()
    wd = P.sb('wd', [128, 22, 1024], BF16)
    for kk in range(22):
        k.ldw(wd[:, kk, :], k.w_down[l, kk * 128:(kk + 1) * 128, :])
    uts = [P.sb('ut%d' % i, [128, 22, 512], BF16) for i in range(2)]
    xts = [P.sb('xtd%d' % i, [128, 8, 512], F32) for i in range(2)]
    yo = P.sb('yo', [128, 8, 512], F32)
    sq = P.sb('sq', [128, 8, 512], BF16)
    rs = P.sb('rs', [128, 512], F32)

    def load_t(n):
        tok = slice(n * 512, (n + 1) * 512)
        P.dma('sp', uts[n % 2][:], k.UT[:, tok].rearrange("(k p) n -> p k n", p=128), reads=['UT:%d:%d' % (c, n) for c in range(22)])
        P.dma('sp', xts[n % 2][:], k.XT[:, tok].rearrange("(k p) n -> p k n", p=128), reads=['XT:%d' % n])

    load_t(0)
    it = 0
    for n in range(T // 512):
        b = n // 4
        tok = slice(n * 512, (n + 1) * 512)
        ut, xt = uts[n % 2], xts[n % 2]
        if n + 1 < T // 512:
            load_t(n + 1)
        for m in range(8):
            ps = k.PSB[1 + (it % 3)]
            it += 1
            for kk in range(22):
                P.mm(ps[:, :], lhsT=wd[:, kk, m * 128:(m + 1) * 128], rhs=ut[:, kk, :], start=(kk == 0), stop=(kk == 21))
            if m % 2 == 0:
                P.act(yo[:, m, :], ps[:, :], AF.Copy)
            else:
                P.copy(yo[:, m, :], ps[:, :])
        k.rstd_of(yo[:], 8, 512, 1.0 / D, 1e-6, sq, psC, rs)
        P.tt(yo[:], yo[:], bc(rs[:].unsqueeze(1), [128, 8, 512]), ALU.mult)
        for m in range(8):
            P.stt(xt[:, m, :], yo[:, m, :], k.G2[:, l, m, b:b + 1], xt[:, m, :], ALU.mult, ALU.add)
        P.dma('sp', k.XT[:, tok].rearrange("(k p) n -> p k n", p=128), xt[:], writes=['XT:%d' % n])
    P.pop()


def prep_shared(inp):
    f = np.float32
    sh = {}
    sh['ada_w'] = np.ascontiguousarray(inp['ada_w'], f)
    sh['ada_bT'] = np.ascontiguousarray(inp['ada_b'].reshape(L, 48, 128).transpose(2, 0, 1), f)
    g = np.stack([inp['pre_g_mix'], inp['post_g_mix'], inp['pre_g_ffn'], inp['post_g_ffn']], 0)
    sh['gains'] = np.ascontiguousarray(g.reshape(4, L, 8, 128).transpose(3, 0, 1, 2), f)
    W = np.zeros((L, D, PC), f)
    M = np.zeros((L, PC), f)
    wi = inp['w_in']
    ms = inp['mu_shift']
    W[:, :, 0:452] = wi[:, :, 0:452]
    W[:, :, 512:2048] = wi[:, :, 452:1988]
    M[:, 512:2048] = ms[:, 0:1536]
    W[:, :, 2048:2176] = wi[:, :, 1988:2116]
    M[:, 2048:2176] = ms[:, 1536:1664]
    W[:, :, 2176:2336] = wi[:, :, 2116:2276]
    M[:, 2176:2336] = ms[:, 1664:1824]
    W[1:, :, 2336:2368] = inp['w_in_vres']
    M[1:, 2336:2368] = inp['mu_vres']
    sh['w_in'] = W
    sh['muT'] = np.ascontiguousarray(M.reshape(L, 19, 128).transpose(2, 0, 1), f)
    sh['w_out'] = np.ascontiguousarray(inp['w_out'], f)
    sh['qng'] = np.ascontiguousarray(inp['q_norm_g'].reshape(L, 2, 128).transpose(2, 0, 1), f)
    sh['kvg'] = np.ascontiguousarray(inp['kv_norm_g'].T, f)
    sh['w_q'] = np.ascontiguousarray(inp['w_q_up'].reshape(L, 256, 512), f)
    sh['w_qi'] = np.ascontiguousarray(inp['w_qi_up'].reshape(L, 256, 256), f)
    wk = inp['w_k_up'].reshape(L, 128, 4, 2, 64)
    sh['wkT'] = np.ascontiguousarray(wk.transpose(0, 3, 4, 2, 1).reshape(L, 128, 4, 128), f)
    wv = np.zeros((L, 128, 8, 128), f)
    for h in range(8):
        wv[:, :, h, (h % 2) * 64:(h % 2) * 64 + 64] = inp['w_v_up'][:, :, h, :]
    sh['wvP'] = wv
    kl = np.stack([inp['kidx_ln_g'], inp['kidx_ln_b']], -1)
    kl = np.concatenate([kl, kl], 1)
    sh['kiln'] = np.ascontiguousarray(kl.transpose(1, 0, 2), f)
    rw = np.stack([inp['w0'], inp['a0'], inp['k_k'], inp['k_a'], inp['lnx_g'], inp['lnx_b'],
                   inp['r_k'].reshape(L, 512)], 0)
    sh['rwp'] = np.ascontiguousarray(rw.reshape(7, L, 4, 128).transpose(3, 0, 1, 2), f)
    v0 = np.zeros((L, 512), f)
    v0[1:] = inp['v0']
    sh['v0T'] = np.ascontiguousarray(v0.reshape(L, 4, 128).transpose(2, 0, 1), f)
    sh['w2a2'] = np.ascontiguousarray(np.concatenate([inp['w2'], inp['a2']], 1), f)
    sh['g2a'] = np.ascontiguousarray(inp['g2'][:, 0:128], f)
    gb = np.zeros((L, 64, 512), f)
    gb[:, 0:32] = inp['g2'][:, 128:160]
    gb[1:, 32:64] = inp['v2']
    sh['g2bv2'] = gb
    sh['w_fc'] = np.ascontiguousarray(inp['w_fc'], f)
    sh['w_down'] = np.ascontiguousarray(inp['w_down'], f)
    c = np.zeros((128, 1024), f)
    c[:, 0:128] = np.eye(128)
    c[0:64, 128:192] = 1.0
    c[64:128, 192:256] = 1.0
    si = np.arange(64)[:, None]
    ti = np.arange(64)[None, :]
    c[0:64, 256:320] = (si < ti)
    c[0:64, 320:384] = (si <= ti)
    c[0:64, 384:448] = (si > ti)
    c[:, 448:480] = 2.0 ** -(np.arange(32) + 1.0)
    sh['consts'] = c
    return sh


def prep_core(inp, ci):
    f = np.float32
    xb = np.asarray(inp['x'][ci * NB:(ci + 1) * NB], f).reshape(T, D)
    cb = np.asarray(inp['c'][ci * NB:(ci + 1) * NB], f)
    return {'xT': np.ascontiguousarray(xb.T),
            'cT': np.ascontiguousarray(cb.reshape(NB, 8, 128).transpose(2, 1, 0))}


_NC = None


def kernel(**inputs):
    global _NC
    inp = {k_: np.asarray(v) for k_, v in inputs.items()}
    if _NC is None:
        _NC = build()
    sh = prep_shared(inp)
    ncores = 8
    in_maps = []
    for ci in range(ncores):
        m = dict(sh)
        m.update(prep_core(inp, ci))
        in_maps.append(m)
    res = run_bass_kernel_spmd(_NC, in_maps, core_ids=list(range(ncores)))
    out = np.empty((16, S, D), np.float32)
    for ci in range(ncores):
        o = np.asarray(res.results[ci]['outT'])
        out[ci * NB:(ci + 1) * NB] = o.T.reshape(NB, S, D)
    return out
```
